# Optimizing a Trainium2 kernel written in Bass

```python
import numpy as np
import jax
import jax.numpy as jnp
from jax import lax

D_MODEL = 1024
BATCH = 4
SEQ = 8192
DEPTH = 4

D_MIX = D_MODEL
BLK = 128
LN_EPS = 1e-5
NORM_EPS = 1e-6
DEEPNORM_ALPHA = (2 * DEPTH) ** 0.25
DEEPNORM_BETA = (8 * DEPTH) ** -0.25

RET_HEADS = 4
RET_DK = 32
RET_DV = 64
RET_W = RET_HEADS * RET_DV
RET_THETA = 10000.0
DIL_HEADS = 4
DIL_DH = 64
DIL_W = DIL_HEADS * DIL_DH
DIL_PATTERNS = ((128, 1), (512, 4), (2048, 16))
ROPE_THETA = 500000.0
ROPE_ROT_DIM = DIL_DH // 4
RWKV_HEADS = 4
RWKV_DH = 64
RWKV_W = RWKV_HEADS * RWKV_DH
DECAY_LORA = 64
AAA_LORA = 64
MV_LORA = 32
GATE_LORA = 128
RWKV_GN_EPS = 64e-5
MLA_HEADS = 4
MLA_NOPE = 64
MLA_ROPE = 32
MLA_DV = 64
MLA_W = MLA_HEADS * MLA_DV
Q_LORA = 256
KV_LORA = 128
MLA_THETA = 10000.0
N_EXPERTS = 32
TOP_K = 4
D_FF_EXPERT = D_MODEL
SWIGLU_LIMIT = 7.0
SWIGLU_ALPHA = 1.702
MOE_BLK = 256

RET_SPLITS = (RET_HEADS * RET_DK, RET_HEADS * RET_DK, RET_W, RET_W)
DIL_SPLITS = (DIL_W, DIL_W, DIL_W)
RWKV_SPLITS = (RWKV_W, RWKV_W, RWKV_W, DECAY_LORA, AAA_LORA, GATE_LORA)
MLA_SPLITS = (Q_LORA, KV_LORA, MLA_ROPE)
A_END = sum(RET_SPLITS)
B_END = A_END + sum(DIL_SPLITS)
C_END = B_END + sum(RWKV_SPLITS)
N_IN = C_END + sum(MLA_SPLITS)
RWKV_SHIFT_W = sum(RWKV_SPLITS)

kernel_name = "hybrid_parallel_heads_moe_trunk"

F32 = jnp.float32


def split_cols(p, sizes):
    idx = np.cumsum(sizes)[:-1].tolist()
    return jnp.split(p, idx, axis=-1)


def layer_norm(x, g, b, eps=LN_EPS):
    xf = x.astype(F32)
    mu = jnp.mean(xf, -1, keepdims=True)
    var = jnp.mean(jnp.square(xf - mu), -1, keepdims=True)
    return ((xf - mu) * lax.rsqrt(var + eps) * g.astype(F32) + b.astype(F32)).astype(x.dtype)


def rms_norm(x, g, eps=NORM_EPS):
    xf = x.astype(F32)
    return (xf * lax.rsqrt(jnp.mean(xf * xf, -1, keepdims=True) + eps) * g.astype(F32)).astype(x.dtype)


def rope_table(n_pos, rot_dim, theta):
    inv_freq = 1.0 / (theta ** (jnp.arange(0, rot_dim, 2, dtype=F32) / rot_dim))
    ang = jnp.arange(n_pos, dtype=F32)[:, None] * inv_freq[None, :]
    return jnp.cos(ang), jnp.sin(ang)


def apply_rope(x, cs):
    cos = cs[0][None, :, None, :]
    sin = cs[1][None, :, None, :]
    x1, x2 = jnp.split(x.astype(F32), 2, axis=-1)
    return jnp.concatenate([x1 * cos - x2 * sin, x1 * sin + x2 * cos], -1).astype(x.dtype)


def apply_partial_rope(x, cs, rot_dim):
    return jnp.concatenate([apply_rope(x[..., :rot_dim], cs), x[..., rot_dim:]], -1)


def retention_mixer(q, k, v, g, norm_g, cs):
    B_, S_, _ = q.shape
    n_ch = S_ // BLK
    q = apply_rope(q.reshape(B_, S_, RET_HEADS, RET_DK), cs)
    k = apply_rope(k.reshape(B_, S_, RET_HEADS, RET_DK), cs) * (RET_DK ** -0.5)
    v = v.reshape(B_, S_, RET_HEADS, RET_DV)
    log_gamma = jnp.log(1.0 - 2.0 ** (-5.0 - jnp.arange(RET_HEADS, dtype=F32)))
    idx = jnp.arange(BLK, dtype=F32)
    dist = idx[:, None] - idx[None, :]
    inner_decay = jnp.where(dist >= 0, jnp.exp(jnp.maximum(dist, 0.0)[None] * log_gamma[:, None, None]), 0.0)
    qc = q.reshape(B_, n_ch, BLK, RET_HEADS, RET_DK)
    kc = k.reshape(B_, n_ch, BLK, RET_HEADS, RET_DK)
    vc = v.reshape(B_, n_ch, BLK, RET_HEADS, RET_DV)
    scores = jnp.einsum('bnihd,bnjhd->bnhij', qc, kc) * inner_decay
    o_inner = jnp.einsum('bnhij,bnjhe->bnihe', scores, vc)
    zeta = jnp.exp((BLK - 1 - idx)[None, :] * log_gamma[:, None])
    kv_chunk = jnp.einsum('bnjhd,hj,bnjhe->nbhde', kc, zeta, vc)
    chunk_decay = jnp.exp(BLK * log_gamma)[None, :, None, None]

    def step(state, kv):
        return chunk_decay * state + kv, state

    _, states = lax.scan(step, jnp.zeros_like(kv_chunk[0]), kv_chunk)
    xi = jnp.exp((idx + 1.0)[None, :] * log_gamma[:, None])
    o_cross = jnp.einsum('bnihd,nbhde,hi->bnihe', qc, states, xi)
    o = (o_inner + o_cross).reshape(B_, S_, RET_HEADS, RET_DV)
    o = rms_norm(o, norm_g.reshape(RET_HEADS, RET_DV))
    return (jax.nn.silu(g.astype(F32)) * o.reshape(B_, S_, RET_W)).astype(q.dtype)


def banded_block_attention(q, k, v, n_back):
    G, L, H, dh = q.shape
    nb = L // BLK
    qb = q.reshape(G, nb, BLK, H, dh)

    def with_prev(t):
        t = t.reshape(G, nb, BLK, H, dh)
        prev = jnp.concatenate([jnp.zeros_like(t[:, :1]), t[:, :-1]], axis=1)
        return jnp.concatenate([prev, t], axis=2)

    kk = with_prev(k)
    vv = with_prev(v)
    s = jnp.einsum('gnihd,gnjhd->gnhij', qb, kk).astype(F32) * (dh ** -0.5)
    qpos = BLK + jnp.arange(BLK)
    kpos = jnp.arange(2 * BLK)
    dist = qpos[:, None] - kpos[None, :]
    band = (dist >= 0) & (dist <= n_back)
    first = band & (kpos[None, :] >= BLK)
    mask = jnp.where((jnp.arange(nb) == 0)[:, None, None], first[None], band[None])
    s = jnp.where(mask[None, :, None], s, -jnp.inf)
    lse = jax.nn.logsumexp(s, axis=-1)
    p = jnp.exp(s - lse[..., None])
    o = jnp.einsum('gnhij,gnjhd->gnihd', p.astype(v.dtype), vv)
    return o.reshape(G, L, H, dh), jnp.swapaxes(lse, 2, 3).reshape(G, L, H)


def dilated_mixer(q, k, v, norm_g, cs):
    B_, S_, _ = q.shape
    shp = (B_, S_, DIL_HEADS, DIL_DH)
    q = apply_partial_rope(q.reshape(shp), cs, ROPE_ROT_DIM)
    k = apply_partial_rope(k.reshape(shp), cs, ROPE_ROT_DIM)
    v = v.reshape(shp)
    outs, lses = [], []
    for window, dil in DIL_PATTERNS:
        span = dil * BLK
        s_pad = -(-S_ // span) * span
        m = s_pad // dil

        def to_residues(t):
            t = jnp.pad(t, ((0, 0), (0, s_pad - S_), (0, 0), (0, 0)))
            return t.reshape(B_, m, dil, DIL_HEADS, DIL_DH).transpose(0, 2, 1, 3, 4).reshape(B_ * dil, m, DIL_HEADS, DIL_DH)

        o, lse = banded_block_attention(to_residues(q), to_residues(k), to_residues(v), window // dil)
        o = o.reshape(B_, dil, m, DIL_HEADS, DIL_DH).transpose(0, 2, 1, 3, 4).reshape(B_, s_pad, DIL_HEADS, DIL_DH)[:, :S_]
        lse = lse.reshape(B_, dil, m, DIL_HEADS).transpose(0, 2, 1, 3).reshape(B_, s_pad, DIL_HEADS)[:, :S_]
        outs.append(o)
        lses.append(lse)
    wts = jax.nn.softmax(jnp.stack(lses), axis=0)
    o = jnp.einsum('pbsh,pbshd->bshd', wts, jnp.stack(outs).astype(F32))
    return rms_norm(o.reshape(B_, S_, DIL_W), norm_g).astype(q.dtype)


def token_shift(p, mu):
    prev = jnp.pad(p, ((0, 0), (1, 0), (0, 0)))[:, :-1]
    return p + (prev - p) * mu


def l2_normalize(x, eps=1e-12):
    xf = x.astype(F32)
    return xf / jnp.maximum(jnp.sqrt(jnp.sum(xf * xf, -1, keepdims=True)), eps)


def rwkv7_scan(r, w, k, v, a, b):
    B_, S_, H, N = r.shape

    def step(state, inp):
        r_t, w_t, k_t, v_t, a_t, b_t = inp
        sa = jnp.einsum('bhij,bhj->bhi', state, a_t)
        state = state * w_t[:, :, None, :] + sa[..., None] * b_t[:, :, None, :] + v_t[..., None] * k_t[:, :, None, :]
        return state, jnp.einsum('bhij,bhj->bhi', state, r_t)

    xs = (jnp.moveaxis(r, 1, 0), jnp.moveaxis(w, 1, 0), jnp.moveaxis(k, 1, 0),
          jnp.moveaxis(v, 1, 0), jnp.moveaxis(a, 1, 0), jnp.moveaxis(b, 1, 0))
    _, y = lax.scan(step, jnp.zeros((B_, H, N, N), F32), xs)
    return jnp.moveaxis(y, 0, 1)


def rwkv7_mixer(r, k, v, wd, ad, gd, w0, w_up, a0, a_up, g_up, k_k, k_a, r_k, ln_g, ln_b):
    B_, S_, _ = r.shape
    hs = (B_, S_, RWKV_HEADS, RWKV_DH)
    w = -jax.nn.softplus(-(w0 + jnp.tanh(wd) @ w_up)) - 0.5
    decay = jnp.exp(-jnp.exp(w.astype(F32)))
    a = jax.nn.sigmoid((a0 + ad @ a_up).astype(F32))
    g = (jax.nn.sigmoid(gd) @ g_up).astype(F32)
    kk = l2_normalize((k * k_k).reshape(hs))
    k = k.astype(F32) * (1.0 + (a - 1.0) * k_a.astype(F32))
    rf = r.astype(F32).reshape(hs)
    kf = k.reshape(hs)
    vf = v.astype(F32).reshape(hs)
    y = rwkv7_scan(rf, decay.reshape(hs), kf, vf, -kk, kk * a.reshape(hs))
    y = layer_norm(y, ln_g.reshape(RWKV_HEADS, RWKV_DH), ln_b.reshape(RWKV_HEADS, RWKV_DH), RWKV_GN_EPS)
    y = y + jnp.sum(rf * kf * r_k.astype(F32), -1, keepdims=True) * vf
    return (y.reshape(B_, S_, RWKV_W) * g).astype(r.dtype)


def causal_block_attention(q, k, v, scale):
    B_, S_, H, dq = q.shape
    nb = S_ // BLK
    qb = jnp.moveaxis(q.reshape(B_, nb, BLK, H, dq), 1, 0)
    kpos = jnp.arange(S_)

    def one_block(args):
        q_blk, blk = args
        s = jnp.einsum('bqhd,bkhd->bhqk', q_blk, k).astype(F32) * scale
        qpos = blk * BLK + jnp.arange(BLK)
        s = jnp.where(kpos[None, :] <= qpos[:, None], s, -jnp.inf)
        p = jax.nn.softmax(s, axis=-1)
        return jnp.einsum('bhqk,bkhd->bqhd', p.astype(v.dtype), v)

    o = lax.map(one_block, (qb, jnp.arange(nb)))
    return jnp.moveaxis(o, 0, 1).reshape(B_, S_, H, v.shape[-1])


def mla_mixer(c_q, c_kv, k_rope, q_norm_g, w_q_up, kv_norm_g, w_kv_up, out_norm_g, cs):
    B_, S_, _ = c_q.shape
    q = (rms_norm(c_q, q_norm_g) @ w_q_up).reshape(B_, S_, MLA_HEADS, MLA_NOPE + MLA_ROPE)
    kv = (rms_norm(c_kv, kv_norm_g) @ w_kv_up).reshape(B_, S_, MLA_HEADS, MLA_NOPE + MLA_DV)
    q = jnp.concatenate([q[..., :MLA_NOPE], apply_rope(q[..., MLA_NOPE:], cs)], -1)
    k_pe = apply_rope(k_rope[:, :, None, :], cs)
    k = jnp.concatenate([kv[..., :MLA_NOPE], jnp.broadcast_to(k_pe, (B_, S_, MLA_HEADS, MLA_ROPE))], -1)
    v = kv[..., MLA_NOPE:]
    o = causal_block_attention(q, k, v, (MLA_NOPE + MLA_ROPE) ** -0.5)
    return rms_norm(o.reshape(B_, S_, MLA_W), out_norm_g)


def moe_ffn(h, w_router, b_router, w_gu, b_gu, w_dn, b_dn):
    B_, S_, D = h.shape
    n_tok = B_ * S_
    t = h.reshape(n_tok, D)
    logits = (t @ w_router + b_router).astype(F32)
    top_val, top_idx = lax.top_k(logits, TOP_K)
    gate = jax.nn.softmax(top_val, axis=-1)
    n_assign = n_tok * TOP_K
    e_flat = top_idx.reshape(-1).astype(jnp.int32)
    tok_flat = jnp.repeat(jnp.arange(n_tok, dtype=jnp.int32), TOP_K)
    g_flat = gate.reshape(-1)
    order = jnp.argsort(e_flat)
    e_sorted = e_flat[order]
    counts = jnp.bincount(e_flat, length=N_EXPERTS).astype(jnp.int32)
    padded = (counts + MOE_BLK - 1) // MOE_BLK * MOE_BLK
    start = jnp.cumsum(counts) - counts
    pend = jnp.cumsum(padded)
    pstart = pend - padded
    dest = pstart[e_sorted] + (jnp.arange(n_assign, dtype=jnp.int32) - start[e_sorted])
    n_rows = (n_assign + N_EXPERTS * (MOE_BLK - 1) + MOE_BLK - 1) // MOE_BLK * MOE_BLK
    n_blocks = n_rows // MOE_BLK
    row_tok = jnp.full((n_rows,), n_tok, jnp.int32).at[dest].set(tok_flat[order])
    row_gate = jnp.zeros((n_rows,), F32).at[dest].set(g_flat[order])
    block_e = jnp.minimum(jnp.searchsorted(pend, jnp.arange(n_blocks, dtype=jnp.int32) * MOE_BLK, side='right'), N_EXPERTS - 1)
    t_pad = jnp.concatenate([t, jnp.zeros((1, D), t.dtype)], axis=0)

    def expert_block(args):
        toks, e = args
        xb = t_pad[toks]
        gu = xb @ w_gu[e] + b_gu[e]
        x_glu = jnp.minimum(gu[:, 0::2], SWIGLU_LIMIT)
        x_lin = jnp.clip(gu[:, 1::2], -SWIGLU_LIMIT, SWIGLU_LIMIT)
        act = x_glu * jax.nn.sigmoid(SWIGLU_ALPHA * x_glu) * (x_lin + 1.0)
        return act @ w_dn[e] + b_dn[e]

    yb = lax.map(expert_block, (row_tok.reshape(n_blocks, MOE_BLK), block_e))
    y = jax.ops.segment_sum(yb.reshape(n_rows, D).astype(F32) * row_gate[:, None], row_tok, num_segments=n_tok + 1)[:n_tok]
    return y.reshape(B_, S_, D).astype(h.dtype)


def setup_inputs(seed: int = 0) -> dict:
    key = jax.random.key(seed)
    keys = jax.random.split(key, 40)
    it = iter(range(40))

    def nk():
        return keys[next(it)]

    def nrm(shape, scale):
        return jax.random.normal(nk(), shape, jnp.float32) * scale

    def unif(shape, lo, hi):
        return jax.random.uniform(nk(), shape, jnp.float32, lo, hi)

    def gain(shape):
        return 1.0 + nrm(shape, 0.02)

    L = DEPTH
    Lv = DEPTH - 1
    return {
        "x": nrm((BATCH, SEQ, D_MODEL), 1.0),
        "w_in": nrm((L, D_MODEL, N_IN), D_MODEL ** -0.5),
        "w_out": nrm((L, D_MIX, D_MODEL), D_MIX ** -0.5 * DEEPNORM_BETA),
        "ret_norm_g": gain((L, RET_W)),
        "dil_norm_g": gain((L, DIL_W)),
        "rwkv_mu": unif((L, RWKV_SHIFT_W), 0.0, 1.0),
        "rwkv_w0": unif((L, RWKV_W), -6.0, -0.5),
        "rwkv_w_up": nrm((L, DECAY_LORA, RWKV_W), 0.1),
        "rwkv_a0": nrm((L, RWKV_W), 0.2),
        "rwkv_a_up": nrm((L, AAA_LORA, RWKV_W), 0.5 * AAA_LORA ** -0.5),
        "rwkv_g_up": nrm((L, GATE_LORA, RWKV_W), GATE_LORA ** -0.5),
        "rwkv_k_k": 0.85 + nrm((L, RWKV_W), 0.02),
        "rwkv_k_a": gain((L, RWKV_W)),
        "rwkv_r_k": nrm((L, RWKV_HEADS, RWKV_DH), 0.1),
        "rwkv_ln_g": gain((L, RWKV_W)),
        "rwkv_ln_b": nrm((L, RWKV_W), 0.02),
        "rwkv_vres_down": nrm((Lv, D_MODEL, MV_LORA), D_MODEL ** -0.5),
        "rwkv_vres_mu": unif((Lv, MV_LORA), 0.0, 1.0),
        "rwkv_v0": 0.5 + nrm((Lv, RWKV_W), 0.1),
        "rwkv_v_up": nrm((Lv, MV_LORA, RWKV_W), 0.5 * MV_LORA ** -0.5),
        "mla_q_norm_g": gain((L, Q_LORA)),
        "mla_w_q_up": nrm((L, Q_LORA, MLA_HEADS * (MLA_NOPE + MLA_ROPE)), Q_LORA ** -0.5),
        "mla_kv_norm_g": gain((L, KV_LORA)),
        "mla_w_kv_up": nrm((L, KV_LORA, MLA_HEADS * (MLA_NOPE + MLA_DV)), KV_LORA ** -0.5),
        "mla_out_norm_g": gain((L, MLA_W)),
        "ln1_g": gain((L, D_MODEL)),
        "ln1_b": nrm((L, D_MODEL), 0.02),
        "router_w": nrm((L, D_MODEL, N_EXPERTS), D_MODEL ** -0.5),
        "router_b": nrm((L, N_EXPERTS), 0.01),
        "exp_w_gu": nrm((L, N_EXPERTS, D_MODEL, 2 * D_FF_EXPERT), D_MODEL ** -0.5),
        "exp_b_gu": nrm((L, N_EXPERTS, 2 * D_FF_EXPERT), 0.02),
        "exp_w_dn": nrm((L, N_EXPERTS, D_FF_EXPERT, D_MODEL), D_FF_EXPERT ** -0.5 * DEEPNORM_BETA),
        "exp_b_dn": nrm((L, N_EXPERTS, D_MODEL), 0.02),
        "ln2_g": gain((L, D_MODEL)),
        "ln2_b": nrm((L, D_MODEL), 0.02),
    }


def reference(x, w_in, w_out, ret_norm_g, dil_norm_g, rwkv_mu, rwkv_w0, rwkv_w_up, rwkv_a0, rwkv_a_up,
              rwkv_g_up, rwkv_k_k, rwkv_k_a, rwkv_r_k, rwkv_ln_g, rwkv_ln_b, rwkv_vres_down, rwkv_vres_mu,
              rwkv_v0, rwkv_v_up, mla_q_norm_g, mla_w_q_up, mla_kv_norm_g, mla_w_kv_up, mla_out_norm_g,
              ln1_g, ln1_b, router_w, router_b, exp_w_gu, exp_b_gu, exp_w_dn, exp_b_dn, ln2_g, ln2_b):
    S_ = x.shape[1]
    ret_cs = rope_table(S_, RET_DK, RET_THETA)
    dil_cs = rope_table(S_, ROPE_ROT_DIM, ROPE_THETA)
    mla_cs = rope_table(S_, MLA_ROPE, MLA_THETA)
    v_first = None
    for l in range(DEPTH):
        if l == 0:
            p = x @ w_in[l]
        else:
            p = x @ jnp.concatenate([w_in[l], rwkv_vres_down[l - 1]], axis=1)

        a_q, a_k, a_v, a_g = split_cols(p[..., :A_END], RET_SPLITS)
        o_a = retention_mixer(a_q, a_k, a_v, a_g, ret_norm_g[l], ret_cs)

        b_q, b_k, b_v = split_cols(p[..., A_END:B_END], DIL_SPLITS)
        o_b = dilated_mixer(b_q, b_k, b_v, dil_norm_g[l], dil_cs)

        c_r, c_k, c_v, c_wd, c_ad, c_gd = split_cols(token_shift(p[..., B_END:C_END], rwkv_mu[l]), RWKV_SPLITS)
        if l == 0:
            v_first = c_v
        else:
            vd = token_shift(p[..., N_IN:], rwkv_vres_mu[l - 1])
            c_v = c_v + (v_first - c_v) * jax.nn.sigmoid(rwkv_v0[l - 1] + vd @ rwkv_v_up[l - 1])
        o_c = rwkv7_mixer(c_r, c_k, c_v, c_wd, c_ad, c_gd, rwkv_w0[l], rwkv_w_up[l], rwkv_a0[l], rwkv_a_up[l],
                          rwkv_g_up[l], rwkv_k_k[l], rwkv_k_a[l], rwkv_r_k[l], rwkv_ln_g[l], rwkv_ln_b[l])

        d_cq, d_ckv, d_kr = split_cols(p[..., C_END:N_IN], MLA_SPLITS)
        o_d = mla_mixer(d_cq, d_ckv, d_kr, mla_q_norm_g[l], mla_w_q_up[l], mla_kv_norm_g[l], mla_w_kv_up[l],
                        mla_out_norm_g[l], mla_cs)

        mix = jnp.concatenate([o_a, o_b, o_c, o_d], axis=-1).astype(x.dtype) @ w_out[l]
        x = layer_norm(DEEPNORM_ALPHA * x + mix, ln1_g[l], ln1_b[l])
        ffn = moe_ffn(x, router_w[l], router_b[l], exp_w_gu[l], exp_b_gu[l], exp_w_dn[l], exp_b_dn[l])
        x = layer_norm(DEEPNORM_ALPHA * x + ffn, ln2_g[l], ln2_b[l])
    return x
```

```python
import math
import numpy as np
from contextlib import ExitStack
import concourse.bass as bass
import concourse.mybir as mybir
from concourse.bass_utils import run_bass_kernel_spmd
from concourse.alu_op_type import AluOpType as ALU

F32 = mybir.dt.float32
BF16 = mybir.dt.bfloat16
AF = mybir.ActivationFunctionType
AX = mybir.AxisListType

D = 1024
SEQ = 8192
DEPTH = 4
NE = 32
ALPHA = (2 * DEPTH) ** 0.25
LN_EPS = 1e-5
NT = SEQ // 128
N_IN = 2976


class Sched:
    ENG = ("pe", "dve", "act", "pool", "sp")
    SEM_MAX = 30000
    NDMA = 32

    def __init__(self, nc, stack):
        self.nc = nc
        self.stack = stack
        self.lists = {e: [] for e in self.ENG}
        self.cur_sem = {}
        self.cnt = {}
        self.nsem = 0
        for e in self.ENG:
            if e != "sp":
                self._new_eng_sem(e)
        self.dma_sems = [self._alloc_sem(f"dma{i}") for i in range(self.NDMA)]
        self.dma_uses = [0] * self.NDMA
        self.dma_rr = 0
        self.seen = {e: {} for e in self.ENG}
        self.res = {}
        self.n_instr = 0

    def _alloc_sem(self, name):
        self.nsem += 1
        return self.stack.enter_context(self.nc.semaphore(name))

    def _new_eng_sem(self, e):
        self.cur_sem[e] = self._alloc_sem(f"c_{e}_{self.nsem}")
        self.cnt[e] = 0

    def _waits_for(self, e, reads, writes):
        need = {}

        def add(ev):
            if ev is None:
                return
            s, v = ev
            k = id(s)
            if self.seen[e].get(k, 0) >= v:
                return
            if k not in need or need[k][1] < v:
                need[k] = (s, v)
        for r in reads:
            st = self.res.get(r)
            if st:
                add(st["w"])
        for w in writes:
            st = self.res.get(w)
            if st:
                add(st["w"])
                for ev in st["r"]:
                    add(ev)
        out = list(need.values())
        for s, v in out:
            self.seen[e][id(s)] = v
        return out

    def _commit(self, ev, reads, writes):
        for r in reads:
            st = self.res.setdefault(r, {"w": None, "r": []})
            st["r"].append(ev)
            if len(st["r"]) > 16:
                best = {}
                for s, v in st["r"]:
                    if id(s) not in best or best[id(s)][1] < v:
                        best[id(s)] = (s, v)
                st["r"] = list(best.values())
        for w in writes:
            self.res[w] = {"w": ev, "r": []}

    def op(self, e, fn, reads=(), writes=()):
        if self.cnt[e] >= self.SEM_MAX:
            self._new_eng_sem(e)
        waits = self._waits_for(e, reads, writes)
        self.cnt[e] += 1
        sem, val = self.cur_sem[e], self.cnt[e]
        self.lists[e].append((waits, fn, sem, 1))
        if e == "pe":
            self.seen[e][id(sem)] = val
        self._commit((sem, val), reads, writes)
        self.n_instr += 1

    def dma(self, e, fn, reads=(), writes=()):
        i = self.dma_rr
        self.dma_rr = (self.dma_rr + 1) % self.NDMA
        sem = self.dma_sems[i]
        waits = self._waits_for(e, reads, writes)
        prev = self.dma_uses[i] * 16
        if prev and self.seen[e].get(id(sem), 0) < prev:
            waits.append((sem, prev))
            self.seen[e][id(sem)] = prev
        self.dma_uses[i] += 1
        val = self.dma_uses[i] * 16
        self.lists[e].append((waits, fn, sem, 16))
        self._commit((sem, val), reads, writes)
        self.n_instr += 1

    def wait_all(self, e, keys):
        waits = self._waits_for(e, list(keys), [])
        self.lists[e].append((waits, None, None, 0))

    def barrier(self):
        evs = [(self.cur_sem[e], self.cnt[e]) for e in self.ENG if e != "sp" and self.cnt[e] > 0]
        evs += [(s, u * 16) for s, u in zip(self.dma_sems, self.dma_uses) if u > 0]
        for e in self.ENG:
            waits = []
            for s, v in evs:
                if self.seen[e].get(id(s), 0) < v:
                    waits.append((s, v))
                    self.seen[e][id(s)] = v
            self.lists[e].append((waits, None, None, 0))

    def emit(self):
        nc = self.nc
        with nc.Block() as block:
            def run(name):
                def body(eng):
                    for waits, fn, sem, inc in self.lists[name]:
                        for s, v in waits:
                            eng.wait_ge(s, v)
                        if fn is not None:
                            fn(eng).then_inc(sem, inc)
                return body
            block.sync(run("sp"))
            block.tensor(run("pe"))
            block.vector(run("dve"))
            block.scalar(run("act"))
            block.gpsimd(run("pool"))


class Prog:
    def __init__(self, cfg):
        self.cfg = cfg
        self.nc = bass.Bass("TRN2", target_bir_lowering=False)
        self.din = {}
        self.dout = {}
        self.wl = cfg.get("wl", lambda l: l)
        self.vl = cfg.get("vl", lambda l: l - 1)

    def inp(self, name, shape, dt=F32):
        t = self.nc.dram_tensor(name, list(shape), dt, kind="ExternalInput").ap()
        self.din[name] = t
        return t

    def outp(self, name, shape, dt=F32):
        t = self.nc.dram_tensor(name, list(shape), dt, kind="ExternalOutput").ap()
        self.dout[name] = t
        return t

    def scratch(self, name, shape, dt=F32):
        return self.nc.dram_tensor(name, list(shape), dt).ap()


def bcast_rows(ap_1d, n, parts=128):
    return ap_1d.rearrange("(o n) -> o n", o=1).broadcast_to([parts, n])


class Arena:
    def __init__(self, nc, stack, nbytes):
        self.nf = nbytes // 4
        self.t = stack.enter_context(nc.sbuf_tensor("arena", [128, self.nf], F32))
        self.off = 0

    def alloc(self, shape, dt=F32, parts=None):
        p = shape[0]
        n = 1
        for d in shape[1:]:
            n *= d
        esz = 2 if dt == BF16 else 4
        nf = (n * esz + 3) // 4
        nf = (nf + 15) // 16 * 16
        assert self.off + nf <= self.nf, f"arena overflow: need {nf * 4}B at {self.off * 4} of {self.nf * 4}"
        ap = self.t[0:p, self.off:self.off + nf]
        self.off += nf
        if dt == BF16:
            ap = ap.bitcast(BF16)
        ap = ap[:, 0:n]
        if len(shape) == 3:
            ap = ap.rearrange("p (a b) -> p a b", a=shape[1])
        elif len(shape) == 4:
            ap = ap.rearrange("p (a b c) -> p a b c", a=shape[1], b=shape[2])
        return ap

    def mark(self):
        return self.off

    def release(self, m):
        self.off = m


def phase_b(P, S, A, l, K, oT_d, xres_d, xnext_d, xTnext_d, h1_d, h1T_d, gT_d, ntiles=NT, npass_tiles=8, do_b2=True, nexp=NE):
    import os
    SQ = os.environ.get("STORE_Q", "sp")
    nc = P.nc
    W = P.din
    ident = K["ident"]
    ps = K["ps"]
    m0 = A.mark()

    def sb(name, shape, dt=F32):
        return A.alloc(list(shape), dt)

    def load_const(t, src, nm):
        S.dma("sp", lambda e: e.dma_start(out=t, in_=src), writes=[nm])

    bdn = sb("bdn", [NE, D]); load_const(bdn, W["exp_b_dn"][P.wl(l)], "bdn")
    bgu = sb("bgu", [128, NE, 8, 2])
    with nc.allow_non_contiguous_dma(reason="tiny bias gather"):
        for e_ in range(NE):
            S.dma("sp", lambda e, e_=e_: e.dma_start(out=bgu[:, e_, :, :], in_=W["exp_b_gu"][P.wl(l), e_].rearrange("(t p two) -> p t two", p=128, two=2)),
                  writes=[f"bgu{e_}"])
    gates = sb("gates", [128, ntiles, NE])
    stg = (list(K["wstg"]) if "wstg" in K else []) + [sb("stg_extra", [128, 2048]) for i in range(1 if "wstg" in K else 3)]
    stg_i = [0]

    def next_stg():
        i = stg_i[0] % 3
        stg_i[0] += 1
        return i

    def layer_norm_tile(r, g, b, out, tag, gname):
        stats = K["stats"]; mv = K["mv"]; sd = K["sd"]
        for hlf in range(2):
            S.op("dve", lambda e, hlf=hlf: e.bn_stats(out=stats[:, hlf, :], in_=r[:, hlf * 512:(hlf + 1) * 512]), reads=[tag + "r"], writes=["stats"])
        S.op("dve", lambda e: e.bn_aggr(out=mv, in_=stats.rearrange("p a b -> p (a b)")), reads=["stats"], writes=["mv"])
        S.op("act", lambda e: e.activation(out=sd[:, 0:1], in_=mv[:, 1:2], func=AF.Sqrt, bias=K["eps_ln"][:, 0:1], scale=1.0), reads=["mv", "kc"], writes=["sd"])
        S.op("dve", lambda e: e.reciprocal(out=sd[:, 1:2], in_=sd[:, 0:1]), reads=["sd"], writes=["sd"])
        S.op("dve", lambda e: e.tensor_scalar(out=r, in0=r, scalar1=mv[:, 0:1], scalar2=sd[:, 1:2], op0=ALU.subtract, op1=ALU.mult),
             reads=[tag + "r", "mv", "sd"], writes=[tag + "r"])
        S.op("pool", lambda e: e.tensor_tensor(out=r, in0=r, in1=g, op=ALU.mult), reads=[tag + "r", gname + "g"], writes=[tag + "r"])
        S.op("dve", lambda e: e.tensor_tensor(out=out, in0=r, in1=b, op=ALU.add), reads=[tag + "r", gname + "b"], writes=[tag + "o"])

    def transpose_to_T(src, tag_src, dst_bf, tag_bf, dst_f32=None, tag_f32=None):
        for hlf in range(2):
            pb = ps[6 + hlf]
            for c4 in range(4):
                c = hlf * 4 + c4
                S.op("pe", lambda e, c=c, c4=c4, pb=pb: e.transpose(out=pb[:, c4 * 128:(c4 + 1) * 128], in_=src[:, c * 128:(c + 1) * 128], identity=ident),
                     reads=[tag_src, "kc"], writes=[f"ps{6 + hlf}"])
            S.op("dve", lambda e, hlf=hlf, pb=pb: e.tensor_copy(out=dst_bf[:, hlf * 4:(hlf + 1) * 4, :], in_=pb[:].rearrange("p (c t) -> p c t", c=4)),
                 reads=[f"ps{6 + hlf}"], writes=[tag_bf])
            if dst_f32 is not None and os.environ.get("T_ACT", "1") == "1":
                S.op("dve", lambda e, hlf=hlf, pb=pb: e.tensor_copy(out=dst_f32[:, hlf * 4:(hlf + 1) * 4, :], in_=pb[:].rearrange("p (c t) -> p c t", c=4)),
                     reads=[f"ps{6 + hlf}"], writes=[tag_f32])

    m1 = A.mark()
    wout_bf = sb("wout_bf", [128, 8, D], BF16)
    g1 = sb("g1", [128, D]); b1 = sb("b1", [128, D])
    rw = sb("rw", [128, 8, NE]); rb = sb("rb", [128, NE])
    for c in range(4):
        i = next_stg()
        S.dma("sp", lambda e, i=i, c=c: e.dma_start(out=stg[i].rearrange("p (c d) -> p c d", c=2),
              in_=W["w_out"][P.wl(l), c * 256:(c + 1) * 256, :].rearrange("(c p) d -> p c d", p=128)), writes=[f"stg{i}"])
        S.op("pool", lambda e, i=i, c=c: e.tensor_copy(out=wout_bf[:, 2 * c:2 * c + 2, :], in_=stg[i].rearrange("p (c d) -> p c d", c=2)),
             reads=[f"stg{i}"], writes=["wout_bf"])
    load_const(g1, bcast_rows(W["ln1_g"][P.wl(l)], D), "ln1g")
    load_const(b1, bcast_rows(W["ln1_b"][P.wl(l)], D), "ln1b")
    load_const(rw, W["router_w"][P.wl(l)].rearrange("(c p) n -> p c n", p=128), "rw")
    load_const(rb, bcast_rows(W["router_b"][P.wl(l)], NE), "rb")
    oT_t = [sb("oT_t", [128, 8, 128], BF16) for i in range(2)]
    x_t = [sb("x_t", [128, D]) for i in range(2)]
    r_t = sb("r_t", [128, D])
    h1_t = [sb("h1_t", [128, D]) for i in range(2)]
    hT_bf = [sb("hT_bf", [128, 8, 128], BF16) for i in range(2)]
    hT_f = sb("hT_f", [128, 8, 128])
    lg = sb("lg", [128, NE]); ex = sb("ex", [128, NE]); mk = sb("mk", [128, NE])
    top8 = sb("top8", [128, 8]); sm = sb("sm", [128, 4])
    gT_t = [sb("gT_t", [NE, 128]) for i in range(2)]

    for n in range(ntiles):
        b = n % 2
        S.dma("sp", lambda e, n=n, b=b: e.dma_start(out=oT_t[b], in_=oT_d[:, n * 128:(n + 1) * 128].rearrange("(c p) t -> p c t", p=128)),
              reads=[f"oT_d{n}"], writes=[f"oT_t{b}"])
        S.dma("sp", lambda e, n=n, b=b: e.dma_start(out=x_t[b], in_=xres_d[n * 128:(n + 1) * 128, :]), reads=[f"xres_d{n}"], writes=[f"x_t{b}"])
        for db in range(2):
            for c in range(8):
                S.op("pe", lambda e, c=c, db=db, b=b: e.matmul(ps[db][:], lhsT=oT_t[b][:, c, :], rhs=wout_bf[:, c, db * 512:(db + 1) * 512], start=(c == 0), stop=(c == 7)),
                     reads=[f"oT_t{b}", "wout_bf"], writes=[f"ps{db}"])
            S.op("dve", lambda e, db=db, b=b: e.scalar_tensor_tensor(out=r_t[:, db * 512:(db + 1) * 512], in0=x_t[b][:, db * 512:(db + 1) * 512], scalar=ALPHA,
                                                                      in1=ps[db][:], op0=ALU.mult, op1=ALU.add),
                 reads=[f"x_t{b}", f"ps{db}"], writes=["B1r"])
        import os
        STOP = int(os.environ.get("B1_STOP", "99"))
        if STOP <= 1:
            S.dma(SQ, lambda e, n=n, b=b: e.dma_start(out=h1_d[n * 128:(n + 1) * 128, :], in_=r_t), reads=["B1r"], writes=[f"h1_d{n}"])
            continue
        layer_norm_tile(r_t, g1, b1, h1_t[b], "B1", "ln1")
        S.dma(SQ, lambda e, n=n, b=b: e.dma_start(out=h1_d[n * 128:(n + 1) * 128, :], in_=h1_t[b]), reads=["B1o"], writes=[f"h1_d{n}"])
        if STOP <= 2:
            continue
        transpose_to_T(h1_t[b], "B1o", hT_bf[b], f"hT_bf{b}", hT_f, "hT_f")
        if os.environ.get("T_STORE", "1") == "1":
            S.dma(SQ, lambda e, n=n, b=b: e.dma_start(out=h1T_d[:, n * 128:(n + 1) * 128].rearrange("(c p) t -> p c t", p=128), in_=hT_bf[b]),
                  reads=[f"hT_bf{b}"], writes=[f"h1T_d{n}"])
        if STOP <= 3:
            continue
        for c in range(8):
            S.op("pe", lambda e, c=c: e.matmul(ps[2][:, 0:NE], lhsT=hT_f[:, c, :], rhs=rw[:, c, :], start=(c == 0), stop=(c == 7)),
                 reads=["hT_f", "rw"], writes=["ps2"])
        S.op("dve", lambda e: e.tensor_tensor(out=lg, in0=ps[2][:, 0:NE], in1=rb, op=ALU.add), reads=["ps2", "rb"], writes=["lg"])
        S.op("dve", lambda e: e.max(out=top8, in_=lg), reads=["lg"], writes=["top8"])
        S.op("dve", lambda e: e.tensor_scalar(out=sm[:, 0:1], in0=top8[:, 0:1], scalar1=-1.0, scalar2=None, op0=ALU.mult), reads=["top8"], writes=["sm"])
        S.op("act", lambda e: e.activation(out=ex, in_=lg, func=AF.Exp, bias=sm[:, 0:1], scale=1.0), reads=["lg", "sm"], writes=["ex"])
        S.op("dve", lambda e: e.tensor_scalar(out=mk, in0=lg, scalar1=top8[:, 3:4], scalar2=None, op0=ALU.is_ge), reads=["lg", "top8"], writes=["mk"])
        S.op("dve", lambda e: e.tensor_tensor(out=ex, in0=ex, in1=mk, op=ALU.mult), reads=["ex", "mk"], writes=["ex"])
        S.op("dve", lambda e: e.tensor_reduce(out=sm[:, 1:2], in_=ex, axis=AX.X, op=ALU.add), reads=["ex"], writes=["sm"])
        S.op("dve", lambda e: e.reciprocal(out=sm[:, 2:3], in_=sm[:, 1:2]), reads=["sm"], writes=["sm"])
        S.op("dve", lambda e, n=n: e.tensor_scalar(out=gates[:, n, :], in0=ex, scalar1=sm[:, 2:3], scalar2=None, op0=ALU.mult), reads=["ex", "sm"], writes=["gates"])
        if STOP <= 4:
            continue
        S.op("pe", lambda e, n=n: e.transpose(out=ps[3][0:NE, 0:128], in_=gates[:, n, :], identity=ident), reads=["gates", "kc"], writes=["ps3"])
        S.op("act", lambda e, b=b: e.copy(out=gT_t[b], in_=ps[3][0:NE, 0:128]), reads=["ps3"], writes=[f"gT_t{b}"])
        S.dma(SQ, lambda e, n=n, b=b: e.dma_start(out=gT_d[:, n * 128:(n + 1) * 128], in_=gT_t[b]), reads=[f"gT_t{b}"], writes=[f"gT_d{n}"])

    S.barrier()
    A.release(m1)
    if not do_b2:
        A.release(m0)
        return
    PT = npass_tiles
    PW = PT * 128
    npass = ntiles // PT
    NU = 2
    g2 = sb("g2", [128, D]); b2 = sb("b2", [128, D])
    load_const(g2, bcast_rows(W["ln2_g"][P.wl(l)], D), "ln2g")
    load_const(b2, bcast_rows(W["ln2_b"][P.wl(l)], D), "ln2b")
    hT_p = sb("hT_p", [128, 8, PW], BF16)
    yacc = sb("yacc", [128, PT, D])
    gT_p = sb("gT_p", [NE, PW])
    wgu = [sb("wgu", [128, 8, 1024], BF16) for i in range(NU)]
    wdn = [sb("wdn", [128, 4, D], BF16) for i in range(NU)]
    actT = [sb("actT", [128, 4, 512], BF16) for i in range(2)]
    xg = [sb("xg", [128, 512]) for i in range(2)]
    sg = [sb("sg", [128, 512]) for i in range(2)]
    xl = [sb("xl", [128, 512]) for i in range(2)]
    h1r = sb("h1r", [128, D])
    r2 = sb("r2", [128, D])
    xn = sb("xn", [128, D])
    xnT = sb("xnT", [128, 8, 128], BF16)
    ui = 0
    gcnt = 0
    acnt = 0
    for p in range(npass):
        tiles = list(range(p * PT, (p + 1) * PT))
        S.dma("sp", lambda e, p=p: e.dma_start(out=hT_p, in_=h1T_d[:, p * PW:(p + 1) * PW].rearrange("(c p) t -> p c t", p=128)),
              reads=[f"h1T_d{n}" for n in tiles], writes=["hT_p"])
        S.dma("sp", lambda e, p=p: e.dma_start(out=gT_p, in_=gT_d[:, p * PW:(p + 1) * PW]), reads=[f"gT_d{n}" for n in tiles], writes=["gT_p"])
        for ex_ in range(nexp):
            for hf in range(2):
                u = ui % NU
                ui += 1
                for c2 in range(4):
                    i = next_stg()
                    S.dma("sp", lambda e, i=i, c2=c2, ex_=ex_, hf=hf: e.dma_start(out=stg[i].rearrange("p (c f) -> p c f", c=2),
                          in_=W["exp_w_gu"][P.wl(l), ex_, c2 * 256:(c2 + 1) * 256, hf * 1024:(hf + 1) * 1024].rearrange("(c p) f -> p c f", p=128)), writes=[f"stg{i}"])
                    S.op("pool", lambda e, i=i, c2=c2, u=u: e.tensor_copy(out=wgu[u][:, 2 * c2:2 * c2 + 2, :].rearrange("p c (two f) -> p c two f", two=2),
                                                                            in_=stg[i].rearrange("p (c f two) -> p c two f", c=2, two=2)),
                         reads=[f"stg{i}"], writes=[f"wgu{u}"])
                for c2 in range(2):
                    i = next_stg()
                    S.dma("sp", lambda e, i=i, c2=c2, ex_=ex_, hf=hf: e.dma_start(out=stg[i].rearrange("p (c d) -> p c d", c=2),
                          in_=W["exp_w_dn"][P.wl(l), ex_, hf * 512 + c2 * 256:hf * 512 + (c2 + 1) * 256, :].rearrange("(c p) d -> p c d", p=128)), writes=[f"stg{i}"])
                    S.op("pool", lambda e, i=i, c2=c2, u=u: e.tensor_copy(out=wdn[u][:, 2 * c2:2 * c2 + 2, :], in_=stg[i].rearrange("p (c d) -> p c d", c=2)),
                         reads=[f"stg{i}"], writes=[f"wdn{u}"])
                first = (ex_ == 0 and hf == 0)
                for tb in range(PW // 512):
                    ab = acnt % 2
                    acnt += 1
                    for f4 in range(4):
                        ft = hf * 4 + f4
                        gb = gcnt % 2
                        gcnt += 1
                        pg, pl = ps[gb * 2], ps[gb * 2 + 1]
                        for c in range(8):
                            S.op("pe", lambda e, c=c, f4=f4, u=u, tb=tb, pg=pg: e.matmul(pg[:], lhsT=wgu[u][:, c, f4 * 128:(f4 + 1) * 128], rhs=hT_p[:, c, tb * 512:(tb + 1) * 512],
                                                                                       start=(c == 0), stop=(c == 7)),
                                 reads=[f"wgu{u}", "hT_p"], writes=[f"ps{gb * 2}"])
                        for c in range(8):
                            S.op("pe", lambda e, c=c, f4=f4, u=u, tb=tb, pl=pl: e.matmul(pl[:], lhsT=wgu[u][:, c, 512 + f4 * 128:512 + (f4 + 1) * 128], rhs=hT_p[:, c, tb * 512:(tb + 1) * 512],
                                                                                       start=(c == 0), stop=(c == 7)),
                                 reads=[f"wgu{u}", "hT_p"], writes=[f"ps{gb * 2 + 1}"])
                        S.op("dve", lambda e, gb=gb, pg=pg, ex_=ex_, ft=ft: e.tensor_scalar(out=xg[gb], in0=pg[:], scalar1=bgu[:, ex_, ft, 0:1], scalar2=7.0, op0=ALU.add, op1=ALU.min),
                             reads=[f"ps{gb * 2}", f"bgu{ex_}"], writes=[f"xg{gb}"])
                        S.op("act", lambda e, gb=gb: e.activation(out=sg[gb], in_=xg[gb], func=AF.Sigmoid, scale=1.702), reads=[f"xg{gb}"], writes=[f"sg{gb}"])
                        S.op("dve", lambda e, gb=gb, pl=pl, ex_=ex_, ft=ft: e.tensor_scalar(out=xl[gb], in0=pl[:], scalar1=bgu[:, ex_, ft, 1:2], scalar2=7.0, op0=ALU.add, op1=ALU.min),
                             reads=[f"ps{gb * 2 + 1}", f"bgu{ex_}"], writes=[f"xl{gb}"])
                        S.op("pool", lambda e, gb=gb: e.tensor_scalar(out=xl[gb], in0=xl[gb], scalar1=-7.0, scalar2=1.0, op0=ALU.max, op1=ALU.add),
                             reads=[f"xl{gb}"], writes=[f"xl{gb}"])
                        S.op("pool", lambda e, gb=gb: e.tensor_tensor(out=xg[gb], in0=xg[gb], in1=xl[gb], op=ALU.mult), reads=[f"xg{gb}", f"xl{gb}"], writes=[f"xg{gb}"])
                        S.op("dve", lambda e, gb=gb, ab=ab, f4=f4: e.tensor_tensor(out=actT[ab][:, f4, :], in0=xg[gb], in1=sg[gb], op=ALU.mult),
                             reads=[f"xg{gb}", f"sg{gb}"], writes=[f"actT{ab}"])
                    for tt in range(4):
                        tl = tb * 4 + tt
                        tg = p * PT + tl
                        for db in range(2):
                            pd = ps[4 + db]
                            for f4 in range(4):
                                S.op("pe", lambda e, f4=f4, tt=tt, db=db, ab=ab, u=u, pd=pd: e.matmul(pd[:], lhsT=actT[ab][:, f4, tt * 128:(tt + 1) * 128], rhs=wdn[u][:, f4, db * 512:(db + 1) * 512],
                                                                                                    start=(f4 == 0), stop=(f4 == 3)),
                                     reads=[f"actT{ab}", f"wdn{u}"], writes=[f"ps{4 + db}"])
                            if first:
                                S.op("dve", lambda e, tl=tl, tg=tg, db=db, pd=pd, ex_=ex_: e.tensor_scalar(out=yacc[:, tl, db * 512:(db + 1) * 512], in0=pd[:], scalar1=gates[:, tg, ex_:ex_ + 1],
                                                                                                       scalar2=None, op0=ALU.mult),
                                     reads=[f"ps{4 + db}", "gates"], writes=[f"yacc{tl}"])
                            else:
                                S.op("dve", lambda e, tl=tl, tg=tg, db=db, pd=pd, ex_=ex_: e.scalar_tensor_tensor(out=yacc[:, tl, db * 512:(db + 1) * 512], in0=pd[:], scalar=gates[:, tg, ex_:ex_ + 1],
                                                                                                              in1=yacc[:, tl, db * 512:(db + 1) * 512], op0=ALU.mult, op1=ALU.add),
                                     reads=[f"ps{4 + db}", "gates", f"yacc{tl}"], writes=[f"yacc{tl}"])
        for tl in range(PT):
            tg = p * PT + tl
            S.dma("sp", lambda e, tg=tg: e.dma_start(out=h1r, in_=h1_d[tg * 128:(tg + 1) * 128, :]), reads=[f"h1_d{tg}"], writes=["h1r"])
            for db in range(2):
                S.op("pe", lambda e, tl=tl, db=db: e.matmul(ps[4 + db][:], lhsT=gT_p[:, tl * 128:(tl + 1) * 128], rhs=bdn[:, db * 512:(db + 1) * 512], start=True, stop=True),
                     reads=["gT_p", "bdn"], writes=[f"ps{4 + db}"])
                S.op("dve", lambda e, tl=tl, db=db: e.tensor_tensor(out=yacc[:, tl, db * 512:(db + 1) * 512], in0=yacc[:, tl, db * 512:(db + 1) * 512], in1=ps[4 + db][:], op=ALU.add),
                     reads=[f"ps{4 + db}", f"yacc{tl}"], writes=[f"yacc{tl}"])
            S.op("dve", lambda e, tl=tl: e.scalar_tensor_tensor(out=r2, in0=h1r, scalar=ALPHA, in1=yacc[:, tl, :], op0=ALU.mult, op1=ALU.add),
                 reads=["h1r", f"yacc{tl}"], writes=["B2r"])
            layer_norm_tile(r2, g2, b2, xn, "B2", "ln2")
            S.dma(SQ, lambda e, tg=tg: e.dma_start(out=xnext_d[tg * 128:(tg + 1) * 128, :], in_=xn), reads=["B2o"], writes=[f"xnext_d{tg}"])
            if xTnext_d is not None:
                transpose_to_T(xn, "B2o", xnT, "xnT")
                S.dma(SQ, lambda e, tg=tg: e.dma_start(out=xTnext_d[:, tg * 128:(tg + 1) * 128].rearrange("(c p) t -> p c t", p=128), in_=xnT),
                      reads=["xnT"], writes=[f"xTnext_d{tg}"])
    S.barrier()
    A.release(m0)


def make_consts(P, S, A, st):
    nc = P.nc
    K = {}
    K["ident"] = A.alloc([128, 128])
    K["eps_ln"] = A.alloc([128, 1])
    K["stats"] = A.alloc([128, 2, 6])
    K["mv"] = A.alloc([128, 2])
    K["sd"] = A.alloc([128, 2])
    K["ps"] = [st.enter_context(nc.psum_tensor(f"ps{i}", [128, 512], F32)) for i in range(8)]
    K["ps_bf"] = K["ps"][3][:, 256:512].bitcast(BF16)
    S.dma("sp", lambda e: e.dma_start(out=K["ident"], in_=P.din["c_ident"]), writes=["kc"])
    S.op("dve", lambda e: e.memset(K["eps_ln"], LN_EPS), writes=["kc2"])
    return K


def build_phase_b_test(ntiles, do_b2=True, nexp=NE):
    P = Prog({})
    for nm, shp in (("w_out", [DEPTH, D, D]), ("ln1_g", [DEPTH, D]), ("ln1_b", [DEPTH, D]), ("ln2_g", [DEPTH, D]), ("ln2_b", [DEPTH, D]),
                    ("router_w", [DEPTH, D, NE]), ("router_b", [DEPTH, NE]), ("exp_w_gu", [DEPTH if do_b2 else 1, nexp, D, 2 * D]), ("exp_b_gu", [DEPTH, NE, 2 * D]),
                    ("exp_w_dn", [DEPTH if do_b2 else 1, nexp, D, D]), ("exp_b_dn", [DEPTH, NE, D]), ("c_ident", [128, 128])):
        P.inp(nm, shp)
    T = ntiles * 128
    oT = P.inp("oT", [D, T], BF16)
    xres = P.inp("xres", [T, D])
    xnext = P.outp("xnext", [T, D])
    h1o = P.outp("h1o", [T, D])
    xTn = P.scratch("xTn", [D, T], BF16)
    h1T_d = P.scratch("h1T_d", [D, T], BF16)
    gT_d = P.scratch("gT_d", [NE, T])
    with ExitStack() as st:
        S = Sched(P.nc, st)
        A = Arena(P.nc, st, 211968)
        K = make_consts(P, S, A, st)
        phase_b(P, S, A, 0, K, oT, xres, xnext, xTn, h1o, h1T_d, gT_d, ntiles=ntiles, npass_tiles=min(8, ntiles), do_b2=do_b2, nexp=nexp)
        S.wait_all("sp", [f"xnext_d{n}" for n in range(ntiles)] + [f"h1_d{n}" for n in range(ntiles)] + [f"xTnext_d{n}" for n in range(ntiles)] + [f"gT_d{n}" for n in range(ntiles)] + [f"h1T_d{n}" for n in range(ntiles)])
        S.emit()
        print("instructions:", S.n_instr)
    return P
TB = 512
NB_FULL = SEQ // TB


def load_w_bf16(S, A, K, src_ap, ncols, name, kchunks=8):
    wb = A.alloc([128, kchunks, ncols], BF16)
    stg = K["wstg"]
    step = 2048 // ncols if ncols <= 2048 else 0
    assert ncols <= 2048
    per = max(1, min(kchunks, 2048 // ncols))
    c = 0
    while c < kchunks:
        n = min(per, kchunks - c)
        i = K["wstg_i"][0] % 2
        K["wstg_i"][0] += 1
        S.dma("sp", lambda e, i=i, c=c, n=n: e.dma_start(out=stg[i][:, 0:n * ncols].rearrange("p (c f) -> p c f", c=n),
              in_=src_ap[c * 128:(c + n) * 128, :].rearrange("(c p) f -> p c f", p=128)), writes=[f"wstg{i}"])
        S.op("pool", lambda e, i=i, c=c, n=n: e.tensor_copy(out=wb[:, c:c + n, :], in_=stg[i][:, 0:n * ncols].rearrange("p (c f) -> p c f", c=n)),
             reads=[f"wstg{i}"], writes=[name])
        c += n
    return wb


def proj_fm(S, ps_t, ps_name, M, wb, wname, col0, xT, xname, K8=8, start=True, stop=True, n=TB):
    for c in range(K8):
        S.op("pe", lambda e, c=c: e.matmul(ps_t[0:M, 0:n], lhsT=wb[:, c, col0:col0 + M], rhs=xT[:, c, 0:n], start=(start and c == 0), stop=(stop and c == K8 - 1)),
             reads=[wname, xname], writes=[ps_name])


def rstd_from_ss(S, K, ss_ps, ss_name, rows, n, scale, eps_name, out, out_name):
    S.op("act", lambda e: e.activation(out=out[0:rows, 0:n], in_=ss_ps[0:rows, 0:n], func=AF.Sqrt, bias=K[eps_name][0:rows, 0:1], scale=scale),
         reads=[ss_name, "kc2"], writes=[out_name])
    S.op("dve", lambda e: e.reciprocal(out=out[0:rows, 0:n], in_=out[0:rows, 0:n]), reads=[out_name], writes=[out_name])


def attention_block(S, K, qb, heads, qT, qname, kT, kname, Vp, vname, masks, mask_lo, kt_lo, Pbuf, oacc, oname, ps, ring=None):
    kt_hi = 4 * qb + 3
    cnt = K["attn_cnt"]
    for h in heads:
        po = ps[4 + (h % 2)]
        first = True
        for kt in range(kt_lo, kt_hi + 1):
            d = 4 * qb - kt
            ks = kt if ring is None else kt % ring
            pb = cnt[0] % 3
            cnt[0] += 1
            S.op("pe", lambda e, h=h, ks=ks, pb=pb: e.matmul(ps[pb][:, 0:TB], lhsT=kT[h][:, ks * 128:(ks + 1) * 128], rhs=qT[h][:, 0:TB], start=True, stop=True),
                 reads=[kname, f"{qname}{h}"], writes=[f"ps{pb}"])
            S.op("act", lambda e, pb=pb: e.activation(out=Pbuf[pb], in_=ps[pb][:, 0:TB], func=AF.Exp), reads=[f"ps{pb}"], writes=[f"P{pb}"])
            if d <= mask_lo:
                eng = "pool" if (cnt[0] % 2) else "dve"
                mi = d + 3
                S.op(eng, lambda e, pb=pb, mi=mi: e.tensor_tensor(out=Pbuf[pb], in0=Pbuf[pb], in1=masks[:, mi, :], op=ALU.mult), reads=[f"P{pb}", "masks"], writes=[f"P{pb}"])
            S.op("pe", lambda e, h=h, ks=ks, pb=pb, po=po, first=first, last=(kt == kt_hi): e.matmul(po[0:65, 0:TB], lhsT=Vp[h][:, ks, :], rhs=Pbuf[pb], start=first, stop=last),
                 reads=[vname, f"P{pb}"], writes=[f"ps{4 + (h % 2)}"])
            first = False
        pn = f"ps{4 + (h % 2)}"
        rs = K["rs_row"]
        S.op("dve", lambda e, po=po: e.reciprocal(out=rs[64:65, 0:TB], in_=po[64:65, 0:TB]), reads=[pn], writes=["rs_row"])
        S.op("pe", lambda e: e.matmul(ps[3][0:64, 0:TB], lhsT=K["ones_f"][64:65, 0:64], rhs=rs[64:65, 0:TB], start=True, stop=True), reads=["rs_row", "kc3"], writes=["ps3"])
        S.op("act", lambda e, h=h, po=po: e.activation(out=oacc[h], in_=po[0:64, 0:TB], func=AF.Identity), reads=[pn], writes=[f"{oname}{h}"])
        S.op("dve", lambda e, h=h: e.tensor_tensor(out=oacc[h], in0=oacc[h], in1=ps[3][0:64, 0:TB], op=ALU.mult), reads=[f"{oname}{h}", "ps3"], writes=[f"{oname}{h}"])


def rms_over_heads_store(S, K, A_, oacc, oname, gcol, gname, out_bf, oT_d, row0, t0, ps, sq, nheads=4, eps_name="eps_n6", tag="o"):
    for h in range(nheads):
        S.op("act", lambda e, h=h: e.activation(out=sq[h % 2], in_=oacc[h], func=AF.Square), reads=[f"{oname}{h}"], writes=[f"sq{h % 2}"])
        S.op("pe", lambda e, h=h: e.matmul(ps[3][0:64, 0:TB], lhsT=K["ones_f"][0:64, 0:64], rhs=sq[h % 2], start=(h == 0), stop=(h == nheads - 1)),
             reads=[f"sq{h % 2}", "kc3"], writes=["ps3"])
    rstd = K["rstd64"]
    rstd_from_ss(S, K, ps[3], "ps3", 64, TB, 1.0 / (64 * nheads), eps_name, rstd, "rstd64")
    for h in range(nheads):
        S.op("dve", lambda e, h=h: e.scalar_tensor_tensor(out=out_bf[h % 2], in0=oacc[h], scalar=gcol[:, h:h + 1], in1=rstd[0:64, 0:TB], op0=ALU.mult, op1=ALU.mult),
             reads=[f"{oname}{h}", "rstd64", gname], writes=[f"obf{h % 2}"])
        S.dma("sp", lambda e, h=h: e.dma_start(out=oT_d[row0 + h * 64:row0 + (h + 1) * 64, t0:t0 + TB], in_=out_bf[h % 2]), reads=[f"obf{h % 2}"], writes=[f"{tag}T_d{t0 // 128}"])


def mixer_dil(P, S, A, l, K, xT_d, oT_d, nblk=NB_FULL):
    W = P.din
    ps = K["ps"]
    m0 = A.mark()
    nt = nblk * 4
    wq = load_w_bf16(S, A, K, W["w_in"][P.wl(l)][:, 768:1024], 256, "dil_wq")
    wk = load_w_bf16(S, A, K, W["w_in"][P.wl(l)][:, 1024:1280], 256, "dil_wk")
    wv = load_w_bf16(S, A, K, W["w_in"][P.wl(l)][:, 1280:1536], 256, "dil_wv")
    wp = load_w_bf16(S, A, K, W["dil_wperm"][P.wl(l)], 128, "dil_wp")
    gcol = A.alloc([64, 4])
    with P.nc.allow_non_contiguous_dma(reason="tiny gain load"):
        S.dma("sp", lambda e: e.dma_start(out=gcol, in_=W["dil_norm_g"][P.wl(l)].rearrange("(h d) -> d h", d=64), allow_slow_non_contiguous=True), writes=["dil_g"])
    masks = A.alloc([128, 20, TB], BF16)
    S.dma("sp", lambda e: e.dma_start(out=masks, in_=W["c_mask_dil"].rearrange("m k q -> k m q")), writes=["masks"])
    RING = 20
    kT = [A.alloc([64, RING * 128], BF16) for h in range(4)]
    Vp = [A.alloc([128, RING, 65], BF16) for h in range(4)]
    for h in range(4):
        S.op("pool", lambda e, h=h: e.memset(Vp[h][:, :, 64:65], 1.0), writes=[f"dilVones{h}"])
    xT = [A.alloc([128, 8, TB], BF16) for i in range(2)]
    ctab = [A.alloc([16, TB]) for i in range(2)]
    stab = [A.alloc([16, TB]) for i in range(2)]
    qT = [A.alloc([64, TB], BF16) for h in range(4)]
    qf = A.alloc([64, TB]); pf = A.alloc([16, TB])
    Pbuf = [A.alloc([128, TB], BF16) for i in range(3)]
    oacc = [A.alloc([64, TB]) for h in range(4)]
    sq = [A.alloc([64, TB]) for i in range(2)]
    obf = [A.alloc([64, TB], BF16) for i in range(2)]
    for tb in range(nblk):
        b = tb % 2
        t0 = tb * TB
        S.dma("sp", lambda e, b=b, t0=t0: e.dma_start(out=xT[b], in_=xT_d[:, t0:t0 + TB].rearrange("(c p) t -> p c t", p=128)),
              reads=[f"xT_d{t0 // 128 + i}" for i in range(4)], writes=[f"xT{b}"])
        S.dma("sp", lambda e, b=b, t0=t0: e.dma_start(out=ctab[b], in_=W["c_rope_dil"][0, :, t0:t0 + TB]), writes=[f"ctab{b}"])
        S.dma("sp", lambda e, b=b, t0=t0: e.dma_start(out=stab[b], in_=W["c_rope_dil"][1, :, t0:t0 + TB]), writes=[f"stab{b}"])
        for h in range(4):
            for which in range(2):
                wmain, wname = (wq, "dil_wq") if which == 0 else (wk, "dil_wk")
                proj_fm(S, ps[6], "ps6", 64, wmain, wname, h * 64, xT[b], f"xT{b}")
                proj_fm(S, ps[7], "ps7", 16, wp, "dil_wp", which * 64 + h * 16, xT[b], f"xT{b}")
                S.op("act", lambda e: e.activation(out=qf, in_=ps[6][0:64, 0:TB], func=AF.Identity), reads=["ps6"], writes=["qf"])
                S.op("dve", lambda e, b=b: e.tensor_tensor(out=pf, in0=ps[7][0:16, 0:TB], in1=stab[b], op=ALU.mult), reads=["ps7", f"stab{b}"], writes=["pf"])
                S.op("dve", lambda e, b=b: e.tensor_tensor(out=qf[0:16, :], in0=qf[0:16, :], in1=ctab[b], op=ALU.mult), reads=["qf", f"ctab{b}"], writes=["qf"])
                S.op("dve", lambda e: e.tensor_tensor(out=qf[0:16, :], in0=qf[0:16, :], in1=pf, op=ALU.add), reads=["qf", "pf"], writes=["qf"])
                if which == 0:
                    S.op("dve", lambda e, h=h: e.tensor_scalar(out=qT[h], in0=qf, scalar1=0.125, scalar2=None, op0=ALU.mult), reads=["qf"], writes=[f"dq{h}"])
                else:
                    S.op("dve", lambda e, h=h, r0=((4 * tb) % RING) * 128: e.tensor_copy(out=kT[h][:, r0:r0 + TB], in_=qf), reads=["qf"], writes=["dil_kT"])
        for tt in range(4):
            for c in range(8):
                S.op("pe", lambda e, c=c, tt=tt, b=b: e.matmul(ps[6][:, 0:256], lhsT=xT[b][:, c, tt * 128:(tt + 1) * 128], rhs=wv[:, c, :], start=(c == 0), stop=(c == 7)),
                     reads=[f"xT{b}", "dil_wv"], writes=["ps6"])
            for h in range(4):
                S.op("dve", lambda e, h=h, sl_=(tb * 4 + tt) % RING: e.tensor_copy(out=Vp[h][:, sl_, 0:64], in_=ps[6][:, h * 64:(h + 1) * 64]), reads=["ps6", f"dilVones{h}"], writes=["dil_V"])
        attention_block(S, K, tb, range(4), qT, "dq", kT, "dil_kT", Vp, "dil_V", masks, 16, max(0, 4 * tb - 16), Pbuf, oacc, "dil_o", ps, ring=RING)
        rms_over_heads_store(S, K, A, oacc, "dil_o", gcol, "dil_g", obf, oT_d, 256, t0, ps, sq)
    S.barrier()
    A.release(m0)
def mixer_mla(P, S, A, l, K, xT_d, oT_d, nblk=NB_FULL):
    W = P.din
    ps = K["ps"]
    m0 = A.mark()
    nt = nblk * 4
    SC = 96 ** -0.5
    wcq = load_w_bf16(S, A, K, W["w_in"][P.wl(l)][:, 2560:2816], 256, "mla_wcq")
    wckv = load_w_bf16(S, A, K, W["w_in"][P.wl(l)][:, 2816:2944], 128, "mla_wckv")
    wkr = load_w_bf16(S, A, K, W["mla_wkr_pad"][P.wl(l)], 192, "mla_wkr")
    wqu = load_w_bf16(S, A, K, W["mla_w_q_up"][P.wl(l)], 384, "mla_wqu", kchunks=2)
    wqp = load_w_bf16(S, A, K, W["mla_wq_perm"][P.wl(l)], 384, "mla_wqp", kchunks=2)
    wkn = load_w_bf16(S, A, K, W["mla_wk_pad"][P.wl(l)], 384, "mla_wkn", kchunks=1)
    wv = load_w_bf16(S, A, K, W["mla_wv"][P.wl(l)], 256, "mla_wv", kchunks=1)
    gq = A.alloc([128, 2]); gkv = A.alloc([128, 1]); gcol = A.alloc([64, 4])
    with P.nc.allow_non_contiguous_dma(reason="tiny gain load"):
        S.dma("sp", lambda e: e.dma_start(out=gq, in_=W["mla_q_norm_g"][P.wl(l)].rearrange("(c p) -> p c", p=128), allow_slow_non_contiguous=True), writes=["mla_gq"])
        S.dma("sp", lambda e: e.dma_start(out=gkv, in_=W["mla_kv_norm_g"][P.wl(l)].rearrange("(c p) -> p c", p=128), allow_slow_non_contiguous=True), writes=["mla_gkv"])
        S.dma("sp", lambda e: e.dma_start(out=gcol, in_=W["mla_out_norm_g"][P.wl(l)].rearrange("(h d) -> d h", d=64), allow_slow_non_contiguous=True), writes=["mla_g"])
    masks = A.alloc([128, 4, TB], BF16)
    S.dma("sp", lambda e: e.dma_start(out=masks, in_=W["c_mask_mla"].rearrange("m k q -> k m q")), writes=["masks"])
    kT = [A.alloc([96, nblk * TB], BF16) for h in range(4)]
    Vp = [A.alloc([128, nt, 65], BF16) for h in range(4)]
    for h in range(4):
        S.op("pool", lambda e, h=h: e.memset(Vp[h][:, :, 64:65], 1.0), writes=[f"mlaVones{h}"])
    xT = [A.alloc([128, 8, TB], BF16) for i in range(2)]
    c96 = [A.alloc([96, TB])] * 2
    s96 = [A.alloc([96, TB])] * 2
    cq = A.alloc([128, 2, TB]); cqn = A.alloc([128, 2, TB], BF16)
    ckv = A.alloc([128, TB]); ckvn = A.alloc([128, TB], BF16)
    sqb = A.alloc([128, TB])
    rstd = K["rstd128"]
    qT = [A.alloc([96, TB], BF16) for h in range(4)]
    qf = A.alloc([96, TB]); pf = A.alloc([96, TB])
    Pbuf = [A.alloc([128, TB], BF16) for i in range(3)]
    oacc = [A.alloc([64, TB]) for h in range(4)]
    sq = [A.alloc([64, TB]) for i in range(2)]
    obf = [A.alloc([64, TB], BF16) for i in range(2)]
    for tb in range(nblk):
        b = tb % 2
        t0 = tb * TB
        S.dma("sp", lambda e, b=b, t0=t0: e.dma_start(out=xT[b], in_=xT_d[:, t0:t0 + TB].rearrange("(c p) t -> p c t", p=128)), writes=[f"xT{b}"])
        S.dma("sp", lambda e, b=b, t0=t0: e.dma_start(out=c96[b], in_=W["c_rope_mla"][0, :, t0:t0 + TB]), writes=["c96"])
        S.dma("sp", lambda e, b=b, t0=t0: e.dma_start(out=s96[b], in_=W["c_rope_mla"][1, :, t0:t0 + TB]), writes=["s96"])
        for kc in range(2):
            proj_fm(S, ps[6 + kc], f"ps{6 + kc}", 128, wcq, "mla_wcq", kc * 128, xT[b], f"xT{b}")
            S.op("act", lambda e, kc=kc: e.activation(out=cq[:, kc, :], in_=ps[6 + kc][:, 0:TB], func=AF.Identity), reads=[f"ps{6 + kc}"], writes=["cq"])
            S.op("dve", lambda e, kc=kc: e.tensor_tensor(out=sqb, in0=ps[6 + kc][:, 0:TB], in1=cq[:, kc, :], op=ALU.mult), reads=[f"ps{6 + kc}", "cq"], writes=["sqb"])
            S.op("pe", lambda e, kc=kc: e.matmul(ps[3][:, 0:TB], lhsT=K["ones_f"], rhs=sqb, start=(kc == 0), stop=(kc == 1)), reads=["sqb", "kc3"], writes=["ps3"])
        rstd_from_ss(S, K, ps[3], "ps3", 128, TB, 1.0 / 256, "eps_n6", rstd, "rstd128")
        for kc in range(2):
            S.op("dve", lambda e, kc=kc: e.scalar_tensor_tensor(out=cqn[:, kc, :], in0=cq[:, kc, :], scalar=gq[:, kc:kc + 1], in1=rstd[:, 0:TB], op0=ALU.mult, op1=ALU.mult),
                 reads=["cq", "rstd128", "mla_gq"], writes=["cqn"])
        proj_fm(S, ps[6], "ps6", 128, wckv, "mla_wckv", 0, xT[b], f"xT{b}")
        S.op("act", lambda e: e.activation(out=ckv, in_=ps[6][:, 0:TB], func=AF.Identity), reads=["ps6"], writes=["ckv"])
        S.op("dve", lambda e: e.tensor_tensor(out=sqb, in0=ps[6][:, 0:TB], in1=ckv, op=ALU.mult), reads=["ps6", "ckv"], writes=["sqb"])
        S.op("pe", lambda e: e.matmul(ps[3][:, 0:TB], lhsT=K["ones_f"], rhs=sqb, start=True, stop=True), reads=["sqb", "kc3"], writes=["ps3"])
        rstd_from_ss(S, K, ps[3], "ps3", 128, TB, 1.0 / 128, "eps_n6", rstd, "rstd128")
        S.op("dve", lambda e: e.scalar_tensor_tensor(out=ckvn, in0=ckv, scalar=gkv[:, 0:1], in1=rstd[:, 0:TB], op0=ALU.mult, op1=ALU.mult),
             reads=["ckv", "rstd128", "mla_gkv"], writes=["ckvn"])
        proj_fm(S, ps[7], "ps7", 96, wkr, "mla_wkr", 96, xT[b], f"xT{b}")
        S.op("dve", lambda e, b=b: e.tensor_tensor(out=pf, in0=ps[7][0:96, 0:TB], in1=s96[b], op=ALU.mult), reads=["ps7", "s96"], writes=["pf"])
        for h in range(4):
            proj_fm(S, ps[6], "ps6", 96, wkr, "mla_wkr", 0, xT[b], f"xT{b}", stop=False)
            S.op("pe", lambda e, h=h: e.matmul(ps[6][0:96, 0:TB], lhsT=wkn[:, 0, h * 96:(h + 1) * 96], rhs=ckvn, start=False, stop=True), reads=["mla_wkn", "ckvn"], writes=["ps6"])
            S.op("dve", lambda e, b=b: e.tensor_tensor(out=qf, in0=ps[6][0:96, 0:TB], in1=c96[b], op=ALU.mult), reads=["ps6", "c96"], writes=["qf"])
            S.op("dve", lambda e, h=h, t0=t0: e.tensor_tensor(out=kT[h][:, t0:t0 + TB], in0=qf, in1=pf, op=ALU.add), reads=["qf", "pf"], writes=["mla_kT"])
        for h in range(4):
            for kc in range(2):
                S.op("pe", lambda e, h=h, kc=kc: e.matmul(ps[6][0:96, 0:TB], lhsT=wqu[:, kc, h * 96:(h + 1) * 96], rhs=cqn[:, kc, :], start=(kc == 0), stop=(kc == 1)),
                     reads=["mla_wqu", "cqn"], writes=["ps6"])
            for kc in range(2):
                S.op("pe", lambda e, h=h, kc=kc: e.matmul(ps[7][0:96, 0:TB], lhsT=wqp[:, kc, h * 96:(h + 1) * 96], rhs=cqn[:, kc, :], start=(kc == 0), stop=(kc == 1)),
                     reads=["mla_wqp", "cqn"], writes=["ps7"])
            S.op("dve", lambda e, b=b: e.tensor_tensor(out=qf, in0=ps[6][0:96, 0:TB], in1=c96[b], op=ALU.mult), reads=["ps6", "c96"], writes=["qf"])
            S.op("dve", lambda e, b=b: e.scalar_tensor_tensor(out=K["qp96"], in0=ps[7][0:96, 0:TB], scalar=SC, in1=s96[b], op0=ALU.mult, op1=ALU.mult), reads=["ps7", "s96"], writes=["qp96"])
            S.op("dve", lambda e, h=h: e.scalar_tensor_tensor(out=qT[h], in0=qf, scalar=SC, in1=K["qp96"], op0=ALU.mult, op1=ALU.add), reads=["qf", "qp96"], writes=[f"mq{h}"])
        for tt in range(4):
            S.op("pe", lambda e, tt=tt: e.matmul(ps[6][:, 0:256], lhsT=ckvn[:, tt * 128:(tt + 1) * 128], rhs=wv[:, 0, :], start=True, stop=True), reads=["ckvn", "mla_wv"], writes=["ps6"])
            for h in range(4):
                S.op("dve", lambda e, h=h, tt=tt, tb=tb: e.tensor_copy(out=Vp[h][:, tb * 4 + tt, 0:64], in_=ps[6][:, h * 64:(h + 1) * 64]), reads=["ps6", f"mlaVones{h}"], writes=["mla_V"])
        attention_block(S, K, tb, range(4), qT, "mq", kT, "mla_kT", Vp, "mla_V", masks, 0, 0, Pbuf, oacc, "mla_o", ps)
        rms_over_heads_store(S, K, A, oacc, "mla_o", gcol, "mla_g", obf, oT_d, 768, t0, ps, sq)
    S.barrier()
    A.release(m0)


def make_consts_a(P, S, A, K):
    K["ones_f"] = A.alloc([128, 128])
    K["eps_n6"] = A.alloc([128, 1])
    K["rs_row"] = A.alloc([128, TB])
    K["rstd64"] = A.alloc([64, TB])
    K["rstd128"] = A.alloc([128, TB])
    K["qp96"] = A.alloc([96, TB])
    K["wstg"] = [A.alloc([128, 2048]) for i in range(2)]
    K["wstg_i"] = [0]
    K["attn_cnt"] = [0]
    S.op("dve", lambda e: e.memset(K["ones_f"], 1.0), writes=["kc3"])
    S.op("dve", lambda e: e.memset(K["eps_n6"], 1e-6), writes=["kc2"])
RET_GAM = [1.0 - 2.0 ** (-5.0 - h) for h in range(4)]


def mixer_ret(P, S, A, l, K, xT_d, oT_d, nblk=NB_FULL):
    W = P.din
    ps = K["ps"]
    m0 = A.mark()
    wq = load_w_bf16(S, A, K, W["w_in"][P.wl(l)][:, 0:128], 128, "ret_wq")
    wk = load_w_bf16(S, A, K, W["w_in"][P.wl(l)][:, 128:256], 128, "ret_wk")
    wv = load_w_bf16(S, A, K, W["w_in"][P.wl(l)][:, 256:512], 256, "ret_wv")
    wg = load_w_bf16(S, A, K, W["w_in"][P.wl(l)][:, 512:768], 256, "ret_wg")
    wp = load_w_bf16(S, A, K, W["ret_wperm"][P.wl(l)], 256, "ret_wp")
    gcol = A.alloc([64, 4])
    S.dma("sp", lambda e: e.dma_start(out=gcol, in_=W["ret_norm_g"][P.wl(l)].rearrange("(h d) -> d h", d=64), allow_slow_non_contiguous=True), writes=["ret_g"])
    DT = A.alloc([128, 4, 128])
    S.dma("sp", lambda e: e.dma_start(out=DT, in_=W["c_ret_decayT"].rearrange("h j i -> j h i")), writes=["ret_DT"])
    xi = [A.alloc([64, TB]) for p in range(2)]
    for p in range(2):
        S.dma("sp", lambda e, p=p: e.dma_start(out=xi[p], in_=W["c_ret_xi"][p * 64:(p + 1) * 64, :]), writes=[f"ret_xi{p}"])
    zmask = A.alloc([128, 4, 64], BF16)
    S.dma("sp", lambda e: e.dma_start(out=zmask, in_=W["c_ret_zmask"].rearrange("h j c -> j h c")), writes=["ret_zm"])
    identb = A.alloc([128, 128], BF16)
    S.op("dve", lambda e: e.tensor_copy(out=identb, in_=K["ident"]), reads=["kc"], writes=["identb"])
    Sall = [A.alloc([64, 64]) for p in range(2)]
    Sbf = [A.alloc([64, 64], BF16) for p in range(2)]
    for p in range(2):
        S.op("dve", lambda e, p=p: e.memset(Sall[p], 0.0), writes=[f"ret_S{p}"])
        S.op("dve", lambda e, p=p: e.memset(Sbf[p], 0.0), writes=[f"ret_Sbf{p}"])
    xT = [A.alloc([128, 8, TB], BF16) for i in range(2)]
    ctab = [A.alloc([64, TB]) for i in range(2)]
    stab = [A.alloc([64, TB]) for i in range(2)]
    qf = A.alloc([64, TB]); pf = A.alloc([64, TB])
    qT = [A.alloc([64, TB], BF16) for p in range(2)]
    qx = [A.alloc([64, TB], BF16) for p in range(2)]
    kTt = [A.alloc([64, TB], BF16) for p in range(2)]
    vbf = A.alloc([128, 4, 256], BF16)
    sg = A.alloc([64, TB]); gl = [A.alloc([64, TB]) for h in range(4)]
    kzp = A.alloc([128, 4, 4, 64], BF16)
    Pb = [A.alloc([128, 128], BF16) for i in range(2)]
    of = A.alloc([64, TB]); sq = A.alloc([64, TB]); obf = [A.alloc([64, TB], BF16) for i in range(2)]
    pst = K["ps_bf"]
    cnt = 0
    for tb in range(nblk):
        b = tb % 2
        t0 = tb * TB
        S.dma("sp", lambda e, b=b, t0=t0: e.dma_start(out=xT[b], in_=xT_d[:, t0:t0 + TB].rearrange("(c p) t -> p c t", p=128)), writes=[f"xT{b}"])
        S.dma("sp", lambda e, b=b, t0=t0: e.dma_start(out=ctab[b], in_=W["c_rope_ret2"][0, :, t0:t0 + TB]), writes=[f"ctab{b}"])
        S.dma("sp", lambda e, b=b, t0=t0: e.dma_start(out=stab[b], in_=W["c_rope_ret2"][1, :, t0:t0 + TB]), writes=[f"stab{b}"])
        for p in range(2):
            for which in range(2):
                wmain, wname = (wq, "ret_wq") if which == 0 else (wk, "ret_wk")
                proj_fm(S, ps[6], "ps6", 64, wmain, wname, p * 64, xT[b], f"xT{b}")
                proj_fm(S, ps[7], "ps7", 64, wp, "ret_wp", which * 128 + p * 64, xT[b], f"xT{b}")
                S.op("dve", lambda e, b=b: e.tensor_tensor(out=qf, in0=ps[6][0:64, 0:TB], in1=ctab[b], op=ALU.mult), reads=["ps6", f"ctab{b}"], writes=["qf"])
                S.op("dve", lambda e, b=b: e.tensor_tensor(out=pf, in0=ps[7][0:64, 0:TB], in1=stab[b], op=ALU.mult), reads=["ps7", f"stab{b}"], writes=["pf"])
                S.op("dve", lambda e: e.tensor_tensor(out=qf, in0=qf, in1=pf, op=ALU.add), reads=["qf", "pf"], writes=["qf"])
                if which == 0:
                    S.op("dve", lambda e, p=p: e.tensor_copy(out=qT[p], in_=qf), reads=["qf"], writes=[f"ret_qT{p}"])
                    S.op("pool", lambda e, p=p: e.tensor_tensor(out=qx[p], in0=qf, in1=xi[p], op=ALU.mult), reads=["qf", f"ret_xi{p}"], writes=[f"ret_qx{p}"])
                else:
                    S.op("dve", lambda e, p=p: e.tensor_scalar(out=kTt[p], in0=qf, scalar1=32 ** -0.5, scalar2=None, op0=ALU.mult), reads=["qf"], writes=[f"ret_kT{p}"])
        for tt in range(4):
            for c in range(8):
                S.op("pe", lambda e, c=c, tt=tt, b=b: e.matmul(ps[6][:, 0:256], lhsT=xT[b][:, c, tt * 128:(tt + 1) * 128], rhs=wv[:, c, :], start=(c == 0), stop=(c == 7)),
                     reads=[f"xT{b}", "ret_wv"], writes=["ps6"])
            S.op("dve", lambda e, tt=tt: e.tensor_copy(out=vbf[:, tt, :], in_=ps[6][:, 0:256]), reads=["ps6"], writes=["ret_v"])
            for p in range(2):
                S.op("pe", lambda e, tt=tt, p=p: e.transpose(out=pst[:, 0:64], in_=kTt[p][:, tt * 128:(tt + 1) * 128], identity=identb[0:64, 0:64]), reads=[f"ret_kT{p}", "identb"], writes=["ps_bf"])
                for h2 in range(2):
                    h = p * 2 + h2
                    S.op("dve", lambda e, tt=tt, h=h: e.tensor_tensor(out=kzp[:, tt, h, :], in0=pst[:, 0:64], in1=zmask[:, h, :], op=ALU.mult), reads=["ps_bf", "ret_zm"], writes=["ret_kzp"])
        for h in range(4):
            proj_fm(S, ps[7], "ps7", 64, wg, "ret_wg", h * 64, xT[b], f"xT{b}")
            S.op("act", lambda e: e.activation(out=sg, in_=ps[7][0:64, 0:TB], func=AF.Sigmoid), reads=["ps7"], writes=["ret_sg"])
            S.op("dve", lambda e, h=h: e.tensor_tensor(out=gl[h], in0=ps[7][0:64, 0:TB], in1=sg, op=ALU.mult), reads=["ps7", "ret_sg"], writes=[f"ret_gl{h}"])
        for h in range(4):
            p = h // 2
            hs = slice((h % 2) * 32, (h % 2 + 1) * 32)
            po = ps[4 + (h % 2)]
            pon = f"ps{4 + (h % 2)}"
            for tt in range(4):
                cs = slice(tt * 128, (tt + 1) * 128)
                pb = cnt % 2
                cnt += 1
                S.op("pe", lambda e, hs=hs, cs=cs, pb=pb, p=p: e.matmul(ps[pb][:, 0:128], lhsT=kTt[p][hs, cs], rhs=qT[p][hs, cs], start=True, stop=True),
                     reads=[f"ret_kT{p}", f"ret_qT{p}"], writes=[f"ps{pb}"])
                S.op("dve", lambda e, h=h, pb=pb: e.tensor_tensor(out=Pb[pb], in0=ps[pb][:, 0:128], in1=DT[:, h, :], op=ALU.mult), reads=[f"ps{pb}", "ret_DT"], writes=[f"ret_P{pb}"])
                S.op("pe", lambda e, h=h, tt=tt, cs=cs, pb=pb, po=po: e.matmul(po[0:64, cs], lhsT=vbf[:, tt, h * 64:(h + 1) * 64], rhs=Pb[pb], start=True, stop=False),
                     reads=["ret_v", f"ret_P{pb}"], writes=[pon])
                S.op("pe", lambda e, hs=hs, cs=cs, po=po, p=p: e.matmul(po[0:64, cs], lhsT=Sbf[p][hs, :], rhs=qx[p][hs, cs], start=False, stop=True),
                     reads=[f"ret_Sbf{p}", f"ret_qx{p}"], writes=[pon])
                S.op("pe", lambda e, h=h, tt=tt: e.matmul(ps[2][0:64, 0:64], lhsT=kzp[:, tt, h, :], rhs=vbf[:, tt, h * 64:(h + 1) * 64], start=True, stop=True),
                     reads=["ret_kzp", "ret_v"], writes=["ps2"])
                S.op("dve", lambda e, h=h, hs=hs, p=p: e.scalar_tensor_tensor(out=Sall[p][hs, :], in0=Sall[p][hs, :], scalar=RET_GAM[h] ** 128, in1=ps[2][hs, 0:64], op0=ALU.mult, op1=ALU.add),
                     reads=["ps2", f"ret_S{p}"], writes=[f"ret_S{p}"])
                S.op("dve", lambda e, hs=hs, p=p: e.tensor_copy(out=Sbf[p][hs, :], in_=Sall[p][hs, :]), reads=[f"ret_S{p}"], writes=[f"ret_Sbf{p}"])
            S.op("act", lambda e, po=po: e.activation(out=of, in_=po[0:64, 0:TB], func=AF.Identity), reads=[pon], writes=["ret_of"])
            S.op("dve", lambda e, po=po: e.tensor_tensor(out=sq, in0=po[0:64, 0:TB], in1=of, op=ALU.mult), reads=[pon, "ret_of"], writes=["ret_sq"])
            S.op("pe", lambda e: e.matmul(ps[3][0:64, 0:TB // 2], lhsT=K["ones_f"][0:64, 0:64], rhs=sq[:, 0:TB // 2], start=True, stop=True), reads=["ret_sq", "kc3"], writes=["ps3"])
            S.op("pe", lambda e: e.matmul(ps[2][0:64, 0:TB // 2], lhsT=K["ones_f"][0:64, 0:64], rhs=sq[:, TB // 2:TB], start=True, stop=True), reads=["ret_sq", "kc3"], writes=["ps2"])
            rstd_from_ss(S, K, ps[3], "ps3", 64, TB // 2, 1.0 / 64, "eps_n6", K["rstd64"], "rstd64")
            S.op("act", lambda e: e.activation(out=K["rstd64"][0:64, TB // 2:TB], in_=ps[2][0:64, 0:TB // 2], func=AF.Sqrt, bias=K["eps_n6"][0:64, 0:1], scale=1.0 / 64),
                 reads=["ps2", "kc2"], writes=["rstd64"])
            S.op("dve", lambda e: e.reciprocal(out=K["rstd64"][0:64, TB // 2:TB], in_=K["rstd64"][0:64, TB // 2:TB]), reads=["rstd64"], writes=["rstd64"])
            S.op("dve", lambda e, h=h: e.scalar_tensor_tensor(out=of, in0=of, scalar=gcol[:, h:h + 1], in1=K["rstd64"][0:64, 0:TB], op0=ALU.mult, op1=ALU.mult),
                 reads=["ret_of", "rstd64", "ret_g"], writes=["ret_of"])
            S.op("dve", lambda e, h=h: e.tensor_tensor(out=obf[h % 2], in0=of, in1=gl[h], op=ALU.mult), reads=["ret_of", f"ret_gl{h}"], writes=[f"robf{h % 2}"])
            S.dma("sp", lambda e, h=h, t0=t0: e.dma_start(out=oT_d[h * 64:(h + 1) * 64, t0:t0 + TB], in_=obf[h % 2]), reads=[f"robf{h % 2}"], writes=[f"oTa_d{t0 // 128}_{h}"])
    S.barrier()
    A.release(m0)


def host_consts_ret():
    import ml_dtypes
    c = {}
    lg = np.log(np.array(RET_GAM, dtype=np.float64))
    j = np.arange(128)[:, None]
    i = np.arange(128)[None, :]
    c["c_ret_decayT"] = np.stack([np.where(i >= j, np.exp((i - j) * lg[h]), 0.0) for h in range(4)]).astype(np.float32)
    t = np.arange(TB)
    xi = np.stack([np.exp(((t % 128) + 1.0) * lg[h]) for h in range(4)])
    c["c_ret_xi"] = np.ascontiguousarray(np.repeat(xi, 32, axis=0).astype(np.float32))
    zm = np.zeros((4, 128, 64), np.float32)
    for h in range(4):
        zm[h, :, (h % 2) * 32:(h % 2 + 1) * 32] = np.exp((127 - np.arange(128)) * lg[h])[:, None]
    c["c_ret_zmask"] = zm.astype(ml_dtypes.bfloat16)
    r = _rope_tables(SEQ, 32, 10000.0)
    c["c_rope_ret2"] = np.ascontiguousarray(np.tile(r, (1, 2, 1)))
    return c
RW_C = 64


def load_small_bf16(S, A, K, src_ap, rows, ncols, name):
    wb = A.alloc([rows, ncols], BF16)
    stg = K["wstg"]
    i = K["wstg_i"][0] % 2
    K["wstg_i"][0] += 1
    S.dma("sp", lambda e: e.dma_start(out=stg[i][0:rows, 0:ncols], in_=src_ap), writes=[f"wstg{i}"])
    S.op("pool", lambda e: e.tensor_copy(out=wb, in_=stg[i][0:rows, 0:ncols]), reads=[f"wstg{i}"], writes=[name])
    return wb


def mixer_rwkv(P, S, A, l, K, xT_d, oT_d, vfirst_d, nblk=NB_FULL):
    W = P.din
    ps = K["ps"]
    m0 = A.mark()
    EXPM05 = math.exp(-0.5)

    def col(src1d, lo, rows, name):
        t = A.alloc([rows, 1])
        S.dma("sp", lambda e: e.dma_start(out=t, in_=src1d[lo:lo + rows].rearrange("(p o) -> p o", o=1)), writes=[name])
        return t

    wr = load_w_bf16(S, A, K, W["w_in"][P.wl(l)][:, 1536:1792], 256, "rw_wr")
    wk = load_w_bf16(S, A, K, W["w_in"][P.wl(l)][:, 1792:2048], 256, "rw_wk")
    wv = load_w_bf16(S, A, K, W["w_in"][P.wl(l)][:, 2048:2304], 256, "rw_wv")
    wlo = load_w_bf16(S, A, K, W["w_in"][P.wl(l)][:, 2304:2560], 256, "rw_wlo")
    w_up = load_small_bf16(S, A, K, W["rwkv_w_up"][P.wl(l)], 64, 256, "rw_wup")
    a_up = load_small_bf16(S, A, K, W["rwkv_a_up"][P.wl(l)], 64, 256, "rw_aup")
    g_up = load_small_bf16(S, A, K, W["rwkv_g_up"][P.wl(l)], 128, 256, "rw_gup")
    if l > 0:
        wvr = load_w_bf16(S, A, K, W["rwkv_vres_down"][P.vl(l)], 32, "rw_wvr")
        v_up = load_small_bf16(S, A, K, W["rwkv_v_up"][P.vl(l)], 32, 256, "rw_vup")
    mu = W["rwkv_mu"][P.wl(l)]
    pc = {}
    for hp in range(2):
        pc[f"mu_r{hp}"] = col(mu, hp * 128, 128, f"rwc")
        pc[f"mu_k{hp}"] = col(mu, 256 + hp * 128, 128, "rwc1")
        pc[f"mu_v{hp}"] = col(mu, 512 + hp * 128, 128, "rwc2")
        pc[f"w0{hp}"] = col(W["rwkv_w0"][P.wl(l)], hp * 128, 128, "rwc3")
        pc[f"a0{hp}"] = col(W["rwkv_a0"][P.wl(l)], hp * 128, 128, "rwc4")
        pc[f"kk{hp}"] = col(W["rwkv_k_k"][P.wl(l)], hp * 128, 128, "rwc5")
        pc[f"ka{hp}"] = col(W["rwkv_k_a"][P.wl(l)], hp * 128, 128, "rwc6")
        pc[f"rk{hp}"] = col(W["rwkv_r_k"][P.wl(l)].rearrange("h d -> (h d)"), hp * 128, 128, "rwc7")
        pc[f"lg{hp}"] = col(W["rwkv_ln_g"][P.wl(l)], hp * 128, 128, "rwc8")
        pc[f"lb{hp}"] = col(W["rwkv_ln_b"][P.wl(l)], hp * 128, 128, "rwc9")
        pc[f"omka{hp}"] = A.alloc([128, 1])
        S.op("dve", lambda e, hp=hp: e.tensor_scalar(out=pc[f"omka{hp}"], in0=pc[f"ka{hp}"], scalar1=-1.0, scalar2=1.0, op0=ALU.mult, op1=ALU.add), reads=["rwc6"], writes=["rwc10"])
        if l > 0:
            pc[f"v0{hp}"] = col(W["rwkv_v0"][P.vl(l)], hp * 128, 128, "rwc11")
    pc["mu_wd"] = col(mu, 768, 64, "rwc12")
    pc["mu_ad"] = col(mu, 832, 64, "rwc13")
    pc["mu_gd"] = col(mu, 896, 128, "rwc14")
    if l > 0:
        pc["mu_vd"] = col(W["rwkv_vres_mu"][P.vl(l)], 0, 32, "rwc15")
    PCN = ["rwc", "rwc1", "rwc2", "rwc3", "rwc4", "rwc5", "rwc6", "rwc7", "rwc8", "rwc9", "rwc10", "rwc11", "rwc12", "rwc13", "rwc14", "rwc15"]
    msk = A.alloc([128, 3, 128])
    S.dma("sp", lambda e: e.dma_start(out=msk, in_=W["c_rwkv_masks"].rearrange("m p c -> p m c")), writes=["rw_msk"])
    bdones = A.alloc([128, 128]); bdavg = A.alloc([128, 128]); rst = A.alloc([128, TB])
    S.dma("sp", lambda e: e.dma_start(out=bdones, in_=W["c_rwkv_bdones"]), writes=["rw_bdo"])
    S.dma("sp", lambda e: e.dma_start(out=bdavg, in_=W["c_rwkv_bdavg"]), writes=["rw_bda"])
    S.dma("sp", lambda e: e.dma_start(out=rst, in_=W["c_rwkv_reset"]), writes=["rw_rst"])
    eps_gn = A.alloc([128, 1])
    S.op("dve", lambda e: e.memset(eps_gn, 64e-5), writes=["rw_eps"])
    ident = K["ident"]

    xT = [A.alloc([128, 8, TB], BF16) for i in range(2)]
    raw = {}
    for nm, rows in (("r0", 128), ("r1", 128), ("k0", 128), ("k1", 128), ("v0", 128), ("v1", 128), ("wd", 64), ("ad", 64), ("gd", 128), ("vd", 32)):
        if nm == "vd" and l == 0:
            continue
        raw[nm] = A.alloc([rows, TB + 1])
        S.op("pool", lambda e, nm=nm: e.memset(raw[nm][:, 0:1], 0.0), writes=[f"raw_{nm}"])
    dif = A.alloc([128, TB])
    T_ = {}
    for hp in range(2):
        for nm in ("rs", "ks", "vs", "lw", "a", "g", "kk", "kp", "cum", "G", "Gi", "Gp", "At", "Bt", "Kt", "Rt", "bon", "yT"):
            T_[f"{nm}{hp}"] = A.alloc([128, TB])
    wds = A.alloc([64, TB]); ads = A.alloc([64, TB], BF16); gds = A.alloc([128, TB]); th = A.alloc([64, TB], BF16); sgd = A.alloc([128, TB], BF16)
    vds = A.alloc([32, TB], BF16) if l > 0 else None
    tmp = A.alloc([128, TB]); tmp2 = A.alloc([128, TB])
    obf = A.alloc([128, TB], BF16)
    H = [A.alloc([128, 128]) for hp in range(2)]
    for hp in range(2):
        S.op("pool", lambda e, hp=hp: e.memset(H[hp], 0.0), writes=[f"rw_H{hp}"])
    BD = {}
    for hp in range(2):
        for nm in ("A", "B", "K", "R", "V"):
            for i in range(2):
                BD[(hp, nm, i)] = A.alloc([128, 128])
                S.op("pool", lambda e, k=(hp, nm, i): e.memset(BD[k], 0.0), writes=[f"bd{hp}{nm}{i}"])
    CH = {}
    for hp in range(2):
        for nm in ("M", "MT", "M2", "M2T", "PT", "NakT", "QbT", "QkT", "Btok", "Ktok", "Vtok", "W1", "U", "Yh"):
            CH[(hp, nm)] = A.alloc([128, 128])

    def slot(hp, i):
        bank = ps[hp * 3 + i // 4]
        q = i % 4
        return bank[:, q * 128:(q + 1) * 128], f"ps{hp * 3 + i // 4}"

    def shift(nm, rows, wb, wname, col0, mucol, out, outname, b, odt_copy=None):
        proj_fm(S, ps[6], "ps6", rows, wb, wname, col0, xT[b], f"xT{b}")
        r = raw[nm]
        S.op("act", lambda e: e.activation(out=r[:, 1:TB + 1], in_=ps[6][0:rows, 0:TB], func=AF.Identity), reads=["ps6"], writes=[f"raw_{nm}"])
        S.op("pool", lambda e: e.tensor_tensor(out=dif[0:rows, :], in0=r[:, 0:TB], in1=r[:, 1:TB + 1], op=ALU.subtract), reads=[f"raw_{nm}"], writes=["rw_dif"])
        S.op("dve", lambda e: e.scalar_tensor_tensor(out=out, in0=dif[0:rows, :], scalar=mucol[:, 0:1], in1=r[:, 1:TB + 1], op0=ALU.mult, op1=ALU.add),
             reads=["rw_dif", f"raw_{nm}"] + PCN, writes=[outname])
        S.op("pool", lambda e: e.tensor_copy(out=r[:, 0:1], in_=r[:, TB:TB + 1]), reads=[f"raw_{nm}"], writes=[f"raw_{nm}"])

    ev = [0]

    def evac(out, out_name, src, src_name, extra_reads=()):
        ev[0] += 1
        if True:
            S.op("dve", lambda e: e.tensor_copy(out=out, in_=src), reads=[src_name] + list(extra_reads), writes=[out_name])
        else:
            S.op("act", lambda e: e.activation(out=out, in_=src, func=AF.Identity), reads=[src_name] + list(extra_reads), writes=[out_name])

    nch = TB // RW_C
    gch = 0
    for tb in range(nblk):
        b = tb % 2
        t0 = tb * TB
        S.dma("sp", lambda e, b=b, t0=t0: e.dma_start(out=xT[b], in_=xT_d[:, t0:t0 + TB].rearrange("(c p) t -> p c t", p=128)), writes=[f"xT{b}"])
        shift("wd", 64, wlo, "rw_wlo", 0, pc["mu_wd"], wds, "rw_wds", b)
        S.op("act", lambda e: e.activation(out=th, in_=wds, func=AF.Tanh), reads=["rw_wds"], writes=["rw_th"])
        shift("ad", 64, wlo, "rw_wlo", 64, pc["mu_ad"], ads, "rw_ads", b)
        shift("gd", 128, wlo, "rw_wlo", 128, pc["mu_gd"], gds, "rw_gds", b)
        S.op("act", lambda e: e.activation(out=sgd, in_=gds, func=AF.Sigmoid), reads=["rw_gds"], writes=["rw_sgd"])
        if l > 0:
            shift("vd", 32, wvr, "rw_wvr", 0, pc["mu_vd"], vds, "rw_vds", b)
        def pair_prep(hp, b, t0, tb):
            t = lambda nm: T_[f"{nm}{hp}"]
            n = lambda nm: f"rw_{nm}{hp}"
            shift(f"r{hp}", 128, wr, "rw_wr", hp * 128, pc[f"mu_r{hp}"], t("rs"), n("rs"), b)
            shift(f"k{hp}", 128, wk, "rw_wk", hp * 128, pc[f"mu_k{hp}"], t("ks"), n("ks"), b)
            shift(f"v{hp}", 128, wv, "rw_wv", hp * 128, pc[f"mu_v{hp}"], t("vs"), n("vs"), b)
            S.op("pe", lambda e, hp=hp: e.matmul(ps[7][:, 0:TB], lhsT=w_up[:, hp * 128:(hp + 1) * 128], rhs=th, start=True, stop=True), reads=["rw_wup", "rw_th"], writes=["ps7"])
            S.op("act", lambda e, hp=hp: e.activation(out=t("lw"), in_=ps[7][:, 0:TB], func=AF.Sigmoid, bias=pc[f"w0{hp}"][:, 0:1], scale=1.0), reads=["ps7"] + PCN, writes=[n("lw")])
            S.op("dve", lambda e, hp=hp: e.tensor_scalar(out=t("lw"), in0=t("lw"), scalar1=-EXPM05, scalar2=None, op0=ALU.mult), reads=[n("lw")], writes=[n("lw")])
            S.op("pe", lambda e, hp=hp: e.matmul(ps[7][:, 0:TB], lhsT=a_up[:, hp * 128:(hp + 1) * 128], rhs=ads, start=True, stop=True), reads=["rw_aup", "rw_ads"], writes=["ps7"])
            S.op("act", lambda e, hp=hp: e.activation(out=t("a"), in_=ps[7][:, 0:TB], func=AF.Sigmoid, bias=pc[f"a0{hp}"][:, 0:1], scale=1.0), reads=["ps7"] + PCN, writes=[n("a")])
            S.op("pe", lambda e, hp=hp: e.matmul(ps[7][:, 0:TB], lhsT=g_up[:, hp * 128:(hp + 1) * 128], rhs=sgd, start=True, stop=True), reads=["rw_gup", "rw_sgd"], writes=["ps7"])
            S.op("dve", lambda e, hp=hp: e.tensor_copy(out=t("g"), in_=ps[7][:, 0:TB]), reads=["ps7"], writes=[n("g")])
            if l > 0:
                S.op("pe", lambda e, hp=hp: e.matmul(ps[7][:, 0:TB], lhsT=v_up[:, hp * 128:(hp + 1) * 128], rhs=vds, start=True, stop=True), reads=["rw_vup", "rw_vds"], writes=["ps7"])
                S.op("act", lambda e, hp=hp: e.activation(out=tmp, in_=ps[7][:, 0:TB], func=AF.Sigmoid, bias=pc[f"v0{hp}"][:, 0:1], scale=1.0), reads=["ps7"] + PCN, writes=["rw_tmp"])
                S.dma("sp", lambda e, hp=hp, t0=t0: e.dma_start(out=tmp2, in_=vfirst_d[hp, :, t0:t0 + TB]), writes=["rw_tmp2"])
                S.op("pool", lambda e, hp=hp: e.tensor_tensor(out=tmp2, in0=tmp2, in1=t("vs"), op=ALU.subtract), reads=["rw_tmp2", n("vs")], writes=["rw_tmp2"])
                S.op("dve", lambda e, hp=hp: e.tensor_tensor(out=tmp2, in0=tmp2, in1=tmp, op=ALU.mult), reads=["rw_tmp2", "rw_tmp"], writes=["rw_tmp2"])
                S.op("dve", lambda e, hp=hp: e.tensor_tensor(out=t("vs"), in0=t("vs"), in1=tmp2, op=ALU.add), reads=["rw_tmp2", n("vs")], writes=[n("vs")])
            else:
                S.dma("sp", lambda e, hp=hp, t0=t0: e.dma_start(out=vfirst_d[hp, :, t0:t0 + TB], in_=t("vs")), reads=[n("vs")], writes=[f"vfirst{hp}_{tb}"])
            S.op("dve", lambda e, hp=hp: e.tensor_scalar(out=t("kk"), in0=t("ks"), scalar1=pc[f"kk{hp}"][:, 0:1], scalar2=None, op0=ALU.mult), reads=[n("ks")] + PCN, writes=[n("kk")])
            S.op("pool", lambda e, hp=hp: e.tensor_tensor(out=tmp, in0=t("kk"), in1=t("kk"), op=ALU.mult), reads=[n("kk")], writes=["rw_tmp"])
            S.op("pe", lambda e: e.matmul(ps[7][:, 0:TB], lhsT=bdones, rhs=tmp, start=True, stop=True), reads=["rw_bdo", "rw_tmp"], writes=["ps7"])
            S.op("act", lambda e: e.activation(out=tmp2, in_=ps[7][:, 0:TB], func=AF.Sqrt), reads=["ps7"], writes=["rw_tmp2"])
            S.op("dve", lambda e: e.tensor_scalar(out=tmp2, in0=tmp2, scalar1=1e-12, scalar2=None, op0=ALU.max), reads=["rw_tmp2"], writes=["rw_tmp2"])
            S.op("dve", lambda e: e.reciprocal(out=tmp2, in_=tmp2), reads=["rw_tmp2"], writes=["rw_tmp2"])
            S.op("dve", lambda e, hp=hp: e.tensor_tensor(out=t("kk"), in0=t("kk"), in1=tmp2, op=ALU.mult), reads=[n("kk"), "rw_tmp2"], writes=[n("kk")])
            S.op("dve", lambda e, hp=hp: e.tensor_scalar(out=tmp, in0=t("a"), scalar1=pc[f"ka{hp}"][:, 0:1], scalar2=pc[f"omka{hp}"][:, 0:1], op0=ALU.mult, op1=ALU.add),
                 reads=[n("a")] + PCN, writes=["rw_tmp"])
            S.op("dve", lambda e, hp=hp: e.tensor_tensor(out=t("kp"), in0=t("ks"), in1=tmp, op=ALU.mult), reads=[n("ks"), "rw_tmp"], writes=[n("kp")])
            S.op("dve", lambda e, hp=hp: e.tensor_tensor_scan(out=t("cum"), data0=rst, data1=t("lw"), initial=0.0, op0=ALU.mult, op1=ALU.add), reads=["rw_rst", n("lw")], writes=[n("cum")])
            S.op("act", lambda e, hp=hp: e.activation(out=t("G"), in_=t("cum"), func=AF.Exp), reads=[n("cum")], writes=[n("G")])
            S.op("act", lambda e, hp=hp: e.activation(out=t("Gi"), in_=t("cum"), func=AF.Exp, scale=-1.0), reads=[n("cum")], writes=[n("Gi")])
            S.op("pool", lambda e, hp=hp: e.tensor_tensor(out=tmp, in0=t("cum"), in1=t("lw"), op=ALU.subtract), reads=[n("cum"), n("lw")], writes=["rw_tmp"])
            S.op("act", lambda e, hp=hp: e.activation(out=t("Gp"), in_=tmp, func=AF.Exp), reads=["rw_tmp"], writes=[n("Gp")])
            S.op("dve", lambda e, hp=hp: e.scalar_tensor_tensor(out=t("At"), in0=t("kk"), scalar=-1.0, in1=t("Gp"), op0=ALU.mult, op1=ALU.mult), reads=[n("kk"), n("Gp")], writes=[n("At")])
            S.op("pool", lambda e, hp=hp: e.tensor_tensor(out=tmp, in0=t("kk"), in1=t("a"), op=ALU.mult), reads=[n("kk"), n("a")], writes=["rw_tmp"])
            S.op("dve", lambda e, hp=hp: e.tensor_tensor(out=t("Bt"), in0=tmp, in1=t("Gi"), op=ALU.mult), reads=["rw_tmp", n("Gi")], writes=[n("Bt")])
            S.op("pool", lambda e, hp=hp: e.tensor_tensor(out=t("Kt"), in0=t("kp"), in1=t("Gi"), op=ALU.mult), reads=[n("kp"), n("Gi")], writes=[n("Kt")])
            S.op("pool", lambda e, hp=hp: e.tensor_tensor(out=t("Rt"), in0=t("rs"), in1=t("G"), op=ALU.mult), reads=[n("rs"), n("G")], writes=[n("Rt")])
            S.op("dve", lambda e, hp=hp: e.scalar_tensor_tensor(out=tmp, in0=t("rs"), scalar=pc[f"rk{hp}"][:, 0:1], in1=t("kp"), op0=ALU.mult, op1=ALU.mult), reads=[n("rs"), n("kp")] + PCN, writes=["rw_tmp"])
            S.op("pe", lambda e: e.matmul(ps[7][:, 0:TB], lhsT=bdones, rhs=tmp, start=True, stop=True), reads=["rw_bdo", "rw_tmp"], writes=["ps7"])
            S.op("dve", lambda e, hp=hp: e.tensor_copy(out=t("bon"), in_=ps[7][:, 0:TB]), reads=["ps7"], writes=[n("bon")])
        for hp in range(2):
            pair_prep(hp, b, t0, tb)

        import os
        RWS = int(os.environ.get("RW_STOP", "99"))
        if RWS <= 1:
            continue

        def chunk_pair(hp, c, cs, bi):
            if True:
                t = lambda nm: T_[f"{nm}{hp}"]
                n = lambda nm: f"rw_{nm}{hp}"
                C_ = lambda nm: CH[(hp, nm)]
                cn = lambda nm: f"ch{hp}{nm}"
                bd = {}
                for k_, src in (("A", "At"), ("B", "Bt"), ("K", "Kt"), ("R", "Rt"), ("V", "vs")):
                    d = BD[(hp, k_, bi)]
                    bd[k_] = (d, f"bd{hp}{k_}{bi}")
                    S.op("pool", lambda e, d=d, src=src, hp=hp: e.tensor_copy(out=d[0:64, 0:64], in_=T_[f"{src}{hp}"][0:64, cs]), reads=[f"rw_{src}{hp}"], writes=[f"bd{hp}{k_}{bi}"])
                    S.op("pool", lambda e, d=d, src=src, hp=hp: e.tensor_copy(out=d[64:128, 64:128], in_=T_[f"{src}{hp}"][64:128, cs]), reads=[f"rw_{src}{hp}"], writes=[f"bd{hp}{k_}{bi}"])

                def prod(si, lk, rk, mask_i, out_nm):
                    sl, sn = slot(hp, si)
                    S.op("pe", lambda e: e.matmul(sl, lhsT=bd[lk][0], rhs=bd[rk][0], start=True, stop=True), reads=[bd[lk][1], bd[rk][1]], writes=[sn])
                    S.op("dve", lambda e: e.tensor_tensor(out=C_(out_nm), in0=sl, in1=msk[:, mask_i, :], op=ALU.mult), reads=[sn, "rw_msk"], writes=[cn(out_nm)])
                prod(0, "A", "B", 0, "M")
                prod(1, "B", "A", 1, "MT")
                prod(2, "K", "A", 1, "NakT")
                prod(3, "B", "R", 2, "QbT")
                prod(4, "K", "R", 2, "QkT")
                if RWS <= 2:
                    return
                S.op("pool", lambda e: e.tensor_tensor(out=C_("PT"), in0=C_("MT"), in1=ident, op=ALU.add), reads=[cn("MT"), "kc"], writes=[cn("PT")])
                curM, curMT, nxtM, nxtMT = "M", "MT", "M2", "M2T"
                for lev in range(5):
                    s5, n5 = slot(hp, 5)
                    s6, n6 = slot(hp, 6)
                    s7, n7 = slot(hp, 7)
                    S.op("pe", lambda e, a_=curMT, b_=curM, s5=s5: e.matmul(s5, lhsT=C_(a_), rhs=C_(b_), start=True, stop=True), reads=[cn(curMT), cn(curM)], writes=[n5])
                    if lev < 4:
                        S.op("pe", lambda e, a_=curM, b_=curMT, s6=s6: e.matmul(s6, lhsT=C_(a_), rhs=C_(b_), start=True, stop=True), reads=[cn(curM), cn(curMT)], writes=[n6])
                    evac(C_(nxtM), cn(nxtM), s5, n5)
                    if lev < 4:
                        evac(C_(nxtMT), cn(nxtMT), s6, n6)
                    S.op("pe", lambda e, a_=nxtM, s7=s7: e.matmul(s7, lhsT=C_(a_), rhs=C_("PT"), start=True, stop=True), reads=[cn(nxtM), cn("PT")], writes=[n7])
                    S.op("dve", lambda e, s7=s7: e.tensor_tensor(out=C_("PT"), in0=C_("PT"), in1=s7, op=ALU.add), reads=[cn("PT"), n7], writes=[cn("PT")])
                    curM, curMT, nxtM, nxtMT = nxtM, nxtMT, curM, curMT
                if RWS <= 3:
                    return
                s8, n8 = slot(hp, 8)
                for k_, onm in (("B", "Btok"), ("K", "Ktok"), ("V", "Vtok")):
                    S.op("pe", lambda e, k_=k_: e.transpose(out=s8, in_=bd[k_][0], identity=ident), reads=[bd[k_][1], "kc"], writes=[n8])
                    evac(C_(onm), cn(onm), s8, n8)
                if RWS <= 4:
                    return
                s9, n9 = slot(hp, 9)
                S.op("pe", lambda e: e.matmul(s9, lhsT=bd["A"][0], rhs=H[hp], start=True, stop=False), reads=[bd["A"][1], f"rw_H{hp}"], writes=[n9])
                S.op("pe", lambda e: e.matmul(s9, lhsT=C_("NakT"), rhs=C_("Vtok"), start=False, stop=True), reads=[cn("NakT"), cn("Vtok")], writes=[n9])
                evac(C_("W1"), cn("W1"), s9, n9)
                S.op("pe", lambda e: e.matmul(s9, lhsT=C_("PT"), rhs=C_("W1"), start=True, stop=True), reads=[cn("PT"), cn("W1")], writes=[n9])
                evac(C_("U"), cn("U"), s9, n9)
                s10, n10 = slot(hp, 10)
                S.op("pe", lambda e: e.matmul(s10, lhsT=H[hp], rhs=bd["R"][0], start=True, stop=False), reads=[f"rw_H{hp}", bd["R"][1]], writes=[n10])
                S.op("pe", lambda e: e.matmul(s10, lhsT=C_("U"), rhs=C_("QbT"), start=False, stop=False), reads=[cn("U"), cn("QbT")], writes=[n10])
                S.op("pe", lambda e: e.matmul(s10, lhsT=C_("Vtok"), rhs=C_("QkT"), start=False, stop=True), reads=[cn("Vtok"), cn("QkT")], writes=[n10])
                S.op("dve", lambda e: e.tensor_copy(out=C_("Yh"), in_=s10), reads=[n10], writes=[cn("Yh")])
                S.op("dve", lambda e, hp=hp: e.tensor_tensor(out=T_[f"yT{hp}"][:, cs], in0=C_("Yh")[:, 0:64], in1=s10[:, 64:128], op=ALU.add), reads=[cn("Yh"), n10], writes=[f"rw_yT{hp}"])
                s11, n11 = slot(hp, 11)
                S.op("pe", lambda e: e.matmul(s11, lhsT=C_("Btok"), rhs=C_("U"), start=True, stop=False), reads=[cn("Btok"), cn("U")], writes=[n11])
                S.op("pe", lambda e: e.matmul(s11, lhsT=C_("Ktok"), rhs=C_("Vtok"), start=False, stop=True), reads=[cn("Ktok"), cn("Vtok")], writes=[n11])
                S.op("dve", lambda e, hp=hp: e.tensor_tensor(out=H[hp], in0=H[hp], in1=s11, op=ALU.add), reads=[f"rw_H{hp}", n11], writes=[f"rw_H{hp}"])
                S.op("dve", lambda e, hp=hp, c=c: e.tensor_scalar(out=H[hp], in0=H[hp], scalar1=T_[f"G{hp}"][:, c * RW_C + RW_C - 1:c * RW_C + RW_C], scalar2=None, op0=ALU.mult),
                     reads=[f"rw_H{hp}", f"rw_G{hp}"], writes=[f"rw_H{hp}"])
        for c in range(nch):
            cs = slice(c * RW_C, (c + 1) * RW_C)
            bi = gch % 2
            gch += 1
            for hp in range(2):
                chunk_pair(hp, c, cs, bi)

        def finish_pair(hp, t0, tb):
            t = lambda nm: T_[f"{nm}{hp}"]
            n = lambda nm: f"rw_{nm}{hp}"
            S.op("pe", lambda e, hp=hp: e.matmul(ps[6][:, 0:TB], lhsT=bdavg, rhs=t("yT"), start=True, stop=True), reads=["rw_bda", n("yT")], writes=["ps6"])
            S.op("pool", lambda e, hp=hp: e.tensor_tensor(out=tmp, in0=t("yT"), in1=t("yT"), op=ALU.mult), reads=[n("yT")], writes=["rw_tmp"])
            S.op("pe", lambda e: e.matmul(ps[7][:, 0:TB], lhsT=bdavg, rhs=tmp, start=True, stop=True), reads=["rw_bda", "rw_tmp"], writes=["ps7"])
            S.op("act", lambda e: e.activation(out=tmp2, in_=ps[6][:, 0:TB], func=AF.Identity), reads=["ps6"], writes=["rw_tmp2"])
            S.op("dve", lambda e: e.tensor_tensor(out=tmp, in0=tmp2, in1=tmp2, op=ALU.mult), reads=["rw_tmp2"], writes=["rw_tmp"])
            S.op("dve", lambda e: e.tensor_tensor(out=tmp, in0=ps[7][:, 0:TB], in1=tmp, op=ALU.subtract), reads=["ps7", "rw_tmp"], writes=["rw_tmp"])
            S.op("dve", lambda e: e.tensor_scalar(out=tmp, in0=tmp, scalar1=0.0, scalar2=None, op0=ALU.max), reads=["rw_tmp"], writes=["rw_tmp"])
            S.op("act", lambda e: e.activation(out=tmp, in_=tmp, func=AF.Sqrt, bias=eps_gn[:, 0:1], scale=1.0), reads=["rw_tmp", "rw_eps"], writes=["rw_tmp"])
            S.op("dve", lambda e: e.reciprocal(out=tmp, in_=tmp), reads=["rw_tmp"], writes=["rw_tmp"])
            S.op("dve", lambda e, hp=hp: e.tensor_tensor(out=tmp2, in0=t("yT"), in1=tmp2, op=ALU.subtract), reads=[n("yT"), "rw_tmp2"], writes=["rw_tmp2"])
            S.op("dve", lambda e: e.tensor_tensor(out=tmp2, in0=tmp2, in1=tmp, op=ALU.mult), reads=["rw_tmp2", "rw_tmp"], writes=["rw_tmp2"])
            S.op("dve", lambda e, hp=hp: e.tensor_scalar(out=tmp2, in0=tmp2, scalar1=pc[f"lg{hp}"][:, 0:1], scalar2=pc[f"lb{hp}"][:, 0:1], op0=ALU.mult, op1=ALU.add),
                 reads=["rw_tmp2"] + PCN, writes=["rw_tmp2"])
            S.op("pool", lambda e, hp=hp: e.tensor_tensor(out=tmp, in0=t("bon"), in1=t("vs"), op=ALU.mult), reads=[n("bon"), n("vs"), "rw_tmp"], writes=["rw_tmp"])
            S.op("dve", lambda e: e.tensor_tensor(out=tmp2, in0=tmp2, in1=tmp, op=ALU.add), reads=["rw_tmp2", "rw_tmp"], writes=["rw_tmp2"])
            S.op("dve", lambda e, hp=hp: e.tensor_tensor(out=obf, in0=tmp2, in1=t("g"), op=ALU.mult), reads=["rw_tmp2", n("g")], writes=["rw_obf"])
            S.dma("sp", lambda e, hp=hp, t0=t0: e.dma_start(out=oT_d[512 + hp * 128:512 + (hp + 1) * 128, t0:t0 + TB], in_=obf), reads=["rw_obf"], writes=[f"oTc_d{tb}_{hp}"])
        for hp in range(2):
            finish_pair(hp, t0, tb)
    S.barrier()
    A.release(m0)


def host_consts_rwkv():
    c = {}
    p = np.arange(128)[:, None]
    q = np.arange(128)[None, :]
    same = (p // 64) == (q // 64)
    tl = p % 64
    sl = q % 64
    c["c_rwkv_masks"] = np.stack([same & (tl > sl), same & (tl < sl), same & (tl <= sl)]).astype(np.float32)
    c["c_rwkv_bdones"] = same.astype(np.float32)
    c["c_rwkv_bdavg"] = (same.astype(np.float32) / 64.0).astype(np.float32)
    r = np.ones((128, TB), np.float32)
    r[:, ::RW_C] = 0.0
    c["c_rwkv_reset"] = r
    return c
def prep_xT(P, S, A, K, x_d, xT_d, ntiles=NT):
    ps = K["ps"]
    m0 = A.mark()
    xt = [A.alloc([128, D]) for i in range(2)]
    xb = [A.alloc([128, 8, 128], BF16) for i in range(2)]
    for n in range(ntiles):
        b = n % 2
        S.dma("sp", lambda e, n=n, b=b: e.dma_start(out=xt[b], in_=x_d[n * 128:(n + 1) * 128, :]), writes=[f"px{b}"])
        for hlf in range(2):
            pb = ps[6 + hlf]
            for c4 in range(4):
                c = hlf * 4 + c4
                S.op("pe", lambda e, c=c, c4=c4, pb=pb, b=b: e.transpose(out=pb[:, c4 * 128:(c4 + 1) * 128], in_=xt[b][:, c * 128:(c + 1) * 128], identity=K["ident"]),
                     reads=[f"px{b}", "kc"], writes=[f"ps{6 + hlf}"])
            S.op("dve", lambda e, hlf=hlf, pb=pb, b=b: e.tensor_copy(out=xb[b][:, hlf * 4:(hlf + 1) * 4, :], in_=pb[:].rearrange("p (c t) -> p c t", c=4)),
                 reads=[f"ps{6 + hlf}"], writes=[f"pxb{b}"])
        S.dma("sp", lambda e, n=n, b=b: e.dma_start(out=xT_d[:, n * 128:(n + 1) * 128].rearrange("(c p) t -> p c t", p=128), in_=xb[b]), reads=[f"pxb{b}"], writes=[f"xT_d{n}"])
    S.barrier()
    A.release(m0)


def _rope_cs(n_pos, rot_dim, theta):
    inv = (1.0 / (np.float32(theta) ** (np.arange(0, rot_dim, 2, dtype=np.float32) / np.float32(rot_dim)))).astype(np.float32)
    ang = (np.arange(n_pos, dtype=np.float32)[:, None] * inv[None, :]).astype(np.float32)
    return np.cos(ang).astype(np.float32), np.sin(ang).astype(np.float32)


def _rope_tables(n_pos, rot_dim, theta, pad_front=0):
    cos, sin = _rope_cs(n_pos, rot_dim, theta)
    C = np.concatenate([cos, cos], axis=1).T
    Sg = np.concatenate([-sin, sin], axis=1).T
    if pad_front:
        C = np.concatenate([np.ones((pad_front, n_pos), np.float32), C], axis=0)
        Sg = np.concatenate([np.zeros((pad_front, n_pos), np.float32), Sg], axis=0)
    return np.ascontiguousarray(np.stack([C, Sg]).astype(np.float32))


def _perm_idx(rot_dim):
    h = rot_dim // 2
    return np.concatenate([np.arange(h, rot_dim), np.arange(0, h)])


def host_consts():
    import ml_dtypes
    c = {}
    c["c_ident"] = np.eye(128, dtype=np.float32)
    k = np.arange(128)[:, None]
    q = np.arange(512)[None, :]
    md = np.zeros((20, 128, 512), np.float32)
    for mi in range(20):
        d = mi - 3
        delta = 128 * d + q - k
        m = ((delta >= 0) & (delta <= 128)).astype(np.float32)
        m += ((delta >= 0) & (delta <= 512) & (delta % 4 == 0)).astype(np.float32)
        m += ((delta >= 0) & (delta <= 2048) & (delta % 16 == 0)).astype(np.float32)
        md[mi] = m
    c["c_mask_dil"] = md.astype(ml_dtypes.bfloat16)
    c["c_mask_mla"] = np.stack([((128 * (mi - 3) + q - k) >= 0).astype(np.float32) for mi in range(4)]).astype(ml_dtypes.bfloat16)
    c["c_rope_dil"] = _rope_tables(SEQ, 16, 500000.0)
    c["c_rope_mla"] = _rope_tables(SEQ, 32, 10000.0, pad_front=64)
    c.update(host_consts_ret())
    c.update(host_consts_rwkv())
    return c


def host_layouts(inp):
    L = {}
    w_in = inp["w_in"]
    nl = w_in.shape[0]
    p16 = _perm_idx(16)
    qcols = np.concatenate([768 + h * 64 + p16 for h in range(4)])
    kcols = np.concatenate([1024 + h * 64 + p16 for h in range(4)])
    L["dil_wperm"] = np.ascontiguousarray(w_in[:, :, np.concatenate([qcols, kcols])])
    p32 = _perm_idx(32)
    z64 = np.zeros((nl, D, 64), np.float32)
    kr = w_in[:, :, 2944:2976]
    L["mla_wkr_pad"] = np.ascontiguousarray(np.concatenate([z64, kr, z64, kr[:, :, p32]], axis=2))
    wq = inp["mla_w_q_up"]
    pq = np.concatenate([np.concatenate([h * 96 + np.arange(64), h * 96 + 64 + p32]) for h in range(4)])
    L["mla_wq_perm"] = np.ascontiguousarray(wq[:, :, pq])
    wkv = inp["mla_w_kv_up"]
    z32 = np.zeros((nl, 128, 32), np.float32)
    L["mla_wk_pad"] = np.ascontiguousarray(np.concatenate([np.concatenate([wkv[:, :, h * 128:h * 128 + 64], z32], axis=2) for h in range(4)], axis=2))
    qc = np.concatenate([h * 32 + p32 for h in range(4)])
    L["ret_wperm"] = np.ascontiguousarray(w_in[:, :, np.concatenate([qc, 128 + qc])])
    L["mla_wv"] = np.ascontiguousarray(np.concatenate([wkv[:, :, h * 128 + 64:h * 128 + 128] for h in range(4)], axis=2))
    return L


A_INPUTS = (("w_in", [DEPTH, D, N_IN]), ("dil_norm_g", [DEPTH, 256]), ("dil_wperm", [DEPTH, D, 128]),
            ("mla_wkr_pad", [DEPTH, D, 192]), ("mla_w_q_up", [DEPTH, 256, 384]), ("mla_wq_perm", [DEPTH, 256, 384]),
            ("mla_wk_pad", [DEPTH, 128, 384]), ("mla_wv", [DEPTH, 128, 256]), ("mla_q_norm_g", [DEPTH, 256]), ("mla_kv_norm_g", [DEPTH, 128]),
            ("mla_out_norm_g", [DEPTH, 256]), ("c_ident", [128, 128]), ("c_rope_dil", [2, 16, SEQ]), ("c_rope_mla", [2, 96, SEQ]), ("c_rope_ret2", [2, 64, SEQ]), ("ret_wperm", [DEPTH, D, 256]), ("ret_norm_g", [DEPTH, 256]),
            ("c_ret_decayT", [4, 128, 128]), ("c_ret_xi", [128, 512]),
            ("rwkv_mu", [DEPTH, 1024]), ("rwkv_w0", [DEPTH, 256]), ("rwkv_w_up", [DEPTH, 64, 256]), ("rwkv_a0", [DEPTH, 256]), ("rwkv_a_up", [DEPTH, 64, 256]),
            ("rwkv_g_up", [DEPTH, 128, 256]), ("rwkv_k_k", [DEPTH, 256]), ("rwkv_k_a", [DEPTH, 256]), ("rwkv_r_k", [DEPTH, 4, 64]), ("rwkv_ln_g", [DEPTH, 256]),
            ("rwkv_ln_b", [DEPTH, 256]), ("rwkv_vres_down", [DEPTH - 1, D, 32]), ("rwkv_vres_mu", [DEPTH - 1, 32]), ("rwkv_v0", [DEPTH - 1, 256]),
            ("rwkv_v_up", [DEPTH - 1, 32, 256]), ("c_rwkv_masks", [3, 128, 128]), ("c_rwkv_bdones", [128, 128]), ("c_rwkv_bdavg", [128, 128]), ("c_rwkv_reset", [128, 512]))
A_INPUTS_BF = (("c_mask_dil", [20, 128, 512]), ("c_mask_mla", [4, 128, 512]), ("c_ret_zmask", [4, 128, 64]))


def build_phase_a_test(nblk, which):
    P = Prog({})
    for nm, shp in A_INPUTS:
        P.inp(nm, shp)
    for nm, shp in A_INPUTS_BF:
        P.inp(nm, shp, BF16)
    T = nblk * TB
    x = P.inp("x", [T, D])
    oT = P.outp("oT", [D, T], BF16)
    xT_d = P.scratch("xT_d", [D, T], BF16)
    with ExitStack() as st:
        S = Sched(P.nc, st)
        A = Arena(P.nc, st, 211968)
        K = make_consts(P, S, A, st)
        make_consts_a(P, S, A, K)
        prep_xT(P, S, A, K, x, xT_d, ntiles=nblk * 4)
        if "dil" in which:
            mixer_dil(P, S, A, 0, K, xT_d, oT, nblk=nblk)
        if "ret" in which:
            mixer_ret(P, S, A, 0, K, xT_d, oT, nblk=nblk)
        if "rwkv" in which:
            vfirst_d = P.scratch("vfirst_d", [2, 128, T])
            mixer_rwkv(P, S, A, 0, K, xT_d, oT, vfirst_d, nblk=nblk)
        if "mla" in which:
            mixer_mla(P, S, A, 0, K, xT_d, oT, nblk=nblk)
        S.barrier()
        S.wait_all("sp", [])
        S.emit()
        print("instructions:", S.n_instr)
    return P
B_INPUTS = (("w_out", [DEPTH, D, D]), ("ln1_g", [DEPTH, D]), ("ln1_b", [DEPTH, D]), ("ln2_g", [DEPTH, D]), ("ln2_b", [DEPTH, D]),
            ("router_w", [DEPTH, D, NE]), ("router_b", [DEPTH, NE]), ("exp_w_gu", [DEPTH, NE, D, 2 * D]), ("exp_b_gu", [DEPTH, NE, 2 * D]),
            ("exp_w_dn", [DEPTH, NE, D, D]), ("exp_b_dn", [DEPTH, NE, D]))
PER_LAYER = {"w_in", "dil_norm_g", "dil_wperm", "mla_wkr_pad", "mla_w_q_up", "mla_wq_perm", "mla_wk_pad", "mla_wv", "mla_q_norm_g", "mla_kv_norm_g",
             "mla_out_norm_g", "ret_wperm", "ret_norm_g", "rwkv_mu", "rwkv_w0", "rwkv_w_up", "rwkv_a0", "rwkv_a_up", "rwkv_g_up", "rwkv_k_k", "rwkv_k_a",
             "rwkv_r_k", "rwkv_ln_g", "rwkv_ln_b"} | {nm for nm, _ in B_INPUTS}
PER_VLAYER = {"rwkv_vres_down", "rwkv_vres_mu", "rwkv_v0", "rwkv_v_up"}


def build_layer(first, nblk=NB_FULL):
    P = Prog({"wl": (lambda l: 0), "vl": (lambda l: 0)})
    for nm, shp in A_INPUTS + B_INPUTS:
        shp = list(shp)
        if nm in PER_LAYER or nm in PER_VLAYER:
            shp[0] = 1
        P.inp(nm, shp)
    for nm, shp in A_INPUTS_BF:
        P.inp(nm, shp, BF16)
    T = nblk * TB
    ntiles = T // 128
    x = P.inp("x", [T, D])
    out = P.outp("out", [T, D])
    if first:
        vfirst_d = P.outp("vfirst_out", [2, 128, T])
    else:
        vfirst_d = P.inp("vfirst_in", [2, 128, T])
    xT_d = P.scratch("xT_d", [D, T], BF16)
    oT_d = P.scratch("oT_d", [D, T], BF16)
    h1_d = P.scratch("h1_d", [T, D])
    h1T_d = P.scratch("h1T_d", [D, T], BF16)
    gT_d = P.scratch("gT_d", [NE, T])
    l = 0 if first else 1
    with ExitStack() as st:
        S = Sched(P.nc, st)
        A = Arena(P.nc, st, 211968)
        K = make_consts(P, S, A, st)
        make_consts_a(P, S, A, K)
        prep_xT(P, S, A, K, x, xT_d, ntiles=ntiles)
        mixer_ret(P, S, A, l, K, xT_d, oT_d, nblk=nblk)
        mixer_dil(P, S, A, l, K, xT_d, oT_d, nblk=nblk)
        mixer_rwkv(P, S, A, l, K, xT_d, oT_d, vfirst_d, nblk=nblk)
        mixer_mla(P, S, A, l, K, xT_d, oT_d, nblk=nblk)
        phase_b(P, S, A, l, K, oT_d, x, out, None, h1_d, h1T_d, gT_d, ntiles=ntiles, npass_tiles=min(8, ntiles))
        S.barrier()
        S.emit()
        P.n_instr = S.n_instr
    return P


_CACHE = {}


def layer_inputs(l, small, hc, hl):
    m = {}
    for nm, shp in list(A_INPUTS) + list(B_INPUTS) + list(A_INPUTS_BF):
        if nm in hc:
            m[nm] = hc[nm]
            continue
        a = hl[nm] if nm in hl else small[nm]
        if nm in PER_LAYER:
            a = np.ascontiguousarray(a[l:l + 1])
        elif nm in PER_VLAYER:
            j = max(l - 1, 0)
            a = np.ascontiguousarray(a[j:j + 1])
        m[nm] = a
    return m


def kernel(**inputs):
    n_cores = 8
    small = {k: np.asarray(v, dtype=np.float32) for k, v in inputs.items() if k != "x"}
    x = np.asarray(inputs["x"], dtype=np.float32)
    if "P0" not in _CACHE:
        _CACHE["P0"] = build_layer(True)
        _CACHE["P1"] = build_layer(False)
        _CACHE["hc"] = host_consts()
    hc = _CACHE["hc"]
    hl = host_layouts(small)
    cur = [np.ascontiguousarray(x[c % 4]) for c in range(n_cores)]
    vfirst = None
    for l in range(DEPTH):
        P = _CACHE["P0"] if l == 0 else _CACHE["P1"]
        shared = layer_inputs(l, small, hc, hl)
        in_maps = []
        for c in range(n_cores):
            m = dict(shared)
            m["x"] = cur[c]
            if l > 0:
                m["vfirst_in"] = vfirst[c]
            in_maps.append(m)
        res = run_bass_kernel_spmd(P.nc, in_maps, core_ids=list(range(n_cores)))
        cur = [np.asarray(res.results[c]["out"], dtype=np.float32) for c in range(n_cores)]
        if l == 0:
            vfirst = [np.asarray(res.results[c]["vfirst_out"], dtype=np.float32) for c in range(n_cores)]
    return np.stack(cur[:4], axis=0)
```

```python
import math
import numpy as np
from contextlib import ExitStack
import concourse.bass as bass
import concourse.mybir as mybir
from concourse.bass_utils import run_bass_kernel_spmd
from concourse.alu_op_type import AluOpType as ALU

F32 = mybir.dt.float32
BF16 = mybir.dt.bfloat16
AF = mybir.ActivationFunctionType
AX = mybir.AxisListType

D = 1024
SEQ = 8192
DEPTH = 4
NE = 32
ALPHA = (2 * DEPTH) ** 0.25
LN_EPS = 1e-5
NT = SEQ // 128
N_IN = 2976


class Sched:
    ENG = ("pe", "dve", "act", "pool", "sp")
    SEM_MAX = 30000
    NDMA = 32

    def __init__(self, nc, stack):
        self.nc = nc
        self.stack = stack
        self.lists = {e: [] for e in self.ENG}
        self.cur_sem = {}
        self.cnt = {}
        self.nsem = 0
        for e in self.ENG:
            if e != "sp":
                self._new_eng_sem(e)
        self.dma_sems = [self._alloc_sem(f"dma{i}") for i in range(self.NDMA)]
        self.dma_uses = [0] * self.NDMA
        self.dma_rr = 0
        self.seen = {e: {} for e in self.ENG}
        self.res = {}
        self.n_instr = 0

    def _alloc_sem(self, name):
        self.nsem += 1
        return self.stack.enter_context(self.nc.semaphore(name))

    def _new_eng_sem(self, e):
        self.cur_sem[e] = self._alloc_sem(f"c_{e}_{self.nsem}")
        self.cnt[e] = 0

    def _waits_for(self, e, reads, writes):
        need = {}

        def add(ev):
            if ev is None:
                return
            s, v = ev
            k = id(s)
            if self.seen[e].get(k, 0) >= v:
                return
            if k not in need or need[k][1] < v:
                need[k] = (s, v)
        for r in reads:
            st = self.res.get(r)
            if st:
                add(st["w"])
        for w in writes:
            st = self.res.get(w)
            if st:
                add(st["w"])
                for ev in st["r"]:
                    add(ev)
        out = list(need.values())
        for s, v in out:
            self.seen[e][id(s)] = v
        return out

    def _commit(self, ev, reads, writes):
        for r in reads:
            st = self.res.setdefault(r, {"w": None, "r": []})
            st["r"].append(ev)
            if len(st["r"]) > 16:
                best = {}
                for s, v in st["r"]:
                    if id(s) not in best or best[id(s)][1] < v:
                        best[id(s)] = (s, v)
                st["r"] = list(best.values())
        for w in writes:
            self.res[w] = {"w": ev, "r": []}

    def op(self, e, fn, reads=(), writes=()):
        if self.cnt[e] >= self.SEM_MAX:
            self._new_eng_sem(e)
        waits = self._waits_for(e, reads, writes)
        self.cnt[e] += 1
        sem, val = self.cur_sem[e], self.cnt[e]
        self.lists[e].append((waits, fn, sem, 1))
        if e == "pe":
            self.seen[e][id(sem)] = val
        self._commit((sem, val), reads, writes)
        self.n_instr += 1

    def dma(self, e, fn, reads=(), writes=()):
        i = self.dma_rr
        self.dma_rr = (self.dma_rr + 1) % self.NDMA
        sem = self.dma_sems[i]
        waits = self._waits_for(e, reads, writes)
        prev = self.dma_uses[i] * 16
        if prev and self.seen[e].get(id(sem), 0) < prev:
            waits.append((sem, prev))
            self.seen[e][id(sem)] = prev
        self.dma_uses[i] += 1
        val = self.dma_uses[i] * 16
        self.lists[e].append((waits, fn, sem, 16))
        self._commit((sem, val), reads, writes)
        self.n_instr += 1

    def wait_all(self, e, keys):
        waits = self._waits_for(e, list(keys), [])
        self.lists[e].append((waits, None, None, 0))

    def barrier(self):
        evs = [(self.cur_sem[e], self.cnt[e]) for e in self.ENG if e != "sp" and self.cnt[e] > 0]
        evs += [(s, u * 16) for s, u in zip(self.dma_sems, self.dma_uses) if u > 0]
        for e in self.ENG:
            waits = []
            for s, v in evs:
                if self.seen[e].get(id(s), 0) < v:
                    waits.append((s, v))
                    self.seen[e][id(s)] = v
            self.lists[e].append((waits, None, None, 0))

    def emit(self):
        nc = self.nc
        with nc.Block() as block:
            def run(name):
                def body(eng):
                    for waits, fn, sem, inc in self.lists[name]:
                        for s, v in waits:
                            eng.wait_ge(s, v)
                        if fn is not None:
                            fn(eng).then_inc(sem, inc)
                return body
            block.sync(run("sp"))
            block.tensor(run("pe"))
            block.vector(run("dve"))
            block.scalar(run("act"))
            block.gpsimd(run("pool"))


class Prog:
    def __init__(self, cfg):
        self.cfg = cfg
        self.nc = bass.Bass("TRN2", target_bir_lowering=False)
        self.din = {}
        self.dout = {}
        self.wl = cfg.get("wl", lambda l: l)
        self.vl = cfg.get("vl", lambda l: l - 1)

    def inp(self, name, shape, dt=F32):
        t = self.nc.dram_tensor(name, list(shape), dt, kind="ExternalInput").ap()
        self.din[name] = t
        return t

    def outp(self, name, shape, dt=F32):
        t = self.nc.dram_tensor(name, list(shape), dt, kind="ExternalOutput").ap()
        self.dout[name] = t
        return t

    def scratch(self, name, shape, dt=F32):
        return self.nc.dram_tensor(name, list(shape), dt).ap()


def bcast_rows(ap_1d, n, parts=128):
    return ap_1d.rearrange("(o n) -> o n", o=1).broadcast_to([parts, n])


class Arena:
    def __init__(self, nc, stack, nbytes):
        self.nf = nbytes // 4
        self.t = stack.enter_context(nc.sbuf_tensor("arena", [128, self.nf], F32))
        self.off = 0

    def alloc(self, shape, dt=F32, parts=None):
        p = shape[0]
        n = 1
        for d in shape[1:]:
            n *= d
        esz = 2 if dt == BF16 else 4
        nf = (n * esz + 3) // 4
        nf = (nf + 15) // 16 * 16
        assert self.off + nf <= self.nf, f"arena overflow: need {nf * 4}B at {self.off * 4} of {self.nf * 4}"
        ap = self.t[0:p, self.off:self.off + nf]
        self.off += nf
        if dt == BF16:
            ap = ap.bitcast(BF16)
        ap = ap[:, 0:n]
        if len(shape) == 3:
            ap = ap.rearrange("p (a b) -> p a b", a=shape[1])
        elif len(shape) == 4:
            ap = ap.rearrange("p (a b c) -> p a b c", a=shape[1], b=shape[2])
        return ap

    def mark(self):
        return self.off

    def release(self, m):
        self.off = m


def phase_b(P, S, A, l, K, oT_d, xres_d, xnext_d, xTnext_d, h1_d, h1T_d, gT_d, ntiles=NT, npass_tiles=8, do_b2=True, nexp=NE):
    import os
    SQ = os.environ.get("STORE_Q", "sp")
    FAST = os.environ.get("MOE_FAST", "1") == "1"
    nc = P.nc
    W = P.din
    ident = K["ident"]
    ps = K["ps"]
    m0 = A.mark()

    def sb(name, shape, dt=F32):
        return A.alloc(list(shape), dt)

    def load_const(t, src, nm):
        S.dma("sp", lambda e: e.dma_start(out=t, in_=src), writes=[nm])

    bdn = sb("bdn", [NE, D]); load_const(bdn, W["exp_b_dn"][P.wl(l)], "bdn")
    bgu = sb("bgu", [128, NE, 8, 2])
    with nc.allow_non_contiguous_dma(reason="tiny bias gather"):
        for e_ in range(NE):
            S.dma("sp", lambda e, e_=e_: e.dma_start(out=bgu[:, e_, :, :], in_=W["exp_b_gu"][P.wl(l), e_].rearrange("(t p two) -> p t two", p=128, two=2)),
                  writes=[f"bgu{e_}"])
    gates = sb("gates", [128, ntiles, NE])
    stg = (list(K["wstg"]) if "wstg" in K else []) + [sb("stg_extra", [128, 2048]) for i in range(1 if "wstg" in K else 3)]
    stg_i = [0]

    def next_stg():
        i = stg_i[0] % 3
        stg_i[0] += 1
        return i

    def layer_norm_tile(r, g, b, out, tag, gname):
        stats = K["stats"]; mv = K["mv"]; sd = K["sd"]
        for hlf in range(2):
            S.op("dve", lambda e, hlf=hlf: e.bn_stats(out=stats[:, hlf, :], in_=r[:, hlf * 512:(hlf + 1) * 512]), reads=[tag + "r"], writes=["stats"])
        S.op("dve", lambda e: e.bn_aggr(out=mv, in_=stats.rearrange("p a b -> p (a b)")), reads=["stats"], writes=["mv"])
        S.op("act", lambda e: e.activation(out=sd[:, 0:1], in_=mv[:, 1:2], func=AF.Sqrt, bias=K["eps_ln"][:, 0:1], scale=1.0), reads=["mv", "kc"], writes=["sd"])
        S.op("dve", lambda e: e.reciprocal(out=sd[:, 1:2], in_=sd[:, 0:1]), reads=["sd"], writes=["sd"])
        S.op("dve", lambda e: e.tensor_scalar(out=r, in0=r, scalar1=mv[:, 0:1], scalar2=sd[:, 1:2], op0=ALU.subtract, op1=ALU.mult),
             reads=[tag + "r", "mv", "sd"], writes=[tag + "r"])
        S.op("pool", lambda e: e.tensor_tensor(out=r, in0=r, in1=g, op=ALU.mult), reads=[tag + "r", gname + "g"], writes=[tag + "r"])
        S.op("dve", lambda e: e.tensor_tensor(out=out, in0=r, in1=b, op=ALU.add), reads=[tag + "r", gname + "b"], writes=[tag + "o"])

    def transpose_to_T(src, tag_src, dst_bf, tag_bf, dst_f32=None, tag_f32=None):
        for hlf in range(2):
            pb = ps[6 + hlf]
            for c4 in range(4):
                c = hlf * 4 + c4
                S.op("pe", lambda e, c=c, c4=c4, pb=pb: e.transpose(out=pb[:, c4 * 128:(c4 + 1) * 128], in_=src[:, c * 128:(c + 1) * 128], identity=ident),
                     reads=[tag_src, "kc"], writes=[f"ps{6 + hlf}"])
            S.op("dve", lambda e, hlf=hlf, pb=pb: e.tensor_copy(out=dst_bf[:, hlf * 4:(hlf + 1) * 4, :], in_=pb[:].rearrange("p (c t) -> p c t", c=4)),
                 reads=[f"ps{6 + hlf}"], writes=[tag_bf])
            if dst_f32 is not None and os.environ.get("T_ACT", "1") == "1":
                S.op("dve", lambda e, hlf=hlf, pb=pb: e.tensor_copy(out=dst_f32[:, hlf * 4:(hlf + 1) * 4, :], in_=pb[:].rearrange("p (c t) -> p c t", c=4)),
                     reads=[f"ps{6 + hlf}"], writes=[tag_f32])

    m1 = A.mark()
    wout_bf = sb("wout_bf", [128, 8, D], BF16)
    g1 = sb("g1", [128, D]); b1 = sb("b1", [128, D])
    rw = sb("rw", [128, 8, NE]); rb = sb("rb", [128, NE])
    for c in range(4):
        i = next_stg()
        S.dma("sp", lambda e, i=i, c=c: e.dma_start(out=stg[i].rearrange("p (c d) -> p c d", c=2),
              in_=W["w_out"][P.wl(l), c * 256:(c + 1) * 256, :].rearrange("(c p) d -> p c d", p=128)), writes=[f"stg{i}"])
        S.op("pool", lambda e, i=i, c=c: e.tensor_copy(out=wout_bf[:, 2 * c:2 * c + 2, :], in_=stg[i].rearrange("p (c d) -> p c d", c=2)),
             reads=[f"stg{i}"], writes=["wout_bf"])
    load_const(g1, bcast_rows(W["ln1_g"][P.wl(l)], D), "ln1g")
    load_const(b1, bcast_rows(W["ln1_b"][P.wl(l)], D), "ln1b")
    load_const(rw, W["router_w"][P.wl(l)].rearrange("(c p) n -> p c n", p=128), "rw")
    load_const(rb, bcast_rows(W["router_b"][P.wl(l)], NE), "rb")
    oT_t = [sb("oT_t", [128, 8, 128], BF16) for i in range(2)]
    x_t = [sb("x_t", [128, D]) for i in range(2)]
    r_t = sb("r_t", [128, D])
    h1_t = [sb("h1_t", [128, D]) for i in range(2)]
    hT_bf = [sb("hT_bf", [128, 8, 128], BF16) for i in range(2)]
    hT_f = sb("hT_f", [128, 8, 128])
    lg = sb("lg", [128, NE]); ex = sb("ex", [128, NE]); mk = sb("mk", [128, NE])
    top8 = sb("top8", [128, 8]); sm = sb("sm", [128, 4])
    gT_t = [sb("gT_t", [NE, 128]) for i in range(2)]

    for n in range(ntiles):
        b = n % 2
        S.dma("sp", lambda e, n=n, b=b: e.dma_start(out=oT_t[b], in_=oT_d[:, n * 128:(n + 1) * 128].rearrange("(c p) t -> p c t", p=128)),
              reads=[f"oT_d{n}"], writes=[f"oT_t{b}"])
        S.dma("sp", lambda e, n=n, b=b: e.dma_start(out=x_t[b], in_=xres_d[n * 128:(n + 1) * 128, :]), reads=[f"xres_d{n}"], writes=[f"x_t{b}"])
        for db in range(2):
            for c in range(8):
                S.op("pe", lambda e, c=c, db=db, b=b: e.matmul(ps[db][:], lhsT=oT_t[b][:, c, :], rhs=wout_bf[:, c, db * 512:(db + 1) * 512], start=(c == 0), stop=(c == 7)),
                     reads=[f"oT_t{b}", "wout_bf"], writes=[f"ps{db}"])
            S.op("dve", lambda e, db=db, b=b: e.scalar_tensor_tensor(out=r_t[:, db * 512:(db + 1) * 512], in0=x_t[b][:, db * 512:(db + 1) * 512], scalar=ALPHA,
                                                                      in1=ps[db][:], op0=ALU.mult, op1=ALU.add),
                 reads=[f"x_t{b}", f"ps{db}"], writes=["B1r"])
        import os
        STOP = int(os.environ.get("B1_STOP", "99"))
        if STOP <= 1:
            S.dma(SQ, lambda e, n=n, b=b: e.dma_start(out=h1_d[n * 128:(n + 1) * 128, :], in_=r_t), reads=["B1r"], writes=[f"h1_d{n}"])
            continue
        layer_norm_tile(r_t, g1, b1, h1_t[b], "B1", "ln1")
        S.dma(SQ, lambda e, n=n, b=b: e.dma_start(out=h1_d[n * 128:(n + 1) * 128, :], in_=h1_t[b]), reads=["B1o"], writes=[f"h1_d{n}"])
        if STOP <= 2:
            continue
        transpose_to_T(h1_t[b], "B1o", hT_bf[b], f"hT_bf{b}", hT_f, "hT_f")
        if os.environ.get("T_STORE", "1") == "1":
            S.dma(SQ, lambda e, n=n, b=b: e.dma_start(out=h1T_d[:, n * 128:(n + 1) * 128].rearrange("(c p) t -> p c t", p=128), in_=hT_bf[b]),
                  reads=[f"hT_bf{b}"], writes=[f"h1T_d{n}"])
        if STOP <= 3:
            continue
        for c in range(8):
            S.op("pe", lambda e, c=c: e.matmul(ps[2][:, 0:NE], lhsT=hT_f[:, c, :], rhs=rw[:, c, :], start=(c == 0), stop=(c == 7)),
                 reads=["hT_f", "rw"], writes=["ps2"])
        S.op("dve", lambda e: e.tensor_tensor(out=lg, in0=ps[2][:, 0:NE], in1=rb, op=ALU.add), reads=["ps2", "rb"], writes=["lg"])
        S.op("dve", lambda e: e.max(out=top8, in_=lg), reads=["lg"], writes=["top8"])
        S.op("dve", lambda e: e.tensor_scalar(out=sm[:, 0:1], in0=top8[:, 0:1], scalar1=-1.0, scalar2=None, op0=ALU.mult), reads=["top8"], writes=["sm"])
        S.op("act", lambda e: e.activation(out=ex, in_=lg, func=AF.Exp, bias=sm[:, 0:1], scale=1.0), reads=["lg", "sm"], writes=["ex"])
        S.op("dve", lambda e: e.tensor_scalar(out=mk, in0=lg, scalar1=top8[:, 3:4], scalar2=None, op0=ALU.is_ge), reads=["lg", "top8"], writes=["mk"])
        S.op("dve", lambda e: e.tensor_tensor(out=ex, in0=ex, in1=mk, op=ALU.mult), reads=["ex", "mk"], writes=["ex"])
        S.op("dve", lambda e: e.tensor_reduce(out=sm[:, 1:2], in_=ex, axis=AX.X, op=ALU.add), reads=["ex"], writes=["sm"])
        S.op("dve", lambda e: e.reciprocal(out=sm[:, 2:3], in_=sm[:, 1:2]), reads=["sm"], writes=["sm"])
        S.op("dve", lambda e, n=n: e.tensor_scalar(out=gates[:, n, :], in0=ex, scalar1=sm[:, 2:3], scalar2=None, op0=ALU.mult), reads=["ex", "sm"], writes=["gates"])
        if STOP <= 4:
            continue
        S.op("pe", lambda e, n=n: e.transpose(out=ps[3][0:NE, 0:128], in_=gates[:, n, :], identity=ident), reads=["gates", "kc"], writes=["ps3"])
        S.op("act", lambda e, b=b: e.copy(out=gT_t[b], in_=ps[3][0:NE, 0:128]), reads=["ps3"], writes=[f"gT_t{b}"])
        S.dma(SQ, lambda e, n=n, b=b: e.dma_start(out=gT_d[:, n * 128:(n + 1) * 128], in_=gT_t[b]), reads=[f"gT_t{b}"], writes=[f"gT_d{n}"])

    S.barrier()
    A.release(m1)
    if not do_b2:
        A.release(m0)
        return
    PT = npass_tiles
    PW = PT * 128
    npass = ntiles // PT
    NU = 2
    g2 = sb("g2", [128, D]); b2 = sb("b2", [128, D])
    load_const(g2, bcast_rows(W["ln2_g"][P.wl(l)], D), "ln2g")
    load_const(b2, bcast_rows(W["ln2_b"][P.wl(l)], D), "ln2b")
    hT_p = sb("hT_p", [128, 8, PW], BF16)
    yacc = sb("yacc", [128, PT, D])
    gT_p = sb("gT_p", [NE, PW])
    wgu = [sb("wgu", [128, 8, 1024], BF16) for i in range(NU)]
    wdn = [sb("wdn", [128, 4, D], BF16) for i in range(NU)]
    actT = [sb("actT", [128, 4, 512], BF16) for i in range(2)]
    xg = [sb("xg", [128, 512]) for i in range(2)]
    sg = [sb("sg", [128, 512]) for i in range(2)]
    xl = [sb("xl", [128, 512]) for i in range(2)]
    h1r = sb("h1r", [128, D])
    r2 = sb("r2", [128, D])
    xn = sb("xn", [128, D])
    xnT = sb("xnT", [128, 8, 128], BF16)
    ui = 0
    gcnt = 0
    acnt = 0
    for p in range(npass):
        tiles = list(range(p * PT, (p + 1) * PT))
        S.dma("sp", lambda e, p=p: e.dma_start(out=hT_p, in_=h1T_d[:, p * PW:(p + 1) * PW].rearrange("(c p) t -> p c t", p=128)),
              reads=[f"h1T_d{n}" for n in tiles], writes=["hT_p"])
        S.dma("sp", lambda e, p=p: e.dma_start(out=gT_p, in_=gT_d[:, p * PW:(p + 1) * PW]), reads=[f"gT_d{n}" for n in tiles], writes=["gT_p"])
        for ex_ in range(nexp):
            for hf in range(2):
                u = ui % NU
                ui += 1
                for c2 in range(4):
                    i = next_stg()
                    S.dma("sp", lambda e, i=i, c2=c2, ex_=ex_, hf=hf: e.dma_start(out=stg[i].rearrange("p (c f) -> p c f", c=2),
                          in_=W["exp_w_gu"][P.wl(l), ex_, c2 * 256:(c2 + 1) * 256, hf * 1024:(hf + 1) * 1024].rearrange("(c p) f -> p c f", p=128)), writes=[f"stg{i}"])
                    if FAST:
                        S.op("act", lambda e, i=i, c2=c2, u=u: e.activation(out=wgu[u][:, 2 * c2:2 * c2 + 2, :].rearrange("p c (two f) -> p c two f", two=2),
                                                                             in_=stg[i].rearrange("p (c f two) -> p c two f", c=2, two=2), func=AF.Identity),
                             reads=[f"stg{i}"], writes=[f"wgu{u}"])
                    else:
                        S.op("pool", lambda e, i=i, c2=c2, u=u: e.tensor_copy(out=wgu[u][:, 2 * c2:2 * c2 + 2, :].rearrange("p c (two f) -> p c two f", two=2),
                                                                                in_=stg[i].rearrange("p (c f two) -> p c two f", c=2, two=2)),
                             reads=[f"stg{i}"], writes=[f"wgu{u}"])
                for c2 in range(2):
                    i = next_stg()
                    S.dma("sp", lambda e, i=i, c2=c2, ex_=ex_, hf=hf: e.dma_start(out=stg[i].rearrange("p (c d) -> p c d", c=2),
                          in_=W["exp_w_dn"][P.wl(l), ex_, hf * 512 + c2 * 256:hf * 512 + (c2 + 1) * 256, :].rearrange("(c p) d -> p c d", p=128)), writes=[f"stg{i}"])
                    S.op("dve" if FAST else "pool", lambda e, i=i, c2=c2, u=u: e.tensor_copy(out=wdn[u][:, 2 * c2:2 * c2 + 2, :], in_=stg[i].rearrange("p (c d) -> p c d", c=2)),
                         reads=[f"stg{i}"], writes=[f"wdn{u}"])
                first = (ex_ == 0 and hf == 0)
                for tb in range(PW // 512):
                    ab = acnt % 2
                    acnt += 1
                    for f4 in range(4):
                        ft = hf * 4 + f4
                        gb = gcnt % 2
                        gcnt += 1
                        pg, pl = ps[gb * 2], ps[gb * 2 + 1]
                        for c in range(8):
                            S.op("pe", lambda e, c=c, f4=f4, u=u, tb=tb, pg=pg: e.matmul(pg[:], lhsT=wgu[u][:, c, f4 * 128:(f4 + 1) * 128], rhs=hT_p[:, c, tb * 512:(tb + 1) * 512],
                                                                                       start=(c == 0), stop=(c == 7)),
                                 reads=[f"wgu{u}", "hT_p"], writes=[f"ps{gb * 2}"])
                        for c in range(8):
                            S.op("pe", lambda e, c=c, f4=f4, u=u, tb=tb, pl=pl: e.matmul(pl[:], lhsT=wgu[u][:, c, 512 + f4 * 128:512 + (f4 + 1) * 128], rhs=hT_p[:, c, tb * 512:(tb + 1) * 512],
                                                                                       start=(c == 0), stop=(c == 7)),
                                 reads=[f"wgu{u}", "hT_p"], writes=[f"ps{gb * 2 + 1}"])
                        S.op("dve", lambda e, gb=gb, pg=pg, ex_=ex_, ft=ft: e.tensor_scalar(out=xg[gb], in0=pg[:], scalar1=bgu[:, ex_, ft, 0:1], scalar2=7.0, op0=ALU.add, op1=ALU.min),
                             reads=[f"ps{gb * 2}", f"bgu{ex_}"], writes=[f"xg{gb}"])
                        S.op("act", lambda e, gb=gb: e.activation(out=sg[gb], in_=xg[gb], func=AF.Sigmoid, scale=1.702), reads=[f"xg{gb}"], writes=[f"sg{gb}"])
                        S.op("dve", lambda e, gb=gb, pl=pl, ex_=ex_, ft=ft: e.tensor_scalar(out=xl[gb], in0=pl[:], scalar1=bgu[:, ex_, ft, 1:2], scalar2=7.0, op0=ALU.add, op1=ALU.min),
                             reads=[f"ps{gb * 2 + 1}", f"bgu{ex_}"], writes=[f"xl{gb}"])
                        S.op("dve" if FAST else "pool", lambda e, gb=gb: e.tensor_scalar(out=xl[gb], in0=xl[gb], scalar1=-7.0, scalar2=1.0, op0=ALU.max, op1=ALU.add),
                             reads=[f"xl{gb}"], writes=[f"xl{gb}"])
                        S.op("dve" if FAST else "pool", lambda e, gb=gb: e.tensor_tensor(out=xg[gb], in0=xg[gb], in1=xl[gb], op=ALU.mult), reads=[f"xg{gb}", f"xl{gb}"], writes=[f"xg{gb}"])
                        S.op("dve", lambda e, gb=gb, ab=ab, f4=f4: e.tensor_tensor(out=actT[ab][:, f4, :], in0=xg[gb], in1=sg[gb], op=ALU.mult),
                             reads=[f"xg{gb}", f"sg{gb}"], writes=[f"actT{ab}"])
                    for tt in range(4):
                        tl = tb * 4 + tt
                        tg = p * PT + tl
                        for db in range(2):
                            pd = ps[4 + db]
                            for f4 in range(4):
                                S.op("pe", lambda e, f4=f4, tt=tt, db=db, ab=ab, u=u, pd=pd: e.matmul(pd[:], lhsT=actT[ab][:, f4, tt * 128:(tt + 1) * 128], rhs=wdn[u][:, f4, db * 512:(db + 1) * 512],
                                                                                                    start=(f4 == 0), stop=(f4 == 3)),
                                     reads=[f"actT{ab}", f"wdn{u}"], writes=[f"ps{4 + db}"])
                            if first:
                                S.op("dve", lambda e, tl=tl, tg=tg, db=db, pd=pd, ex_=ex_: e.tensor_scalar(out=yacc[:, tl, db * 512:(db + 1) * 512], in0=pd[:], scalar1=gates[:, tg, ex_:ex_ + 1],
                                                                                                       scalar2=None, op0=ALU.mult),
                                     reads=[f"ps{4 + db}", "gates"], writes=[f"yacc{tl}"])
                            else:
                                S.op("dve", lambda e, tl=tl, tg=tg, db=db, pd=pd, ex_=ex_: e.scalar_tensor_tensor(out=yacc[:, tl, db * 512:(db + 1) * 512], in0=pd[:], scalar=gates[:, tg, ex_:ex_ + 1],
                                                                                                              in1=yacc[:, tl, db * 512:(db + 1) * 512], op0=ALU.mult, op1=ALU.add),
                                     reads=[f"ps{4 + db}", "gates", f"yacc{tl}"], writes=[f"yacc{tl}"])
        for tl in range(PT):
            tg = p * PT + tl
            S.dma("sp", lambda e, tg=tg: e.dma_start(out=h1r, in_=h1_d[tg * 128:(tg + 1) * 128, :]), reads=[f"h1_d{tg}"], writes=["h1r"])
            for db in range(2):
                S.op("pe", lambda e, tl=tl, db=db: e.matmul(ps[4 + db][:], lhsT=gT_p[:, tl * 128:(tl + 1) * 128], rhs=bdn[:, db * 512:(db + 1) * 512], start=True, stop=True),
                     reads=["gT_p", "bdn"], writes=[f"ps{4 + db}"])
                S.op("dve", lambda e, tl=tl, db=db: e.tensor_tensor(out=yacc[:, tl, db * 512:(db + 1) * 512], in0=yacc[:, tl, db * 512:(db + 1) * 512], in1=ps[4 + db][:], op=ALU.add),
                     reads=[f"ps{4 + db}", f"yacc{tl}"], writes=[f"yacc{tl}"])
            S.op("dve", lambda e, tl=tl: e.scalar_tensor_tensor(out=r2, in0=h1r, scalar=ALPHA, in1=yacc[:, tl, :], op0=ALU.mult, op1=ALU.add),
                 reads=["h1r", f"yacc{tl}"], writes=["B2r"])
            layer_norm_tile(r2, g2, b2, xn, "B2", "ln2")
            S.dma(SQ, lambda e, tg=tg: e.dma_start(out=xnext_d[tg * 128:(tg + 1) * 128, :], in_=xn), reads=["B2o"], writes=[f"xnext_d{tg}"])
            if xTnext_d is not None:
                transpose_to_T(xn, "B2o", xnT, "xnT")
                S.dma(SQ, lambda e, tg=tg: e.dma_start(out=xTnext_d[:, tg * 128:(tg + 1) * 128].rearrange("(c p) t -> p c t", p=128), in_=xnT),
                      reads=["xnT"], writes=[f"xTnext_d{tg}"])
    S.barrier()
    A.release(m0)


def make_consts(P, S, A, st):
    nc = P.nc
    K = {}
    K["ident"] = A.alloc([128, 128])
    K["eps_ln"] = A.alloc([128, 1])
    K["stats"] = A.alloc([128, 2, 6])
    K["mv"] = A.alloc([128, 2])
    K["sd"] = A.alloc([128, 2])
    K["ps"] = [st.enter_context(nc.psum_tensor(f"ps{i}", [128, 512], F32)) for i in range(8)]
    K["ps_bf"] = K["ps"][3][:, 256:512].bitcast(BF16)
    S.dma("sp", lambda e: e.dma_start(out=K["ident"], in_=P.din["c_ident"]), writes=["kc"])
    S.op("dve", lambda e: e.memset(K["eps_ln"], LN_EPS), writes=["kc2"])
    return K


def build_phase_b_test(ntiles, do_b2=True, nexp=NE):
    P = Prog({})
    for nm, shp in (("w_out", [DEPTH, D, D]), ("ln1_g", [DEPTH, D]), ("ln1_b", [DEPTH, D]), ("ln2_g", [DEPTH, D]), ("ln2_b", [DEPTH, D]),
                    ("router_w", [DEPTH, D, NE]), ("router_b", [DEPTH, NE]), ("exp_w_gu", [1, nexp, D, 2 * D]), ("exp_b_gu", [DEPTH, NE, 2 * D]),
                    ("exp_w_dn", [1, nexp, D, D]), ("exp_b_dn", [DEPTH, NE, D]), ("c_ident", [128, 128])):
        P.inp(nm, shp)
    T = ntiles * 128
    oT = P.inp("oT", [D, T], BF16)
    xres = P.inp("xres", [T, D])
    xnext = P.outp("xnext", [T, D])
    h1o = P.outp("h1o", [T, D])
    xTn = P.scratch("xTn", [D, T], BF16)
    h1T_d = P.scratch("h1T_d", [D, T], BF16)
    gT_d = P.scratch("gT_d", [NE, T])
    with ExitStack() as st:
        S = Sched(P.nc, st)
        A = Arena(P.nc, st, 211968)
        K = make_consts(P, S, A, st)
        phase_b(P, S, A, 0, K, oT, xres, xnext, xTn, h1o, h1T_d, gT_d, ntiles=ntiles, npass_tiles=min(8, ntiles), do_b2=do_b2, nexp=nexp)
        S.wait_all("sp", [f"xnext_d{n}" for n in range(ntiles)] + [f"h1_d{n}" for n in range(ntiles)] + [f"xTnext_d{n}" for n in range(ntiles)] + [f"gT_d{n}" for n in range(ntiles)] + [f"h1T_d{n}" for n in range(ntiles)])
        S.emit()
        print("instructions:", S.n_instr)
    return P
TB = 512
NB_FULL = SEQ // TB


def load_w_bf16(S, A, K, src_ap, ncols, name, kchunks=8):
    wb = A.alloc([128, kchunks, ncols], BF16)
    stg = K["wstg"]
    step = 2048 // ncols if ncols <= 2048 else 0
    assert ncols <= 2048
    per = max(1, min(kchunks, 2048 // ncols))
    c = 0
    while c < kchunks:
        n = min(per, kchunks - c)
        i = K["wstg_i"][0] % 2
        K["wstg_i"][0] += 1
        S.dma("sp", lambda e, i=i, c=c, n=n: e.dma_start(out=stg[i][:, 0:n * ncols].rearrange("p (c f) -> p c f", c=n),
              in_=src_ap[c * 128:(c + n) * 128, :].rearrange("(c p) f -> p c f", p=128)), writes=[f"wstg{i}"])
        S.op("pool", lambda e, i=i, c=c, n=n: e.tensor_copy(out=wb[:, c:c + n, :], in_=stg[i][:, 0:n * ncols].rearrange("p (c f) -> p c f", c=n)),
             reads=[f"wstg{i}"], writes=[name])
        c += n
    return wb


def proj_fm(S, ps_t, ps_name, M, wb, wname, col0, xT, xname, K8=8, start=True, stop=True, n=TB):
    for c in range(K8):
        S.op("pe", lambda e, c=c: e.matmul(ps_t[0:M, 0:n], lhsT=wb[:, c, col0:col0 + M], rhs=xT[:, c, 0:n], start=(start and c == 0), stop=(stop and c == K8 - 1)),
             reads=[wname, xname], writes=[ps_name])


def rstd_from_ss(S, K, ss_ps, ss_name, rows, n, scale, eps_name, out, out_name):
    S.op("act", lambda e: e.activation(out=out[0:rows, 0:n], in_=ss_ps[0:rows, 0:n], func=AF.Sqrt, bias=K[eps_name][0:rows, 0:1], scale=scale),
         reads=[ss_name, "kc2"], writes=[out_name])
    S.op("dve", lambda e: e.reciprocal(out=out[0:rows, 0:n], in_=out[0:rows, 0:n]), reads=[out_name], writes=[out_name])


def attention_block(S, K, qb, heads, qT, qname, kT, kname, Vp, vname, masks, mask_lo, kt_lo, Pbuf, oacc, oname, ps, ring=None):
    kt_hi = 4 * qb + 3
    cnt = K["attn_cnt"]
    for h in heads:
        po = ps[4 + (h % 2)]
        first = True
        for kt in range(kt_lo, kt_hi + 1):
            d = 4 * qb - kt
            ks = kt if ring is None else kt % ring
            pb = cnt[0] % 3
            cnt[0] += 1
            S.op("pe", lambda e, h=h, ks=ks, pb=pb: e.matmul(ps[pb][:, 0:TB], lhsT=kT[h][:, ks * 128:(ks + 1) * 128], rhs=qT[h][:, 0:TB], start=True, stop=True),
                 reads=[kname, f"{qname}{h}"], writes=[f"ps{pb}"])
            S.op("act", lambda e, pb=pb: e.activation(out=Pbuf[pb], in_=ps[pb][:, 0:TB], func=AF.Exp), reads=[f"ps{pb}"], writes=[f"P{pb}"])
            if d <= mask_lo:
                eng = "pool" if (cnt[0] % 2) else "dve"
                mi = d + 3
                S.op(eng, lambda e, pb=pb, mi=mi: e.tensor_tensor(out=Pbuf[pb], in0=Pbuf[pb], in1=masks[:, mi, :], op=ALU.mult), reads=[f"P{pb}", "masks"], writes=[f"P{pb}"])
            S.op("pe", lambda e, h=h, ks=ks, pb=pb, po=po, first=first, last=(kt == kt_hi): e.matmul(po[0:65, 0:TB], lhsT=Vp[h][:, ks, :], rhs=Pbuf[pb], start=first, stop=last),
                 reads=[vname, f"P{pb}"], writes=[f"ps{4 + (h % 2)}"])
            first = False
        pn = f"ps{4 + (h % 2)}"
        rs = K["rs_row"]
        S.op("dve", lambda e, po=po: e.reciprocal(out=rs[64:65, 0:TB], in_=po[64:65, 0:TB]), reads=[pn], writes=["rs_row"])
        S.op("pe", lambda e: e.matmul(ps[3][0:64, 0:TB], lhsT=K["ones_f"][64:65, 0:64], rhs=rs[64:65, 0:TB], start=True, stop=True), reads=["rs_row", "kc3"], writes=["ps3"])
        S.op("act", lambda e, h=h, po=po: e.activation(out=oacc[h], in_=po[0:64, 0:TB], func=AF.Identity), reads=[pn], writes=[f"{oname}{h}"])
        S.op("dve", lambda e, h=h: e.tensor_tensor(out=oacc[h], in0=oacc[h], in1=ps[3][0:64, 0:TB], op=ALU.mult), reads=[f"{oname}{h}", "ps3"], writes=[f"{oname}{h}"])


def rms_over_heads_store(S, K, A_, oacc, oname, gcol, gname, out_bf, oT_d, row0, t0, ps, sq, nheads=4, eps_name="eps_n6", tag="o"):
    for h in range(nheads):
        S.op("act", lambda e, h=h: e.activation(out=sq[h % 2], in_=oacc[h], func=AF.Square), reads=[f"{oname}{h}"], writes=[f"sq{h % 2}"])
        S.op("pe", lambda e, h=h: e.matmul(ps[3][0:64, 0:TB], lhsT=K["ones_f"][0:64, 0:64], rhs=sq[h % 2], start=(h == 0), stop=(h == nheads - 1)),
             reads=[f"sq{h % 2}", "kc3"], writes=["ps3"])
    rstd = K["rstd64"]
    rstd_from_ss(S, K, ps[3], "ps3", 64, TB, 1.0 / (64 * nheads), eps_name, rstd, "rstd64")
    for h in range(nheads):
        S.op("dve", lambda e, h=h: e.scalar_tensor_tensor(out=out_bf[h % 2], in0=oacc[h], scalar=gcol[:, h:h + 1], in1=rstd[0:64, 0:TB], op0=ALU.mult, op1=ALU.mult),
             reads=[f"{oname}{h}", "rstd64", gname], writes=[f"obf{h % 2}"])
        S.dma("sp", lambda e, h=h: e.dma_start(out=oT_d[row0 + h * 64:row0 + (h + 1) * 64, t0:t0 + TB], in_=out_bf[h % 2]), reads=[f"obf{h % 2}"], writes=[f"{tag}T_d{t0 // 128}"])


def mixer_dil(P, S, A, l, K, xT_d, oT_d, nblk=NB_FULL):
    W = P.din
    ps = K["ps"]
    m0 = A.mark()
    nt = nblk * 4
    wq = load_w_bf16(S, A, K, W["w_in"][P.wl(l)][:, 768:1024], 256, "dil_wq")
    wk = load_w_bf16(S, A, K, W["w_in"][P.wl(l)][:, 1024:1280], 256, "dil_wk")
    wv = load_w_bf16(S, A, K, W["w_in"][P.wl(l)][:, 1280:1536], 256, "dil_wv")
    wp = load_w_bf16(S, A, K, W["dil_wperm"][P.wl(l)], 128, "dil_wp")
    gcol = A.alloc([64, 4])
    with P.nc.allow_non_contiguous_dma(reason="tiny gain load"):
        S.dma("sp", lambda e: e.dma_start(out=gcol, in_=W["dil_norm_g"][P.wl(l)].rearrange("(h d) -> d h", d=64), allow_slow_non_contiguous=True), writes=["dil_g"])
    masks = A.alloc([128, 20, TB], BF16)
    S.dma("sp", lambda e: e.dma_start(out=masks, in_=W["c_mask_dil"].rearrange("m k q -> k m q")), writes=["masks"])
    RING = 20
    kT = [A.alloc([64, RING * 128], BF16) for h in range(4)]
    Vp = [A.alloc([128, RING, 65], BF16) for h in range(4)]
    for h in range(4):
        S.op("pool", lambda e, h=h: e.memset(Vp[h][:, :, 64:65], 1.0), writes=[f"dilVones{h}"])
    xT = [A.alloc([128, 8, TB], BF16) for i in range(2)]
    ctab = [A.alloc([16, TB]) for i in range(2)]
    stab = [A.alloc([16, TB]) for i in range(2)]
    qT = [A.alloc([64, TB], BF16) for h in range(4)]
    qf = A.alloc([64, TB]); pf = A.alloc([16, TB])
    Pbuf = [A.alloc([128, TB], BF16) for i in range(3)]
    oacc = [A.alloc([64, TB]) for h in range(4)]
    sq = [A.alloc([64, TB]) for i in range(2)]
    obf = [A.alloc([64, TB], BF16) for i in range(2)]
    for tb in range(nblk):
        b = tb % 2
        t0 = tb * TB
        S.dma("sp", lambda e, b=b, t0=t0: e.dma_start(out=xT[b], in_=xT_d[:, t0:t0 + TB].rearrange("(c p) t -> p c t", p=128)),
              reads=[f"xT_d{t0 // 128 + i}" for i in range(4)], writes=[f"xT{b}"])
        S.dma("sp", lambda e, b=b, t0=t0: e.dma_start(out=ctab[b], in_=W["c_rope_dil"][0, :, t0:t0 + TB]), writes=[f"ctab{b}"])
        S.dma("sp", lambda e, b=b, t0=t0: e.dma_start(out=stab[b], in_=W["c_rope_dil"][1, :, t0:t0 + TB]), writes=[f"stab{b}"])
        for h in range(4):
            for which in range(2):
                wmain, wname = (wq, "dil_wq") if which == 0 else (wk, "dil_wk")
                proj_fm(S, ps[6], "ps6", 64, wmain, wname, h * 64, xT[b], f"xT{b}")
                proj_fm(S, ps[7], "ps7", 16, wp, "dil_wp", which * 64 + h * 16, xT[b], f"xT{b}")
                S.op("act", lambda e: e.activation(out=qf, in_=ps[6][0:64, 0:TB], func=AF.Identity), reads=["ps6"], writes=["qf"])
                S.op("dve", lambda e, b=b: e.tensor_tensor(out=pf, in0=ps[7][0:16, 0:TB], in1=stab[b], op=ALU.mult), reads=["ps7", f"stab{b}"], writes=["pf"])
                S.op("dve", lambda e, b=b: e.tensor_tensor(out=qf[0:16, :], in0=qf[0:16, :], in1=ctab[b], op=ALU.mult), reads=["qf", f"ctab{b}"], writes=["qf"])
                S.op("dve", lambda e: e.tensor_tensor(out=qf[0:16, :], in0=qf[0:16, :], in1=pf, op=ALU.add), reads=["qf", "pf"], writes=["qf"])
                if which == 0:
                    S.op("dve", lambda e, h=h: e.tensor_scalar(out=qT[h], in0=qf, scalar1=0.125, scalar2=None, op0=ALU.mult), reads=["qf"], writes=[f"dq{h}"])
                else:
                    S.op("dve", lambda e, h=h, r0=((4 * tb) % RING) * 128: e.tensor_copy(out=kT[h][:, r0:r0 + TB], in_=qf), reads=["qf"], writes=["dil_kT"])
        for tt in range(4):
            for c in range(8):
                S.op("pe", lambda e, c=c, tt=tt, b=b: e.matmul(ps[6][:, 0:256], lhsT=xT[b][:, c, tt * 128:(tt + 1) * 128], rhs=wv[:, c, :], start=(c == 0), stop=(c == 7)),
                     reads=[f"xT{b}", "dil_wv"], writes=["ps6"])
            for h in range(4):
                S.op("dve", lambda e, h=h, sl_=(tb * 4 + tt) % RING: e.tensor_copy(out=Vp[h][:, sl_, 0:64], in_=ps[6][:, h * 64:(h + 1) * 64]), reads=["ps6", f"dilVones{h}"], writes=["dil_V"])
        attention_block(S, K, tb, range(4), qT, "dq", kT, "dil_kT", Vp, "dil_V", masks, 16, max(0, 4 * tb - 16), Pbuf, oacc, "dil_o", ps, ring=RING)
        rms_over_heads_store(S, K, A, oacc, "dil_o", gcol, "dil_g", obf, oT_d, 256, t0, ps, sq)
    S.barrier()
    A.release(m0)
def mixer_mla(P, S, A, l, K, xT_d, oT_d, nblk=NB_FULL):
    W = P.din
    ps = K["ps"]
    m0 = A.mark()
    nt = nblk * 4
    SC = 96 ** -0.5
    wcq = load_w_bf16(S, A, K, W["w_in"][P.wl(l)][:, 2560:2816], 256, "mla_wcq")
    wckv = load_w_bf16(S, A, K, W["w_in"][P.wl(l)][:, 2816:2944], 128, "mla_wckv")
    wkr = load_w_bf16(S, A, K, W["mla_wkr_pad"][P.wl(l)], 192, "mla_wkr")
    wqu = load_w_bf16(S, A, K, W["mla_w_q_up"][P.wl(l)], 384, "mla_wqu", kchunks=2)
    wqp = load_w_bf16(S, A, K, W["mla_wq_perm"][P.wl(l)], 384, "mla_wqp", kchunks=2)
    wkn = load_w_bf16(S, A, K, W["mla_wk_pad"][P.wl(l)], 384, "mla_wkn", kchunks=1)
    wv = load_w_bf16(S, A, K, W["mla_wv"][P.wl(l)], 256, "mla_wv", kchunks=1)
    gq = A.alloc([128, 2]); gkv = A.alloc([128, 1]); gcol = A.alloc([64, 4])
    with P.nc.allow_non_contiguous_dma(reason="tiny gain load"):
        S.dma("sp", lambda e: e.dma_start(out=gq, in_=W["mla_q_norm_g"][P.wl(l)].rearrange("(c p) -> p c", p=128), allow_slow_non_contiguous=True), writes=["mla_gq"])
        S.dma("sp", lambda e: e.dma_start(out=gkv, in_=W["mla_kv_norm_g"][P.wl(l)].rearrange("(c p) -> p c", p=128), allow_slow_non_contiguous=True), writes=["mla_gkv"])
        S.dma("sp", lambda e: e.dma_start(out=gcol, in_=W["mla_out_norm_g"][P.wl(l)].rearrange("(h d) -> d h", d=64), allow_slow_non_contiguous=True), writes=["mla_g"])
    masks = A.alloc([128, 4, TB], BF16)
    S.dma("sp", lambda e: e.dma_start(out=masks, in_=W["c_mask_mla"].rearrange("m k q -> k m q")), writes=["masks"])
    kT = [A.alloc([96, nblk * TB], BF16) for h in range(4)]
    Vp = [A.alloc([128, nt, 65], BF16) for h in range(4)]
    for h in range(4):
        S.op("pool", lambda e, h=h: e.memset(Vp[h][:, :, 64:65], 1.0), writes=[f"mlaVones{h}"])
    xT = [A.alloc([128, 8, TB], BF16) for i in range(2)]
    c96 = [A.alloc([96, TB])] * 2
    s96 = [A.alloc([96, TB])] * 2
    cq = A.alloc([128, 2, TB]); cqn = A.alloc([128, 2, TB], BF16)
    ckv = A.alloc([128, TB]); ckvn = A.alloc([128, TB], BF16)
    sqb = A.alloc([128, TB])
    rstd = K["rstd128"]
    qT = [A.alloc([96, TB], BF16) for h in range(4)]
    qf = A.alloc([96, TB]); pf = A.alloc([96, TB])
    Pbuf = [A.alloc([128, TB], BF16) for i in range(3)]
    oacc = [A.alloc([64, TB]) for h in range(4)]
    sq = [A.alloc([64, TB]) for i in range(2)]
    obf = [A.alloc([64, TB], BF16) for i in range(2)]
    for tb in range(nblk):
        b = tb % 2
        t0 = tb * TB
        S.dma("sp", lambda e, b=b, t0=t0: e.dma_start(out=xT[b], in_=xT_d[:, t0:t0 + TB].rearrange("(c p) t -> p c t", p=128)), writes=[f"xT{b}"])
        S.dma("sp", lambda e, b=b, t0=t0: e.dma_start(out=c96[b], in_=W["c_rope_mla"][0, :, t0:t0 + TB]), writes=["c96"])
        S.dma("sp", lambda e, b=b, t0=t0: e.dma_start(out=s96[b], in_=W["c_rope_mla"][1, :, t0:t0 + TB]), writes=["s96"])
        for kc in range(2):
            proj_fm(S, ps[6 + kc], f"ps{6 + kc}", 128, wcq, "mla_wcq", kc * 128, xT[b], f"xT{b}")
            S.op("act", lambda e, kc=kc: e.activation(out=cq[:, kc, :], in_=ps[6 + kc][:, 0:TB], func=AF.Identity), reads=[f"ps{6 + kc}"], writes=["cq"])
            S.op("dve", lambda e, kc=kc: e.tensor_tensor(out=sqb, in0=ps[6 + kc][:, 0:TB], in1=cq[:, kc, :], op=ALU.mult), reads=[f"ps{6 + kc}", "cq"], writes=["sqb"])
            S.op("pe", lambda e, kc=kc: e.matmul(ps[3][:, 0:TB], lhsT=K["ones_f"], rhs=sqb, start=(kc == 0), stop=(kc == 1)), reads=["sqb", "kc3"], writes=["ps3"])
        rstd_from_ss(S, K, ps[3], "ps3", 128, TB, 1.0 / 256, "eps_n6", rstd, "rstd128")
        for kc in range(2):
            S.op("dve", lambda e, kc=kc: e.scalar_tensor_tensor(out=cqn[:, kc, :], in0=cq[:, kc, :], scalar=gq[:, kc:kc + 1], in1=rstd[:, 0:TB], op0=ALU.mult, op1=ALU.mult),
                 reads=["cq", "rstd128", "mla_gq"], writes=["cqn"])
        proj_fm(S, ps[6], "ps6", 128, wckv, "mla_wckv", 0, xT[b], f"xT{b}")
        S.op("act", lambda e: e.activation(out=ckv, in_=ps[6][:, 0:TB], func=AF.Identity), reads=["ps6"], writes=["ckv"])
        S.op("dve", lambda e: e.tensor_tensor(out=sqb, in0=ps[6][:, 0:TB], in1=ckv, op=ALU.mult), reads=["ps6", "ckv"], writes=["sqb"])
        S.op("pe", lambda e: e.matmul(ps[3][:, 0:TB], lhsT=K["ones_f"], rhs=sqb, start=True, stop=True), reads=["sqb", "kc3"], writes=["ps3"])
        rstd_from_ss(S, K, ps[3], "ps3", 128, TB, 1.0 / 128, "eps_n6", rstd, "rstd128")
        S.op("dve", lambda e: e.scalar_tensor_tensor(out=ckvn, in0=ckv, scalar=gkv[:, 0:1], in1=rstd[:, 0:TB], op0=ALU.mult, op1=ALU.mult),
             reads=["ckv", "rstd128", "mla_gkv"], writes=["ckvn"])
        proj_fm(S, ps[7], "ps7", 96, wkr, "mla_wkr", 96, xT[b], f"xT{b}")
        S.op("dve", lambda e, b=b: e.tensor_tensor(out=pf, in0=ps[7][0:96, 0:TB], in1=s96[b], op=ALU.mult), reads=["ps7", "s96"], writes=["pf"])
        for h in range(4):
            proj_fm(S, ps[6], "ps6", 96, wkr, "mla_wkr", 0, xT[b], f"xT{b}", stop=False)
            S.op("pe", lambda e, h=h: e.matmul(ps[6][0:96, 0:TB], lhsT=wkn[:, 0, h * 96:(h + 1) * 96], rhs=ckvn, start=False, stop=True), reads=["mla_wkn", "ckvn"], writes=["ps6"])
            S.op("dve", lambda e, b=b: e.tensor_tensor(out=qf, in0=ps[6][0:96, 0:TB], in1=c96[b], op=ALU.mult), reads=["ps6", "c96"], writes=["qf"])
            S.op("dve", lambda e, h=h, t0=t0: e.tensor_tensor(out=kT[h][:, t0:t0 + TB], in0=qf, in1=pf, op=ALU.add), reads=["qf", "pf"], writes=["mla_kT"])
        for h in range(4):
            for kc in range(2):
                S.op("pe", lambda e, h=h, kc=kc: e.matmul(ps[6][0:96, 0:TB], lhsT=wqu[:, kc, h * 96:(h + 1) * 96], rhs=cqn[:, kc, :], start=(kc == 0), stop=(kc == 1)),
                     reads=["mla_wqu", "cqn"], writes=["ps6"])
            for kc in range(2):
                S.op("pe", lambda e, h=h, kc=kc: e.matmul(ps[7][0:96, 0:TB], lhsT=wqp[:, kc, h * 96:(h + 1) * 96], rhs=cqn[:, kc, :], start=(kc == 0), stop=(kc == 1)),
                     reads=["mla_wqp", "cqn"], writes=["ps7"])
            S.op("dve", lambda e, b=b: e.tensor_tensor(out=qf, in0=ps[6][0:96, 0:TB], in1=c96[b], op=ALU.mult), reads=["ps6", "c96"], writes=["qf"])
            S.op("dve", lambda e, b=b: e.scalar_tensor_tensor(out=K["qp96"], in0=ps[7][0:96, 0:TB], scalar=SC, in1=s96[b], op0=ALU.mult, op1=ALU.mult), reads=["ps7", "s96"], writes=["qp96"])
            S.op("dve", lambda e, h=h: e.scalar_tensor_tensor(out=qT[h], in0=qf, scalar=SC, in1=K["qp96"], op0=ALU.mult, op1=ALU.add), reads=["qf", "qp96"], writes=[f"mq{h}"])
        for tt in range(4):
            S.op("pe", lambda e, tt=tt: e.matmul(ps[6][:, 0:256], lhsT=ckvn[:, tt * 128:(tt + 1) * 128], rhs=wv[:, 0, :], start=True, stop=True), reads=["ckvn", "mla_wv"], writes=["ps6"])
            for h in range(4):
                S.op("dve", lambda e, h=h, tt=tt, tb=tb: e.tensor_copy(out=Vp[h][:, tb * 4 + tt, 0:64], in_=ps[6][:, h * 64:(h + 1) * 64]), reads=["ps6", f"mlaVones{h}"], writes=["mla_V"])
        attention_block(S, K, tb, range(4), qT, "mq", kT, "mla_kT", Vp, "mla_V", masks, 0, 0, Pbuf, oacc, "mla_o", ps)
        rms_over_heads_store(S, K, A, oacc, "mla_o", gcol, "mla_g", obf, oT_d, 768, t0, ps, sq)
    S.barrier()
    A.release(m0)


def make_consts_a(P, S, A, K):
    K["ones_f"] = A.alloc([128, 128])
    K["eps_n6"] = A.alloc([128, 1])
    K["rs_row"] = A.alloc([128, TB])
    K["rstd64"] = A.alloc([64, TB])
    K["rstd128"] = A.alloc([128, TB])
    K["qp96"] = A.alloc([96, TB])
    K["wstg"] = [A.alloc([128, 2048]) for i in range(2)]
    K["wstg_i"] = [0]
    K["attn_cnt"] = [0]
    S.op("dve", lambda e: e.memset(K["ones_f"], 1.0), writes=["kc3"])
    S.op("dve", lambda e: e.memset(K["eps_n6"], 1e-6), writes=["kc2"])
RET_GAM = [1.0 - 2.0 ** (-5.0 - h) for h in range(4)]


def mixer_ret(P, S, A, l, K, xT_d, oT_d, nblk=NB_FULL):
    W = P.din
    ps = K["ps"]
    m0 = A.mark()
    wq = load_w_bf16(S, A, K, W["w_in"][P.wl(l)][:, 0:128], 128, "ret_wq")
    wk = load_w_bf16(S, A, K, W["w_in"][P.wl(l)][:, 128:256], 128, "ret_wk")
    wv = load_w_bf16(S, A, K, W["w_in"][P.wl(l)][:, 256:512], 256, "ret_wv")
    wg = load_w_bf16(S, A, K, W["w_in"][P.wl(l)][:, 512:768], 256, "ret_wg")
    wp = load_w_bf16(S, A, K, W["ret_wperm"][P.wl(l)], 256, "ret_wp")
    gcol = A.alloc([64, 4])
    S.dma("sp", lambda e: e.dma_start(out=gcol, in_=W["ret_norm_g"][P.wl(l)].rearrange("(h d) -> d h", d=64), allow_slow_non_contiguous=True), writes=["ret_g"])
    DT = A.alloc([128, 4, 128])
    S.dma("sp", lambda e: e.dma_start(out=DT, in_=W["c_ret_decayT"].rearrange("h j i -> j h i")), writes=["ret_DT"])
    xi = [A.alloc([64, TB]) for p in range(2)]
    for p in range(2):
        S.dma("sp", lambda e, p=p: e.dma_start(out=xi[p], in_=W["c_ret_xi"][p * 64:(p + 1) * 64, :]), writes=[f"ret_xi{p}"])
    zmask = A.alloc([128, 4, 64], BF16)
    S.dma("sp", lambda e: e.dma_start(out=zmask, in_=W["c_ret_zmask"].rearrange("h j c -> j h c")), writes=["ret_zm"])
    identb = A.alloc([128, 128], BF16)
    S.op("dve", lambda e: e.tensor_copy(out=identb, in_=K["ident"]), reads=["kc"], writes=["identb"])
    Sall = [A.alloc([64, 64]) for p in range(2)]
    Sbf = [A.alloc([64, 64], BF16) for p in range(2)]
    for p in range(2):
        S.op("dve", lambda e, p=p: e.memset(Sall[p], 0.0), writes=[f"ret_S{p}"])
        S.op("dve", lambda e, p=p: e.memset(Sbf[p], 0.0), writes=[f"ret_Sbf{p}"])
    xT = [A.alloc([128, 8, TB], BF16) for i in range(2)]
    ctab = [A.alloc([64, TB]) for i in range(2)]
    stab = [A.alloc([64, TB]) for i in range(2)]
    qf = A.alloc([64, TB]); pf = A.alloc([64, TB])
    qT = [A.alloc([64, TB], BF16) for p in range(2)]
    qx = [A.alloc([64, TB], BF16) for p in range(2)]
    kTt = [A.alloc([64, TB], BF16) for p in range(2)]
    vbf = A.alloc([128, 4, 256], BF16)
    sg = A.alloc([64, TB]); gl = [A.alloc([64, TB]) for h in range(4)]
    kzp = A.alloc([128, 4, 4, 64], BF16)
    Pb = [A.alloc([128, 128], BF16) for i in range(2)]
    of = A.alloc([64, TB]); sq = A.alloc([64, TB]); obf = [A.alloc([64, TB], BF16) for i in range(2)]
    pst = K["ps_bf"]
    cnt = 0
    for tb in range(nblk):
        b = tb % 2
        t0 = tb * TB
        S.dma("sp", lambda e, b=b, t0=t0: e.dma_start(out=xT[b], in_=xT_d[:, t0:t0 + TB].rearrange("(c p) t -> p c t", p=128)), writes=[f"xT{b}"])
        S.dma("sp", lambda e, b=b, t0=t0: e.dma_start(out=ctab[b], in_=W["c_rope_ret2"][0, :, t0:t0 + TB]), writes=[f"ctab{b}"])
        S.dma("sp", lambda e, b=b, t0=t0: e.dma_start(out=stab[b], in_=W["c_rope_ret2"][1, :, t0:t0 + TB]), writes=[f"stab{b}"])
        for p in range(2):
            for which in range(2):
                wmain, wname = (wq, "ret_wq") if which == 0 else (wk, "ret_wk")
                proj_fm(S, ps[6], "ps6", 64, wmain, wname, p * 64, xT[b], f"xT{b}")
                proj_fm(S, ps[7], "ps7", 64, wp, "ret_wp", which * 128 + p * 64, xT[b], f"xT{b}")
                S.op("dve", lambda e, b=b: e.tensor_tensor(out=qf, in0=ps[6][0:64, 0:TB], in1=ctab[b], op=ALU.mult), reads=["ps6", f"ctab{b}"], writes=["qf"])
                S.op("dve", lambda e, b=b: e.tensor_tensor(out=pf, in0=ps[7][0:64, 0:TB], in1=stab[b], op=ALU.mult), reads=["ps7", f"stab{b}"], writes=["pf"])
                S.op("dve", lambda e: e.tensor_tensor(out=qf, in0=qf, in1=pf, op=ALU.add), reads=["qf", "pf"], writes=["qf"])
                if which == 0:
                    S.op("dve", lambda e, p=p: e.tensor_copy(out=qT[p], in_=qf), reads=["qf"], writes=[f"ret_qT{p}"])
                    S.op("pool", lambda e, p=p: e.tensor_tensor(out=qx[p], in0=qf, in1=xi[p], op=ALU.mult), reads=["qf", f"ret_xi{p}"], writes=[f"ret_qx{p}"])
                else:
                    S.op("dve", lambda e, p=p: e.tensor_scalar(out=kTt[p], in0=qf, scalar1=32 ** -0.5, scalar2=None, op0=ALU.mult), reads=["qf"], writes=[f"ret_kT{p}"])
        for tt in range(4):
            for c in range(8):
                S.op("pe", lambda e, c=c, tt=tt, b=b: e.matmul(ps[6][:, 0:256], lhsT=xT[b][:, c, tt * 128:(tt + 1) * 128], rhs=wv[:, c, :], start=(c == 0), stop=(c == 7)),
                     reads=[f"xT{b}", "ret_wv"], writes=["ps6"])
            S.op("dve", lambda e, tt=tt: e.tensor_copy(out=vbf[:, tt, :], in_=ps[6][:, 0:256]), reads=["ps6"], writes=["ret_v"])
            for p in range(2):
                S.op("pe", lambda e, tt=tt, p=p: e.transpose(out=pst[:, 0:64], in_=kTt[p][:, tt * 128:(tt + 1) * 128], identity=identb[0:64, 0:64]), reads=[f"ret_kT{p}", "identb"], writes=["ps_bf"])
                for h2 in range(2):
                    h = p * 2 + h2
                    S.op("dve", lambda e, tt=tt, h=h: e.tensor_tensor(out=kzp[:, tt, h, :], in0=pst[:, 0:64], in1=zmask[:, h, :], op=ALU.mult), reads=["ps_bf", "ret_zm"], writes=["ret_kzp"])
        for h in range(4):
            proj_fm(S, ps[7], "ps7", 64, wg, "ret_wg", h * 64, xT[b], f"xT{b}")
            S.op("act", lambda e: e.activation(out=sg, in_=ps[7][0:64, 0:TB], func=AF.Sigmoid), reads=["ps7"], writes=["ret_sg"])
            S.op("dve", lambda e, h=h: e.tensor_tensor(out=gl[h], in0=ps[7][0:64, 0:TB], in1=sg, op=ALU.mult), reads=["ps7", "ret_sg"], writes=[f"ret_gl{h}"])
        for h in range(4):
            p = h // 2
            hs = slice((h % 2) * 32, (h % 2 + 1) * 32)
            po = ps[4 + (h % 2)]
            pon = f"ps{4 + (h % 2)}"
            for tt in range(4):
                cs = slice(tt * 128, (tt + 1) * 128)
                pb = cnt % 2
                cnt += 1
                S.op("pe", lambda e, hs=hs, cs=cs, pb=pb, p=p: e.matmul(ps[pb][:, 0:128], lhsT=kTt[p][hs, cs], rhs=qT[p][hs, cs], start=True, stop=True),
                     reads=[f"ret_kT{p}", f"ret_qT{p}"], writes=[f"ps{pb}"])
                S.op("dve", lambda e, h=h, pb=pb: e.tensor_tensor(out=Pb[pb], in0=ps[pb][:, 0:128], in1=DT[:, h, :], op=ALU.mult), reads=[f"ps{pb}", "ret_DT"], writes=[f"ret_P{pb}"])
                S.op("pe", lambda e, h=h, tt=tt, cs=cs, pb=pb, po=po: e.matmul(po[0:64, cs], lhsT=vbf[:, tt, h * 64:(h + 1) * 64], rhs=Pb[pb], start=True, stop=False),
                     reads=["ret_v", f"ret_P{pb}"], writes=[pon])
                S.op("pe", lambda e, hs=hs, cs=cs, po=po, p=p: e.matmul(po[0:64, cs], lhsT=Sbf[p][hs, :], rhs=qx[p][hs, cs], start=False, stop=True),
                     reads=[f"ret_Sbf{p}", f"ret_qx{p}"], writes=[pon])
                S.op("pe", lambda e, h=h, tt=tt: e.matmul(ps[2][0:64, 0:64], lhsT=kzp[:, tt, h, :], rhs=vbf[:, tt, h * 64:(h + 1) * 64], start=True, stop=True),
                     reads=["ret_kzp", "ret_v"], writes=["ps2"])
                S.op("dve", lambda e, h=h, hs=hs, p=p: e.scalar_tensor_tensor(out=Sall[p][hs, :], in0=Sall[p][hs, :], scalar=RET_GAM[h] ** 128, in1=ps[2][hs, 0:64], op0=ALU.mult, op1=ALU.add),
                     reads=["ps2", f"ret_S{p}"], writes=[f"ret_S{p}"])
                S.op("dve", lambda e, hs=hs, p=p: e.tensor_copy(out=Sbf[p][hs, :], in_=Sall[p][hs, :]), reads=[f"ret_S{p}"], writes=[f"ret_Sbf{p}"])
            S.op("act", lambda e, po=po: e.activation(out=of, in_=po[0:64, 0:TB], func=AF.Identity), reads=[pon], writes=["ret_of"])
            S.op("dve", lambda e, po=po: e.tensor_tensor(out=sq, in0=po[0:64, 0:TB], in1=of, op=ALU.mult), reads=[pon, "ret_of"], writes=["ret_sq"])
            S.op("pe", lambda e: e.matmul(ps[3][0:64, 0:TB // 2], lhsT=K["ones_f"][0:64, 0:64], rhs=sq[:, 0:TB // 2], start=True, stop=True), reads=["ret_sq", "kc3"], writes=["ps3"])
            S.op("pe", lambda e: e.matmul(ps[2][0:64, 0:TB // 2], lhsT=K["ones_f"][0:64, 0:64], rhs=sq[:, TB // 2:TB], start=True, stop=True), reads=["ret_sq", "kc3"], writes=["ps2"])
            rstd_from_ss(S, K, ps[3], "ps3", 64, TB // 2, 1.0 / 64, "eps_n6", K["rstd64"], "rstd64")
            S.op("act", lambda e: e.activation(out=K["rstd64"][0:64, TB // 2:TB], in_=ps[2][0:64, 0:TB // 2], func=AF.Sqrt, bias=K["eps_n6"][0:64, 0:1], scale=1.0 / 64),
                 reads=["ps2", "kc2"], writes=["rstd64"])
            S.op("dve", lambda e: e.reciprocal(out=K["rstd64"][0:64, TB // 2:TB], in_=K["rstd64"][0:64, TB // 2:TB]), reads=["rstd64"], writes=["rstd64"])
            S.op("dve", lambda e, h=h: e.scalar_tensor_tensor(out=of, in0=of, scalar=gcol[:, h:h + 1], in1=K["rstd64"][0:64, 0:TB], op0=ALU.mult, op1=ALU.mult),
                 reads=["ret_of", "rstd64", "ret_g"], writes=["ret_of"])
            S.op("dve", lambda e, h=h: e.tensor_tensor(out=obf[h % 2], in0=of, in1=gl[h], op=ALU.mult), reads=["ret_of", f"ret_gl{h}"], writes=[f"robf{h % 2}"])
            S.dma("sp", lambda e, h=h, t0=t0: e.dma_start(out=oT_d[h * 64:(h + 1) * 64, t0:t0 + TB], in_=obf[h % 2]), reads=[f"robf{h % 2}"], writes=[f"oTa_d{t0 // 128}_{h}"])
    S.barrier()
    A.release(m0)


def host_consts_ret():
    import ml_dtypes
    c = {}
    lg = np.log(np.array(RET_GAM, dtype=np.float64))
    j = np.arange(128)[:, None]
    i = np.arange(128)[None, :]
    c["c_ret_decayT"] = np.stack([np.where(i >= j, np.exp((i - j) * lg[h]), 0.0) for h in range(4)]).astype(np.float32)
    t = np.arange(TB)
    xi = np.stack([np.exp(((t % 128) + 1.0) * lg[h]) for h in range(4)])
    c["c_ret_xi"] = np.ascontiguousarray(np.repeat(xi, 32, axis=0).astype(np.float32))
    zm = np.zeros((4, 128, 64), np.float32)
    for h in range(4):
        zm[h, :, (h % 2) * 32:(h % 2 + 1) * 32] = np.exp((127 - np.arange(128)) * lg[h])[:, None]
    c["c_ret_zmask"] = zm.astype(ml_dtypes.bfloat16)
    r = _rope_tables(SEQ, 32, 10000.0)
    c["c_rope_ret2"] = np.ascontiguousarray(np.tile(r, (1, 2, 1)))
    return c
RW_C = 64


def load_small_bf16(S, A, K, src_ap, rows, ncols, name):
    wb = A.alloc([rows, ncols], BF16)
    stg = K["wstg"]
    i = K["wstg_i"][0] % 2
    K["wstg_i"][0] += 1
    S.dma("sp", lambda e: e.dma_start(out=stg[i][0:rows, 0:ncols], in_=src_ap), writes=[f"wstg{i}"])
    S.op("pool", lambda e: e.tensor_copy(out=wb, in_=stg[i][0:rows, 0:ncols]), reads=[f"wstg{i}"], writes=[name])
    return wb


def mixer_rwkv(P, S, A, l, K, xT_d, oT_d, vfirst_d, nblk=NB_FULL):
    W = P.din
    ps = K["ps"]
    m0 = A.mark()
    EXPM05 = math.exp(-0.5)

    def col(src1d, lo, rows, name):
        t = A.alloc([rows, 1])
        S.dma("sp", lambda e: e.dma_start(out=t, in_=src1d[lo:lo + rows].rearrange("(p o) -> p o", o=1)), writes=[name])
        return t

    wr = load_w_bf16(S, A, K, W["w_in"][P.wl(l)][:, 1536:1792], 256, "rw_wr")
    wk = load_w_bf16(S, A, K, W["w_in"][P.wl(l)][:, 1792:2048], 256, "rw_wk")
    wv = load_w_bf16(S, A, K, W["w_in"][P.wl(l)][:, 2048:2304], 256, "rw_wv")
    wlo = load_w_bf16(S, A, K, W["w_in"][P.wl(l)][:, 2304:2560], 256, "rw_wlo")
    w_up = load_small_bf16(S, A, K, W["rwkv_w_up"][P.wl(l)], 64, 256, "rw_wup")
    a_up = load_small_bf16(S, A, K, W["rwkv_a_up"][P.wl(l)], 64, 256, "rw_aup")
    g_up = load_small_bf16(S, A, K, W["rwkv_g_up"][P.wl(l)], 128, 256, "rw_gup")
    if l > 0:
        wvr = load_w_bf16(S, A, K, W["rwkv_vres_down"][P.vl(l)], 32, "rw_wvr")
        v_up = load_small_bf16(S, A, K, W["rwkv_v_up"][P.vl(l)], 32, 256, "rw_vup")
    mu = W["rwkv_mu"][P.wl(l)]
    pc = {}
    for hp in range(2):
        pc[f"mu_r{hp}"] = col(mu, hp * 128, 128, f"rwc")
        pc[f"mu_k{hp}"] = col(mu, 256 + hp * 128, 128, "rwc1")
        pc[f"mu_v{hp}"] = col(mu, 512 + hp * 128, 128, "rwc2")
        pc[f"w0{hp}"] = col(W["rwkv_w0"][P.wl(l)], hp * 128, 128, "rwc3")
        pc[f"a0{hp}"] = col(W["rwkv_a0"][P.wl(l)], hp * 128, 128, "rwc4")
        pc[f"kk{hp}"] = col(W["rwkv_k_k"][P.wl(l)], hp * 128, 128, "rwc5")
        pc[f"ka{hp}"] = col(W["rwkv_k_a"][P.wl(l)], hp * 128, 128, "rwc6")
        pc[f"rk{hp}"] = col(W["rwkv_r_k"][P.wl(l)].rearrange("h d -> (h d)"), hp * 128, 128, "rwc7")
        pc[f"lg{hp}"] = col(W["rwkv_ln_g"][P.wl(l)], hp * 128, 128, "rwc8")
        pc[f"lb{hp}"] = col(W["rwkv_ln_b"][P.wl(l)], hp * 128, 128, "rwc9")
        pc[f"omka{hp}"] = A.alloc([128, 1])
        S.op("dve", lambda e, hp=hp: e.tensor_scalar(out=pc[f"omka{hp}"], in0=pc[f"ka{hp}"], scalar1=-1.0, scalar2=1.0, op0=ALU.mult, op1=ALU.add), reads=["rwc6"], writes=["rwc10"])
        if l > 0:
            pc[f"v0{hp}"] = col(W["rwkv_v0"][P.vl(l)], hp * 128, 128, "rwc11")
    pc["mu_wd"] = col(mu, 768, 64, "rwc12")
    pc["mu_ad"] = col(mu, 832, 64, "rwc13")
    pc["mu_gd"] = col(mu, 896, 128, "rwc14")
    if l > 0:
        pc["mu_vd"] = col(W["rwkv_vres_mu"][P.vl(l)], 0, 32, "rwc15")
    PCN = ["rwc", "rwc1", "rwc2", "rwc3", "rwc4", "rwc5", "rwc6", "rwc7", "rwc8", "rwc9", "rwc10", "rwc11", "rwc12", "rwc13", "rwc14", "rwc15"]
    msk = A.alloc([128, 3, 128])
    S.dma("sp", lambda e: e.dma_start(out=msk, in_=W["c_rwkv_masks"].rearrange("m p c -> p m c")), writes=["rw_msk"])
    bdones = A.alloc([128, 128]); bdavg = A.alloc([128, 128]); rst = A.alloc([128, TB])
    S.dma("sp", lambda e: e.dma_start(out=bdones, in_=W["c_rwkv_bdones"]), writes=["rw_bdo"])
    S.dma("sp", lambda e: e.dma_start(out=bdavg, in_=W["c_rwkv_bdavg"]), writes=["rw_bda"])
    S.dma("sp", lambda e: e.dma_start(out=rst, in_=W["c_rwkv_reset"]), writes=["rw_rst"])
    eps_gn = A.alloc([128, 1])
    S.op("dve", lambda e: e.memset(eps_gn, 64e-5), writes=["rw_eps"])
    ident = K["ident"]

    xT = [A.alloc([128, 8, TB], BF16) for i in range(2)]
    raw = {}
    for nm, rows in (("r0", 128), ("r1", 128), ("k0", 128), ("k1", 128), ("v0", 128), ("v1", 128), ("wd", 64), ("ad", 64), ("gd", 128), ("vd", 32)):
        if nm == "vd" and l == 0:
            continue
        raw[nm] = A.alloc([rows, TB + 1])
        S.op("pool", lambda e, nm=nm: e.memset(raw[nm][:, 0:1], 0.0), writes=[f"raw_{nm}"])
    dif = A.alloc([128, TB])
    T_ = {}
    for hp in range(2):
        for nm in ("rs", "ks", "vs", "lw", "a", "g", "kk", "kp", "cum", "G", "Gi", "Gp", "At", "Bt", "Kt", "Rt", "bon", "yT"):
            T_[f"{nm}{hp}"] = A.alloc([128, TB])
    wds = A.alloc([64, TB]); ads = A.alloc([64, TB], BF16); gds = A.alloc([128, TB]); th = A.alloc([64, TB], BF16); sgd = A.alloc([128, TB], BF16)
    vds = A.alloc([32, TB], BF16) if l > 0 else None
    tmp = A.alloc([128, TB]); tmp2 = A.alloc([128, TB])
    obf = A.alloc([128, TB], BF16)
    H = [A.alloc([128, 128]) for hp in range(2)]
    for hp in range(2):
        S.op("pool", lambda e, hp=hp: e.memset(H[hp], 0.0), writes=[f"rw_H{hp}"])
    BD = {}
    for hp in range(2):
        for nm in ("A", "B", "K", "R", "V"):
            for i in range(2):
                BD[(hp, nm, i)] = A.alloc([128, 128])
                S.op("pool", lambda e, k=(hp, nm, i): e.memset(BD[k], 0.0), writes=[f"bd{hp}{nm}{i}"])
    CH = {}
    for hp in range(2):
        for nm in ("M", "MT", "M2", "M2T", "PT", "NakT", "QbT", "QkT", "Btok", "Ktok", "Vtok", "W1", "U", "Yh"):
            CH[(hp, nm)] = A.alloc([128, 128])

    def slot(hp, i):
        bank = ps[hp * 3 + i // 4]
        q = i % 4
        return bank[:, q * 128:(q + 1) * 128], f"ps{hp * 3 + i // 4}"

    def shift(nm, rows, wb, wname, col0, mucol, out, outname, b, odt_copy=None):
        proj_fm(S, ps[6], "ps6", rows, wb, wname, col0, xT[b], f"xT{b}")
        r = raw[nm]
        S.op("act", lambda e: e.activation(out=r[:, 1:TB + 1], in_=ps[6][0:rows, 0:TB], func=AF.Identity), reads=["ps6"], writes=[f"raw_{nm}"])
        S.op("pool", lambda e: e.tensor_tensor(out=dif[0:rows, :], in0=r[:, 0:TB], in1=r[:, 1:TB + 1], op=ALU.subtract), reads=[f"raw_{nm}"], writes=["rw_dif"])
        S.op("dve", lambda e: e.scalar_tensor_tensor(out=out, in0=dif[0:rows, :], scalar=mucol[:, 0:1], in1=r[:, 1:TB + 1], op0=ALU.mult, op1=ALU.add),
             reads=["rw_dif", f"raw_{nm}"] + PCN, writes=[outname])
        S.op("pool", lambda e: e.tensor_copy(out=r[:, 0:1], in_=r[:, TB:TB + 1]), reads=[f"raw_{nm}"], writes=[f"raw_{nm}"])

    ev = [0]

    def evac(out, out_name, src, src_name, extra_reads=()):
        ev[0] += 1
        if True:
            S.op("dve", lambda e: e.tensor_copy(out=out, in_=src), reads=[src_name] + list(extra_reads), writes=[out_name])
        else:
            S.op("act", lambda e: e.activation(out=out, in_=src, func=AF.Identity), reads=[src_name] + list(extra_reads), writes=[out_name])

    nch = TB // RW_C
    gch = 0
    for tb in range(nblk):
        b = tb % 2
        t0 = tb * TB
        S.dma("sp", lambda e, b=b, t0=t0: e.dma_start(out=xT[b], in_=xT_d[:, t0:t0 + TB].rearrange("(c p) t -> p c t", p=128)), writes=[f"xT{b}"])
        shift("wd", 64, wlo, "rw_wlo", 0, pc["mu_wd"], wds, "rw_wds", b)
        S.op("act", lambda e: e.activation(out=th, in_=wds, func=AF.Tanh), reads=["rw_wds"], writes=["rw_th"])
        shift("ad", 64, wlo, "rw_wlo", 64, pc["mu_ad"], ads, "rw_ads", b)
        shift("gd", 128, wlo, "rw_wlo", 128, pc["mu_gd"], gds, "rw_gds", b)
        S.op("act", lambda e: e.activation(out=sgd, in_=gds, func=AF.Sigmoid), reads=["rw_gds"], writes=["rw_sgd"])
        if l > 0:
            shift("vd", 32, wvr, "rw_wvr", 0, pc["mu_vd"], vds, "rw_vds", b)
        def pair_prep(hp, b, t0, tb):
            t = lambda nm: T_[f"{nm}{hp}"]
            n = lambda nm: f"rw_{nm}{hp}"
            shift(f"r{hp}", 128, wr, "rw_wr", hp * 128, pc[f"mu_r{hp}"], t("rs"), n("rs"), b)
            shift(f"k{hp}", 128, wk, "rw_wk", hp * 128, pc[f"mu_k{hp}"], t("ks"), n("ks"), b)
            shift(f"v{hp}", 128, wv, "rw_wv", hp * 128, pc[f"mu_v{hp}"], t("vs"), n("vs"), b)
            S.op("pe", lambda e, hp=hp: e.matmul(ps[7][:, 0:TB], lhsT=w_up[:, hp * 128:(hp + 1) * 128], rhs=th, start=True, stop=True), reads=["rw_wup", "rw_th"], writes=["ps7"])
            S.op("act", lambda e, hp=hp: e.activation(out=t("lw"), in_=ps[7][:, 0:TB], func=AF.Sigmoid, bias=pc[f"w0{hp}"][:, 0:1], scale=1.0), reads=["ps7"] + PCN, writes=[n("lw")])
            S.op("dve", lambda e, hp=hp: e.tensor_scalar(out=t("lw"), in0=t("lw"), scalar1=-EXPM05, scalar2=None, op0=ALU.mult), reads=[n("lw")], writes=[n("lw")])
            S.op("pe", lambda e, hp=hp: e.matmul(ps[7][:, 0:TB], lhsT=a_up[:, hp * 128:(hp + 1) * 128], rhs=ads, start=True, stop=True), reads=["rw_aup", "rw_ads"], writes=["ps7"])
            S.op("act", lambda e, hp=hp: e.activation(out=t("a"), in_=ps[7][:, 0:TB], func=AF.Sigmoid, bias=pc[f"a0{hp}"][:, 0:1], scale=1.0), reads=["ps7"] + PCN, writes=[n("a")])
            S.op("pe", lambda e, hp=hp: e.matmul(ps[7][:, 0:TB], lhsT=g_up[:, hp * 128:(hp + 1) * 128], rhs=sgd, start=True, stop=True), reads=["rw_gup", "rw_sgd"], writes=["ps7"])
            S.op("dve", lambda e, hp=hp: e.tensor_copy(out=t("g"), in_=ps[7][:, 0:TB]), reads=["ps7"], writes=[n("g")])
            if l > 0:
                S.op("pe", lambda e, hp=hp: e.matmul(ps[7][:, 0:TB], lhsT=v_up[:, hp * 128:(hp + 1) * 128], rhs=vds, start=True, stop=True), reads=["rw_vup", "rw_vds"], writes=["ps7"])
                S.op("act", lambda e, hp=hp: e.activation(out=tmp, in_=ps[7][:, 0:TB], func=AF.Sigmoid, bias=pc[f"v0{hp}"][:, 0:1], scale=1.0), reads=["ps7"] + PCN, writes=["rw_tmp"])
                S.dma("sp", lambda e, hp=hp, t0=t0: e.dma_start(out=tmp2, in_=vfirst_d[hp, :, t0:t0 + TB]), writes=["rw_tmp2"])
                S.op("pool", lambda e, hp=hp: e.tensor_tensor(out=tmp2, in0=tmp2, in1=t("vs"), op=ALU.subtract), reads=["rw_tmp2", n("vs")], writes=["rw_tmp2"])
                S.op("dve", lambda e, hp=hp: e.tensor_tensor(out=tmp2, in0=tmp2, in1=tmp, op=ALU.mult), reads=["rw_tmp2", "rw_tmp"], writes=["rw_tmp2"])
                S.op("dve", lambda e, hp=hp: e.tensor_tensor(out=t("vs"), in0=t("vs"), in1=tmp2, op=ALU.add), reads=["rw_tmp2", n("vs")], writes=[n("vs")])
            else:
                S.dma("sp", lambda e, hp=hp, t0=t0: e.dma_start(out=vfirst_d[hp, :, t0:t0 + TB], in_=t("vs")), reads=[n("vs")], writes=[f"vfirst{hp}_{tb}"])
            S.op("dve", lambda e, hp=hp: e.tensor_scalar(out=t("kk"), in0=t("ks"), scalar1=pc[f"kk{hp}"][:, 0:1], scalar2=None, op0=ALU.mult), reads=[n("ks")] + PCN, writes=[n("kk")])
            S.op("pool", lambda e, hp=hp: e.tensor_tensor(out=tmp, in0=t("kk"), in1=t("kk"), op=ALU.mult), reads=[n("kk")], writes=["rw_tmp"])
            S.op("pe", lambda e: e.matmul(ps[7][:, 0:TB], lhsT=bdones, rhs=tmp, start=True, stop=True), reads=["rw_bdo", "rw_tmp"], writes=["ps7"])
            S.op("act", lambda e: e.activation(out=tmp2, in_=ps[7][:, 0:TB], func=AF.Sqrt), reads=["ps7"], writes=["rw_tmp2"])
            S.op("dve", lambda e: e.tensor_scalar(out=tmp2, in0=tmp2, scalar1=1e-12, scalar2=None, op0=ALU.max), reads=["rw_tmp2"], writes=["rw_tmp2"])
            S.op("dve", lambda e: e.reciprocal(out=tmp2, in_=tmp2), reads=["rw_tmp2"], writes=["rw_tmp2"])
            S.op("dve", lambda e, hp=hp: e.tensor_tensor(out=t("kk"), in0=t("kk"), in1=tmp2, op=ALU.mult), reads=[n("kk"), "rw_tmp2"], writes=[n("kk")])
            S.op("dve", lambda e, hp=hp: e.tensor_scalar(out=tmp, in0=t("a"), scalar1=pc[f"ka{hp}"][:, 0:1], scalar2=pc[f"omka{hp}"][:, 0:1], op0=ALU.mult, op1=ALU.add),
                 reads=[n("a")] + PCN, writes=["rw_tmp"])
            S.op("dve", lambda e, hp=hp: e.tensor_tensor(out=t("kp"), in0=t("ks"), in1=tmp, op=ALU.mult), reads=[n("ks"), "rw_tmp"], writes=[n("kp")])
            S.op("dve", lambda e, hp=hp: e.tensor_tensor_scan(out=t("cum"), data0=rst, data1=t("lw"), initial=0.0, op0=ALU.mult, op1=ALU.add), reads=["rw_rst", n("lw")], writes=[n("cum")])
            S.op("act", lambda e, hp=hp: e.activation(out=t("G"), in_=t("cum"), func=AF.Exp), reads=[n("cum")], writes=[n("G")])
            S.op("act", lambda e, hp=hp: e.activation(out=t("Gi"), in_=t("cum"), func=AF.Exp, scale=-1.0), reads=[n("cum")], writes=[n("Gi")])
            S.op("pool", lambda e, hp=hp: e.tensor_tensor(out=tmp, in0=t("cum"), in1=t("lw"), op=ALU.subtract), reads=[n("cum"), n("lw")], writes=["rw_tmp"])
            S.op("act", lambda e, hp=hp: e.activation(out=t("Gp"), in_=tmp, func=AF.Exp), reads=["rw_tmp"], writes=[n("Gp")])
            S.op("dve", lambda e, hp=hp: e.scalar_tensor_tensor(out=t("At"), in0=t("kk"), scalar=-1.0, in1=t("Gp"), op0=ALU.mult, op1=ALU.mult), reads=[n("kk"), n("Gp")], writes=[n("At")])
            S.op("pool", lambda e, hp=hp: e.tensor_tensor(out=tmp, in0=t("kk"), in1=t("a"), op=ALU.mult), reads=[n("kk"), n("a")], writes=["rw_tmp"])
            S.op("dve", lambda e, hp=hp: e.tensor_tensor(out=t("Bt"), in0=tmp, in1=t("Gi"), op=ALU.mult), reads=["rw_tmp", n("Gi")], writes=[n("Bt")])
            S.op("pool", lambda e, hp=hp: e.tensor_tensor(out=t("Kt"), in0=t("kp"), in1=t("Gi"), op=ALU.mult), reads=[n("kp"), n("Gi")], writes=[n("Kt")])
            S.op("pool", lambda e, hp=hp: e.tensor_tensor(out=t("Rt"), in0=t("rs"), in1=t("G"), op=ALU.mult), reads=[n("rs"), n("G")], writes=[n("Rt")])
            S.op("dve", lambda e, hp=hp: e.scalar_tensor_tensor(out=tmp, in0=t("rs"), scalar=pc[f"rk{hp}"][:, 0:1], in1=t("kp"), op0=ALU.mult, op1=ALU.mult), reads=[n("rs"), n("kp")] + PCN, writes=["rw_tmp"])
            S.op("pe", lambda e: e.matmul(ps[7][:, 0:TB], lhsT=bdones, rhs=tmp, start=True, stop=True), reads=["rw_bdo", "rw_tmp"], writes=["ps7"])
            S.op("dve", lambda e, hp=hp: e.tensor_copy(out=t("bon"), in_=ps[7][:, 0:TB]), reads=["ps7"], writes=[n("bon")])
        for hp in range(2):
            pair_prep(hp, b, t0, tb)

        import os
        RWS = int(os.environ.get("RW_STOP", "99"))
        if RWS <= 1:
            continue

        def chunk_pair(hp, c, cs, bi):
            if True:
                t = lambda nm: T_[f"{nm}{hp}"]
                n = lambda nm: f"rw_{nm}{hp}"
                C_ = lambda nm: CH[(hp, nm)]
                cn = lambda nm: f"ch{hp}{nm}"
                bd = {}
                for k_, src in (("A", "At"), ("B", "Bt"), ("K", "Kt"), ("R", "Rt"), ("V", "vs")):
                    d = BD[(hp, k_, bi)]
                    bd[k_] = (d, f"bd{hp}{k_}{bi}")
                    S.op("pool", lambda e, d=d, src=src, hp=hp: e.tensor_copy(out=d[0:64, 0:64], in_=T_[f"{src}{hp}"][0:64, cs]), reads=[f"rw_{src}{hp}"], writes=[f"bd{hp}{k_}{bi}"])
                    S.op("pool", lambda e, d=d, src=src, hp=hp: e.tensor_copy(out=d[64:128, 64:128], in_=T_[f"{src}{hp}"][64:128, cs]), reads=[f"rw_{src}{hp}"], writes=[f"bd{hp}{k_}{bi}"])

                def prod(si, lk, rk, mask_i, out_nm):
                    sl, sn = slot(hp, si)
                    S.op("pe", lambda e: e.matmul(sl, lhsT=bd[lk][0], rhs=bd[rk][0], start=True, stop=True), reads=[bd[lk][1], bd[rk][1]], writes=[sn])
                    S.op("dve", lambda e: e.tensor_tensor(out=C_(out_nm), in0=sl, in1=msk[:, mask_i, :], op=ALU.mult), reads=[sn, "rw_msk"], writes=[cn(out_nm)])
                prod(0, "A", "B", 0, "M")
                prod(1, "B", "A", 1, "MT")
                prod(2, "K", "A", 1, "NakT")
                prod(3, "B", "R", 2, "QbT")
                prod(4, "K", "R", 2, "QkT")
                if RWS <= 2:
                    return
                S.op("pool", lambda e: e.tensor_tensor(out=C_("PT"), in0=C_("MT"), in1=ident, op=ALU.add), reads=[cn("MT"), "kc"], writes=[cn("PT")])
                curM, curMT, nxtM, nxtMT = "M", "MT", "M2", "M2T"
                for lev in range(5):
                    s5, n5 = slot(hp, 5)
                    s6, n6 = slot(hp, 6)
                    s7, n7 = slot(hp, 7)
                    S.op("pe", lambda e, a_=curMT, b_=curM, s5=s5: e.matmul(s5, lhsT=C_(a_), rhs=C_(b_), start=True, stop=True), reads=[cn(curMT), cn(curM)], writes=[n5])
                    if lev < 4:
                        S.op("pe", lambda e, a_=curM, b_=curMT, s6=s6: e.matmul(s6, lhsT=C_(a_), rhs=C_(b_), start=True, stop=True), reads=[cn(curM), cn(curMT)], writes=[n6])
                    evac(C_(nxtM), cn(nxtM), s5, n5)
                    if lev < 4:
                        evac(C_(nxtMT), cn(nxtMT), s6, n6)
                    S.op("pe", lambda e, a_=nxtM, s7=s7: e.matmul(s7, lhsT=C_(a_), rhs=C_("PT"), start=True, stop=True), reads=[cn(nxtM), cn("PT")], writes=[n7])
                    S.op("dve", lambda e, s7=s7: e.tensor_tensor(out=C_("PT"), in0=C_("PT"), in1=s7, op=ALU.add), reads=[cn("PT"), n7], writes=[cn("PT")])
                    curM, curMT, nxtM, nxtMT = nxtM, nxtMT, curM, curMT
                if RWS <= 3:
                    return
                s8, n8 = slot(hp, 8)
                for k_, onm in (("B", "Btok"), ("K", "Ktok"), ("V", "Vtok")):
                    S.op("pe", lambda e, k_=k_: e.transpose(out=s8, in_=bd[k_][0], identity=ident), reads=[bd[k_][1], "kc"], writes=[n8])
                    evac(C_(onm), cn(onm), s8, n8)
                if RWS <= 4:
                    return
                s9, n9 = slot(hp, 9)
                S.op("pe", lambda e: e.matmul(s9, lhsT=bd["A"][0], rhs=H[hp], start=True, stop=False), reads=[bd["A"][1], f"rw_H{hp}"], writes=[n9])
                S.op("pe", lambda e: e.matmul(s9, lhsT=C_("NakT"), rhs=C_("Vtok"), start=False, stop=True), reads=[cn("NakT"), cn("Vtok")], writes=[n9])
                evac(C_("W1"), cn("W1"), s9, n9)
                S.op("pe", lambda e: e.matmul(s9, lhsT=C_("PT"), rhs=C_("W1"), start=True, stop=True), reads=[cn("PT"), cn("W1")], writes=[n9])
                evac(C_("U"), cn("U"), s9, n9)
                s10, n10 = slot(hp, 10)
                S.op("pe", lambda e: e.matmul(s10, lhsT=H[hp], rhs=bd["R"][0], start=True, stop=False), reads=[f"rw_H{hp}", bd["R"][1]], writes=[n10])
                S.op("pe", lambda e: e.matmul(s10, lhsT=C_("U"), rhs=C_("QbT"), start=False, stop=False), reads=[cn("U"), cn("QbT")], writes=[n10])
                S.op("pe", lambda e: e.matmul(s10, lhsT=C_("Vtok"), rhs=C_("QkT"), start=False, stop=True), reads=[cn("Vtok"), cn("QkT")], writes=[n10])
                S.op("dve", lambda e: e.tensor_copy(out=C_("Yh"), in_=s10), reads=[n10], writes=[cn("Yh")])
                S.op("dve", lambda e, hp=hp: e.tensor_tensor(out=T_[f"yT{hp}"][:, cs], in0=C_("Yh")[:, 0:64], in1=s10[:, 64:128], op=ALU.add), reads=[cn("Yh"), n10], writes=[f"rw_yT{hp}"])
                s11, n11 = slot(hp, 11)
                S.op("pe", lambda e: e.matmul(s11, lhsT=C_("Btok"), rhs=C_("U"), start=True, stop=False), reads=[cn("Btok"), cn("U")], writes=[n11])
                S.op("pe", lambda e: e.matmul(s11, lhsT=C_("Ktok"), rhs=C_("Vtok"), start=False, stop=True), reads=[cn("Ktok"), cn("Vtok")], writes=[n11])
                S.op("dve", lambda e, hp=hp: e.tensor_tensor(out=H[hp], in0=H[hp], in1=s11, op=ALU.add), reads=[f"rw_H{hp}", n11], writes=[f"rw_H{hp}"])
                S.op("dve", lambda e, hp=hp, c=c: e.tensor_scalar(out=H[hp], in0=H[hp], scalar1=T_[f"G{hp}"][:, c * RW_C + RW_C - 1:c * RW_C + RW_C], scalar2=None, op0=ALU.mult),
                     reads=[f"rw_H{hp}", f"rw_G{hp}"], writes=[f"rw_H{hp}"])
        for c in range(nch):
            cs = slice(c * RW_C, (c + 1) * RW_C)
            bi = gch % 2
            gch += 1
            for hp in range(2):
                chunk_pair(hp, c, cs, bi)

        def finish_pair(hp, t0, tb):
            t = lambda nm: T_[f"{nm}{hp}"]
            n = lambda nm: f"rw_{nm}{hp}"
            S.op("pe", lambda e, hp=hp: e.matmul(ps[6][:, 0:TB], lhsT=bdavg, rhs=t("yT"), start=True, stop=True), reads=["rw_bda", n("yT")], writes=["ps6"])
            S.op("pool", lambda e, hp=hp: e.tensor_tensor(out=tmp, in0=t("yT"), in1=t("yT"), op=ALU.mult), reads=[n("yT")], writes=["rw_tmp"])
            S.op("pe", lambda e: e.matmul(ps[7][:, 0:TB], lhsT=bdavg, rhs=tmp, start=True, stop=True), reads=["rw_bda", "rw_tmp"], writes=["ps7"])
            S.op("act", lambda e: e.activation(out=tmp2, in_=ps[6][:, 0:TB], func=AF.Identity), reads=["ps6"], writes=["rw_tmp2"])
            S.op("dve", lambda e: e.tensor_tensor(out=tmp, in0=tmp2, in1=tmp2, op=ALU.mult), reads=["rw_tmp2"], writes=["rw_tmp"])
            S.op("dve", lambda e: e.tensor_tensor(out=tmp, in0=ps[7][:, 0:TB], in1=tmp, op=ALU.subtract), reads=["ps7", "rw_tmp"], writes=["rw_tmp"])
            S.op("dve", lambda e: e.tensor_scalar(out=tmp, in0=tmp, scalar1=0.0, scalar2=None, op0=ALU.max), reads=["rw_tmp"], writes=["rw_tmp"])
            S.op("act", lambda e: e.activation(out=tmp, in_=tmp, func=AF.Sqrt, bias=eps_gn[:, 0:1], scale=1.0), reads=["rw_tmp", "rw_eps"], writes=["rw_tmp"])
            S.op("dve", lambda e: e.reciprocal(out=tmp, in_=tmp), reads=["rw_tmp"], writes=["rw_tmp"])
            S.op("dve", lambda e, hp=hp: e.tensor_tensor(out=tmp2, in0=t("yT"), in1=tmp2, op=ALU.subtract), reads=[n("yT"), "rw_tmp2"], writes=["rw_tmp2"])
            S.op("dve", lambda e: e.tensor_tensor(out=tmp2, in0=tmp2, in1=tmp, op=ALU.mult), reads=["rw_tmp2", "rw_tmp"], writes=["rw_tmp2"])
            S.op("dve", lambda e, hp=hp: e.tensor_scalar(out=tmp2, in0=tmp2, scalar1=pc[f"lg{hp}"][:, 0:1], scalar2=pc[f"lb{hp}"][:, 0:1], op0=ALU.mult, op1=ALU.add),
                 reads=["rw_tmp2"] + PCN, writes=["rw_tmp2"])
            S.op("pool", lambda e, hp=hp: e.tensor_tensor(out=tmp, in0=t("bon"), in1=t("vs"), op=ALU.mult), reads=[n("bon"), n("vs"), "rw_tmp"], writes=["rw_tmp"])
            S.op("dve", lambda e: e.tensor_tensor(out=tmp2, in0=tmp2, in1=tmp, op=ALU.add), reads=["rw_tmp2", "rw_tmp"], writes=["rw_tmp2"])
            S.op("dve", lambda e, hp=hp: e.tensor_tensor(out=obf, in0=tmp2, in1=t("g"), op=ALU.mult), reads=["rw_tmp2", n("g")], writes=["rw_obf"])
            S.dma("sp", lambda e, hp=hp, t0=t0: e.dma_start(out=oT_d[512 + hp * 128:512 + (hp + 1) * 128, t0:t0 + TB], in_=obf), reads=["rw_obf"], writes=[f"oTc_d{tb}_{hp}"])
        for hp in range(2):
            finish_pair(hp, t0, tb)
    S.barrier()
    A.release(m0)


def host_consts_rwkv():
    c = {}
    p = np.arange(128)[:, None]
    q = np.arange(128)[None, :]
    same = (p // 64) == (q // 64)
    tl = p % 64
    sl = q % 64
    c["c_rwkv_masks"] = np.stack([same & (tl > sl), same & (tl < sl), same & (tl <= sl)]).astype(np.float32)
    c["c_rwkv_bdones"] = same.astype(np.float32)
    c["c_rwkv_bdavg"] = (same.astype(np.float32) / 64.0).astype(np.float32)
    r = np.ones((128, TB), np.float32)
    r[:, ::RW_C] = 0.0
    c["c_rwkv_reset"] = r
    return c
def prep_xT(P, S, A, K, x_d, xT_d, ntiles=NT):
    ps = K["ps"]
    m0 = A.mark()
    xt = [A.alloc([128, D]) for i in range(2)]
    xb = [A.alloc([128, 8, 128], BF16) for i in range(2)]
    for n in range(ntiles):
        b = n % 2
        S.dma("sp", lambda e, n=n, b=b: e.dma_start(out=xt[b], in_=x_d[n * 128:(n + 1) * 128, :]), writes=[f"px{b}"])
        for hlf in range(2):
            pb = ps[6 + hlf]
            for c4 in range(4):
                c = hlf * 4 + c4
                S.op("pe", lambda e, c=c, c4=c4, pb=pb, b=b: e.transpose(out=pb[:, c4 * 128:(c4 + 1) * 128], in_=xt[b][:, c * 128:(c + 1) * 128], identity=K["ident"]),
                     reads=[f"px{b}", "kc"], writes=[f"ps{6 + hlf}"])
            S.op("dve", lambda e, hlf=hlf, pb=pb, b=b: e.tensor_copy(out=xb[b][:, hlf * 4:(hlf + 1) * 4, :], in_=pb[:].rearrange("p (c t) -> p c t", c=4)),
                 reads=[f"ps{6 + hlf}"], writes=[f"pxb{b}"])
        S.dma("sp", lambda e, n=n, b=b: e.dma_start(out=xT_d[:, n * 128:(n + 1) * 128].rearrange("(c p) t -> p c t", p=128), in_=xb[b]), reads=[f"pxb{b}"], writes=[f"xT_d{n}"])
    S.barrier()
    A.release(m0)


def _rope_cs(n_pos, rot_dim, theta):
    inv = (1.0 / (np.float32(theta) ** (np.arange(0, rot_dim, 2, dtype=np.float32) / np.float32(rot_dim)))).astype(np.float32)
    ang = (np.arange(n_pos, dtype=np.float32)[:, None] * inv[None, :]).astype(np.float32)
    return np.cos(ang).astype(np.float32), np.sin(ang).astype(np.float32)


def _rope_tables(n_pos, rot_dim, theta, pad_front=0):
    cos, sin = _rope_cs(n_pos, rot_dim, theta)
    C = np.concatenate([cos, cos], axis=1).T
    Sg = np.concatenate([-sin, sin], axis=1).T
    if pad_front:
        C = np.concatenate([np.ones((pad_front, n_pos), np.float32), C], axis=0)
        Sg = np.concatenate([np.zeros((pad_front, n_pos), np.float32), Sg], axis=0)
    return np.ascontiguousarray(np.stack([C, Sg]).astype(np.float32))


def _perm_idx(rot_dim):
    h = rot_dim // 2
    return np.concatenate([np.arange(h, rot_dim), np.arange(0, h)])


def host_consts():
    import ml_dtypes
    c = {}
    c["c_ident"] = np.eye(128, dtype=np.float32)
    k = np.arange(128)[:, None]
    q = np.arange(512)[None, :]
    md = np.zeros((20, 128, 512), np.float32)
    for mi in range(20):
        d = mi - 3
        delta = 128 * d + q - k
        m = ((delta >= 0) & (delta <= 128)).astype(np.float32)
        m += ((delta >= 0) & (delta <= 512) & (delta % 4 == 0)).astype(np.float32)
        m += ((delta >= 0) & (delta <= 2048) & (delta % 16 == 0)).astype(np.float32)
        md[mi] = m
    c["c_mask_dil"] = md.astype(ml_dtypes.bfloat16)
    c["c_mask_mla"] = np.stack([((128 * (mi - 3) + q - k) >= 0).astype(np.float32) for mi in range(4)]).astype(ml_dtypes.bfloat16)
    c["c_rope_dil"] = _rope_tables(SEQ, 16, 500000.0)
    c["c_rope_mla"] = _rope_tables(SEQ, 32, 10000.0, pad_front=64)
    c.update(host_consts_ret())
    c.update(host_consts_rwkv())
    return c


def host_layouts(inp):
    L = {}
    w_in = inp["w_in"]
    nl = w_in.shape[0]
    p16 = _perm_idx(16)
    qcols = np.concatenate([768 + h * 64 + p16 for h in range(4)])
    kcols = np.concatenate([1024 + h * 64 + p16 for h in range(4)])
    L["dil_wperm"] = np.ascontiguousarray(w_in[:, :, np.concatenate([qcols, kcols])])
    p32 = _perm_idx(32)
    z64 = np.zeros((nl, D, 64), np.float32)
    kr = w_in[:, :, 2944:2976]
    L["mla_wkr_pad"] = np.ascontiguousarray(np.concatenate([z64, kr, z64, kr[:, :, p32]], axis=2))
    wq = inp["mla_w_q_up"]
    pq = np.concatenate([np.concatenate([h * 96 + np.arange(64), h * 96 + 64 + p32]) for h in range(4)])
    L["mla_wq_perm"] = np.ascontiguousarray(wq[:, :, pq])
    wkv = inp["mla_w_kv_up"]
    z32 = np.zeros((nl, 128, 32), np.float32)
    L["mla_wk_pad"] = np.ascontiguousarray(np.concatenate([np.concatenate([wkv[:, :, h * 128:h * 128 + 64], z32], axis=2) for h in range(4)], axis=2))
    qc = np.concatenate([h * 32 + p32 for h in range(4)])
    L["ret_wperm"] = np.ascontiguousarray(w_in[:, :, np.concatenate([qc, 128 + qc])])
    L["mla_wv"] = np.ascontiguousarray(np.concatenate([wkv[:, :, h * 128 + 64:h * 128 + 128] for h in range(4)], axis=2))
    return L


A_INPUTS = (("w_in", [DEPTH, D, N_IN]), ("dil_norm_g", [DEPTH, 256]), ("dil_wperm", [DEPTH, D, 128]),
            ("mla_wkr_pad", [DEPTH, D, 192]), ("mla_w_q_up", [DEPTH, 256, 384]), ("mla_wq_perm", [DEPTH, 256, 384]),
            ("mla_wk_pad", [DEPTH, 128, 384]), ("mla_wv", [DEPTH, 128, 256]), ("mla_q_norm_g", [DEPTH, 256]), ("mla_kv_norm_g", [DEPTH, 128]),
            ("mla_out_norm_g", [DEPTH, 256]), ("c_ident", [128, 128]), ("c_rope_dil", [2, 16, SEQ]), ("c_rope_mla", [2, 96, SEQ]), ("c_rope_ret2", [2, 64, SEQ]), ("ret_wperm", [DEPTH, D, 256]), ("ret_norm_g", [DEPTH, 256]),
            ("c_ret_decayT", [4, 128, 128]), ("c_ret_xi", [128, 512]),
            ("rwkv_mu", [DEPTH, 1024]), ("rwkv_w0", [DEPTH, 256]), ("rwkv_w_up", [DEPTH, 64, 256]), ("rwkv_a0", [DEPTH, 256]), ("rwkv_a_up", [DEPTH, 64, 256]),
            ("rwkv_g_up", [DEPTH, 128, 256]), ("rwkv_k_k", [DEPTH, 256]), ("rwkv_k_a", [DEPTH, 256]), ("rwkv_r_k", [DEPTH, 4, 64]), ("rwkv_ln_g", [DEPTH, 256]),
            ("rwkv_ln_b", [DEPTH, 256]), ("rwkv_vres_down", [DEPTH - 1, D, 32]), ("rwkv_vres_mu", [DEPTH - 1, 32]), ("rwkv_v0", [DEPTH - 1, 256]),
            ("rwkv_v_up", [DEPTH - 1, 32, 256]), ("c_rwkv_masks", [3, 128, 128]), ("c_rwkv_bdones", [128, 128]), ("c_rwkv_bdavg", [128, 128]), ("c_rwkv_reset", [128, 512]))
A_INPUTS_BF = (("c_mask_dil", [20, 128, 512]), ("c_mask_mla", [4, 128, 512]), ("c_ret_zmask", [4, 128, 64]))


def build_phase_a_test(nblk, which):
    P = Prog({})
    for nm, shp in A_INPUTS:
        P.inp(nm, shp)
    for nm, shp in A_INPUTS_BF:
        P.inp(nm, shp, BF16)
    T = nblk * TB
    x = P.inp("x", [T, D])
    oT = P.outp("oT", [D, T], BF16)
    xT_d = P.scratch("xT_d", [D, T], BF16)
    with ExitStack() as st:
        S = Sched(P.nc, st)
        A = Arena(P.nc, st, 211968)
        K = make_consts(P, S, A, st)
        make_consts_a(P, S, A, K)
        prep_xT(P, S, A, K, x, xT_d, ntiles=nblk * 4)
        if "dil" in which:
            mixer_dil(P, S, A, 0, K, xT_d, oT, nblk=nblk)
        if "ret" in which:
            mixer_ret(P, S, A, 0, K, xT_d, oT, nblk=nblk)
        if "rwkv" in which:
            vfirst_d = P.scratch("vfirst_d", [2, 128, T])
            mixer_rwkv(P, S, A, 0, K, xT_d, oT, vfirst_d, nblk=nblk)
        if "mla" in which:
            mixer_mla(P, S, A, 0, K, xT_d, oT, nblk=nblk)
        S.barrier()
        S.wait_all("sp", [])
        S.emit()
        print("instructions:", S.n_instr)
    return P
B_INPUTS = (("w_out", [DEPTH, D, D]), ("ln1_g", [DEPTH, D]), ("ln1_b", [DEPTH, D]), ("ln2_g", [DEPTH, D]), ("ln2_b", [DEPTH, D]),
            ("router_w", [DEPTH, D, NE]), ("router_b", [DEPTH, NE]), ("exp_w_gu", [DEPTH, NE, D, 2 * D]), ("exp_b_gu", [DEPTH, NE, 2 * D]),
            ("exp_w_dn", [DEPTH, NE, D, D]), ("exp_b_dn", [DEPTH, NE, D]))
PER_LAYER = {"w_in", "dil_norm_g", "dil_wperm", "mla_wkr_pad", "mla_w_q_up", "mla_wq_perm", "mla_wk_pad", "mla_wv", "mla_q_norm_g", "mla_kv_norm_g",
             "mla_out_norm_g", "ret_wperm", "ret_norm_g", "rwkv_mu", "rwkv_w0", "rwkv_w_up", "rwkv_a0", "rwkv_a_up", "rwkv_g_up", "rwkv_k_k", "rwkv_k_a",
             "rwkv_r_k", "rwkv_ln_g", "rwkv_ln_b"} | {nm for nm, _ in B_INPUTS}
PER_VLAYER = {"rwkv_vres_down", "rwkv_vres_mu", "rwkv_v0", "rwkv_v_up"}


def build_layer(first, nblk=NB_FULL):
    P = Prog({"wl": (lambda l: 0), "vl": (lambda l: 0)})
    for nm, shp in A_INPUTS + B_INPUTS:
        shp = list(shp)
        if nm in PER_LAYER or nm in PER_VLAYER:
            shp[0] = 1
        P.inp(nm, shp)
    for nm, shp in A_INPUTS_BF:
        P.inp(nm, shp, BF16)
    T = nblk * TB
    ntiles = T // 128
    x = P.inp("x", [T, D])
    out = P.outp("out", [T, D])
    if first:
        vfirst_d = P.outp("vfirst_out", [2, 128, T])
    else:
        vfirst_d = P.inp("vfirst_in", [2, 128, T])
    xT_d = P.scratch("xT_d", [D, T], BF16)
    oT_d = P.scratch("oT_d", [D, T], BF16)
    h1_d = P.scratch("h1_d", [T, D])
    h1T_d = P.scratch("h1T_d", [D, T], BF16)
    gT_d = P.scratch("gT_d", [NE, T])
    l = 0 if first else 1
    with ExitStack() as st:
        S = Sched(P.nc, st)
        A = Arena(P.nc, st, 211968)
        K = make_consts(P, S, A, st)
        make_consts_a(P, S, A, K)
        prep_xT(P, S, A, K, x, xT_d, ntiles=ntiles)
        mixer_ret(P, S, A, l, K, xT_d, oT_d, nblk=nblk)
        mixer_dil(P, S, A, l, K, xT_d, oT_d, nblk=nblk)
        mixer_rwkv(P, S, A, l, K, xT_d, oT_d, vfirst_d, nblk=nblk)
        mixer_mla(P, S, A, l, K, xT_d, oT_d, nblk=nblk)
        phase_b(P, S, A, l, K, oT_d, x, out, None, h1_d, h1T_d, gT_d, ntiles=ntiles, npass_tiles=min(8, ntiles))
        S.barrier()
        S.emit()
        P.n_instr = S.n_instr
    return P


_CACHE = {}


def layer_inputs(l, small, hc, hl):
    m = {}
    for nm, shp in list(A_INPUTS) + list(B_INPUTS) + list(A_INPUTS_BF):
        if nm in hc:
            m[nm] = hc[nm]
            continue
        a = hl[nm] if nm in hl else small[nm]
        if nm in PER_LAYER:
            a = np.ascontiguousarray(a[l:l + 1])
        elif nm in PER_VLAYER:
            j = max(l - 1, 0)
            a = np.ascontiguousarray(a[j:j + 1])
        m[nm] = a
    return m


def kernel(**inputs):
    n_cores = 8
    small = {k: np.asarray(v, dtype=np.float32) for k, v in inputs.items() if k != "x"}
    x = np.asarray(inputs["x"], dtype=np.float32)
    if "P0" not in _CACHE:
        _CACHE["P0"] = build_layer(True)
        _CACHE["P1"] = build_layer(False)
        _CACHE["hc"] = host_consts()
    hc = _CACHE["hc"]
    hl = host_layouts(small)
    cur = [np.ascontiguousarray(x[c % 4]) for c in range(n_cores)]
    vfirst = None
    for l in range(DEPTH):
        P = _CACHE["P0"] if l == 0 else _CACHE["P1"]
        shared = layer_inputs(l, small, hc, hl)
        in_maps = []
        for c in range(n_cores):
            m = dict(shared)
            m["x"] = cur[c]
            if l > 0:
                m["vfirst_in"] = vfirst[c]
            in_maps.append(m)
        res = run_bass_kernel_spmd(P.nc, in_maps, core_ids=list(range(n_cores)))
        cur = [np.asarray(res.results[c]["out"], dtype=np.float32) for c in range(n_cores)]
        if l == 0:
            vfirst = [np.asarray(res.results[c]["vfirst_out"], dtype=np.float32) for c in range(n_cores)]
    return np.stack(cur[:4], axis=0)
```
